# Optimizing a Trainium2 kernel written in Bass

```python
import jax
import jax.numpy as jnp
from jax import lax
import numpy as np

D_MODEL = 1024
BATCH = 16
SEQ = 2048
DEPTH = 4

N_MIXERS = 3
HEAD_DIM = 64
MEM_LEN = 256
MEM_HEADS = 4
MEM_WIDTH = MEM_HEADS * HEAD_DIM
MIX_WIDTH = 3 * D_MODEL // 4
CONV_WIDTH = 3
DIL_GROUPS = ((128, 1), (512, 4), (2048, 16))
DIL_HEADS = 4
DIL_WIDTH = DIL_HEADS * HEAD_DIM
DIL_QKV_WIDTH = len(DIL_GROUPS) * DIL_WIDTH
POOL_WINDOWS = (2, 4, 8, 16)
POOL_GROUP = MIX_WIDTH // len(POOL_WINDOWS)
D_FF = 2816
N_EXPERTS = 8
TOP_K = 2
D_FF_EXPERT = 3584
MOE_BLOCK = 256
LN_EPS = 1e-5
NEG_INF = -1e30
DEEPNORM_ALPHA = (2 * DEPTH) ** 0.25
DEEPNORM_BETA = (8 * DEPTH) ** -0.25
N_A = (DEPTH + 2) // 3
N_B = (DEPTH + 1) // 3
N_C = DEPTH // 3
N_DENSE = (DEPTH + 1) // 2
N_MOE = DEPTH // 2

kernel_name = "hybrid_conv_dilattn_pool_moe_encoder"


def _normal(key, shape, fan_in, scale=1.0):
    return jax.random.normal(key, shape, jnp.float32) * (scale * fan_in ** -0.5)


def setup_inputs(seed: int = 0) -> dict:
    key = jax.random.key(seed)
    ks = iter(jax.random.split(key, 32))
    d = D_MODEL
    beta = DEEPNORM_BETA
    x = jax.random.normal(next(ks), (BATCH, SEQ, d), jnp.float32)
    mem = jax.random.normal(next(ks), (BATCH, MEM_LEN, d), jnp.float32)
    w_mem_kv = jnp.concatenate([_normal(next(ks), (d, MEM_WIDTH), d),
                                _normal(next(ks), (d, MEM_WIDTH), d, beta)], axis=1)
    a_w_in = _normal(next(ks), (N_A, d, 3 * MIX_WIDTH + MEM_WIDTH), d)
    a_conv_w = _normal(next(ks), (N_A, CONV_WIDTH, MIX_WIDTH), CONV_WIDTH)
    a_w_out = _normal(next(ks), (N_A, MIX_WIDTH + MEM_WIDTH, d), MIX_WIDTH + MEM_WIDTH, beta)
    b_w_in = jnp.concatenate([_normal(next(ks), (N_B, d, 2 * DIL_QKV_WIDTH), d),
                              _normal(next(ks), (N_B, d, DIL_QKV_WIDTH), d, beta),
                              _normal(next(ks), (N_B, d, MEM_WIDTH), d)], axis=-1)
    b_w_out = _normal(next(ks), (N_B, DIL_WIDTH + MEM_WIDTH, d), DIL_WIDTH + MEM_WIDTH, beta)
    c_w_in = _normal(next(ks), (N_C, d, MIX_WIDTH + MEM_WIDTH), d)
    c_pool_w = _normal(next(ks), (N_C, len(POOL_WINDOWS), POOL_GROUP, POOL_GROUP), POOL_GROUP)
    c_pool_scale = 1.0 + 0.1 * jax.random.normal(next(ks), (N_C, MIX_WIDTH), jnp.float32)
    c_w_out = _normal(next(ks), (N_C, MIX_WIDTH + MEM_WIDTH, d), MIX_WIDTH + MEM_WIDTH, beta)
    ln_g = 1.0 + 0.02 * jax.random.normal(next(ks), (DEPTH, 2, d), jnp.float32)
    ln_b = 0.02 * jax.random.normal(next(ks), (DEPTH, 2, d), jnp.float32)
    ffn_w_gate = _normal(next(ks), (N_DENSE, d, D_FF), d)
    ffn_w_up = _normal(next(ks), (N_DENSE, d, D_FF), d)
    ffn_w_down = _normal(next(ks), (N_DENSE, D_FF, d), D_FF, beta)
    moe_router = _normal(next(ks), (N_MOE, d, N_EXPERTS), d)
    moe_w_gate = _normal(next(ks), (N_MOE, N_EXPERTS, d, D_FF_EXPERT), d)
    moe_w_up = _normal(next(ks), (N_MOE, N_EXPERTS, d, D_FF_EXPERT), d)
    moe_w_down = _normal(next(ks), (N_MOE, N_EXPERTS, D_FF_EXPERT, d), D_FF_EXPERT, beta)
    return {"x": x, "mem": mem, "w_mem_kv": w_mem_kv,
            "a_w_in": a_w_in, "a_conv_w": a_conv_w, "a_w_out": a_w_out,
            "b_w_in": b_w_in, "b_w_out": b_w_out,
            "c_w_in": c_w_in, "c_pool_w": c_pool_w, "c_pool_scale": c_pool_scale, "c_w_out": c_w_out,
            "ln_g": ln_g, "ln_b": ln_b,
            "ffn_w_gate": ffn_w_gate, "ffn_w_up": ffn_w_up, "ffn_w_down": ffn_w_down,
            "moe_router": moe_router, "moe_w_gate": moe_w_gate, "moe_w_up": moe_w_up,
            "moe_w_down": moe_w_down}


def _layer_norm(x, g, b):
    xf = x.astype(jnp.float32)
    mu = jnp.mean(xf, axis=-1, keepdims=True)
    var = jnp.mean(jnp.square(xf - mu), axis=-1, keepdims=True)
    return ((xf - mu) * lax.rsqrt(var + LN_EPS) * g + b).astype(x.dtype)


def _alibi_slopes(n):
    return 2.0 ** (-8.0 * jnp.arange(1, n + 1, dtype=jnp.float32) / n)


def _mem_attention(q, mem_k, mem_v):
    b, s, _ = q.shape
    q = q.reshape(b, s, MEM_HEADS, HEAD_DIM)
    scores = jnp.einsum('bshc,bmhc->bhsm', q, mem_k).astype(jnp.float32) * HEAD_DIM ** -0.5
    p = jax.nn.softmax(scores, axis=-1)
    out = jnp.einsum('bhsm,bmhc->bshc', p.astype(mem_v.dtype), mem_v)
    return out.reshape(b, s, MEM_WIDTH)


def _short_conv_mixer(h, conv_w):
    gate_b, gate_c, u = jnp.split(h, 3, axis=-1)
    z = jnp.pad(gate_c * u, ((0, 0), (1, 1), (0, 0)))
    conv = conv_w[0] * z[:, :-2] + conv_w[1] * z[:, 1:-1] + conv_w[2] * z[:, 2:]
    return gate_b * conv


def _dilated_group(q, k, v, window, dilation, slopes):
    b, s, h, c = q.shape
    radius = (window // 2) // dilation
    blk = radius
    n_sub = s // dilation
    nb = -(-n_sub // blk)
    lp = nb * blk

    def by_residue(a):
        return a.reshape(b, n_sub, dilation, h, c)

    qs = jnp.pad(by_residue(q), ((0, 0), (0, lp - n_sub), (0, 0), (0, 0), (0, 0)))
    qs = qs.reshape(b, nb, blk, dilation, h, c)

    def key_windows(a):
        ap = jnp.pad(by_residue(a), ((0, 0), (blk, lp - n_sub + blk), (0, 0), (0, 0), (0, 0)))
        ap = ap.reshape(b, nb + 2, blk, dilation, h, c)
        return jnp.concatenate([ap[:, :-2], ap[:, 1:-1], ap[:, 2:]], axis=2)

    kw = key_windows(k)
    vw = key_windows(v)
    scores = jnp.einsum('bnqrhc,bnkrhc->bnrhqk', qs, kw).astype(jnp.float32) * c ** -0.5
    qi = jnp.arange(blk)
    ki = jnp.arange(3 * blk)
    diff = ki[None, :] - blk - qi[:, None]
    key_j = jnp.arange(nb)[:, None] * blk - blk + ki[None, :]
    mask = (jnp.abs(diff) <= radius)[None] & ((key_j >= 0) & (key_j < n_sub))[:, None, :]
    bias = -slopes[:, None, None] * (jnp.abs(diff) * dilation).astype(jnp.float32)[None]
    scores = jnp.where(mask[None, :, None, None], scores + bias[None, None, None], NEG_INF)
    m = jnp.max(scores, axis=-1, keepdims=True)
    p = jnp.exp(scores - m)
    den = jnp.sum(p, axis=-1)
    lse = m[..., 0] + jnp.log(den)
    out = jnp.einsum('bnrhqk,bnkrhc->bnqrhc', p, vw.astype(jnp.float32))
    out = out / jnp.transpose(den, (0, 1, 4, 2, 3))[..., None]
    out = out.reshape(b, lp, dilation, h, c)[:, :n_sub].reshape(b, s, h, c)
    lse = jnp.transpose(lse, (0, 1, 4, 2, 3)).reshape(b, lp, dilation, h)[:, :n_sub].reshape(b, s, h)
    return out, lse


def _dilated_mixer(h):
    b, s, _ = h.shape
    n_g = len(DIL_GROUPS)
    q, k, v = [h[..., i * DIL_QKV_WIDTH:(i + 1) * DIL_QKV_WIDTH].reshape(b, s, n_g, DIL_HEADS, HEAD_DIM)
               for i in range(3)]
    slopes = _alibi_slopes(n_g * DIL_HEADS).reshape(n_g, DIL_HEADS)
    outs, lses = [], []
    for g, (window, dilation) in enumerate(DIL_GROUPS):
        o, l = _dilated_group(q[:, :, g], k[:, :, g], v[:, :, g], window, dilation, slopes[g])
        outs.append(o)
        lses.append(l)
    wts = jax.nn.softmax(jnp.stack(lses), axis=0)
    out = jnp.sum(wts[..., None] * jnp.stack(outs), axis=0)
    return out.reshape(b, s, DIL_WIDTH).astype(h.dtype)


def _pool_mixer(u, pool_w, pool_scale):
    b, s, _ = u.shape
    cs = jnp.pad(jnp.cumsum(u.astype(jnp.float32), axis=1), ((0, 0), (1, 0), (0, 0)))
    t = jnp.arange(s)
    pooled = []
    for g, w in enumerate(POOL_WINDOWS):
        lo = jnp.clip(t - w // 2, 0, s - 1)
        hi = jnp.clip(t + w // 2 - 1, 0, s - 1)
        csg = cs[..., g * POOL_GROUP:(g + 1) * POOL_GROUP]
        cnt = (hi - lo + 1).astype(jnp.float32)[None, :, None]
        pooled.append((jnp.take(csg, hi + 1, axis=1) - jnp.take(csg, lo, axis=1)) / cnt)
    pooled = jnp.stack(pooled, axis=2)
    ug = u.reshape(b, s, len(POOL_WINDOWS), POOL_GROUP)
    diff = (pooled - ug.astype(jnp.float32)).astype(u.dtype)
    y = jnp.einsum('bsgc,gcd->bsgd', diff, pool_w).reshape(b, s, MIX_WIDTH)
    return y * pool_scale


def _swiglu(x, w_gate, w_up, w_down):
    hdn = jax.nn.silu(jnp.einsum('bsd,df->bsf', x, w_gate)) * jnp.einsum('bsd,df->bsf', x, w_up)
    return jnp.einsum('bsf,fd->bsd', hdn, w_down)


def _moe(x, router, w_gate, w_up, w_down):
    b, s, d = x.shape
    xf = x.reshape(-1, d)
    t = xf.shape[0]
    logits = (xf @ router).astype(jnp.float32)
    top_logit, top_idx = lax.top_k(logits, TOP_K)
    top_w = jax.nn.softmax(top_logit, axis=-1)
    a = t * TOP_K
    e_flat = top_idx.reshape(a)
    tok_flat = jnp.repeat(jnp.arange(t, dtype=jnp.int32), TOP_K)
    w_flat = top_w.reshape(a)
    order = jnp.argsort(e_flat)
    e_sorted = e_flat[order]
    counts = jnp.bincount(e_flat, length=N_EXPERTS)
    padded = (counts + MOE_BLOCK - 1) // MOE_BLOCK * MOE_BLOCK
    pad_end = jnp.cumsum(padded)
    pad_start = pad_end - padded
    start = jnp.cumsum(counts) - counts
    dest = pad_start[e_sorted] + jnp.arange(a) - start[e_sorted]
    n_blocks = -(-a // MOE_BLOCK) + N_EXPERTS
    rows = n_blocks * MOE_BLOCK
    row_tok = jnp.zeros((rows,), jnp.int32).at[dest].set(tok_flat[order])
    row_w = jnp.zeros((rows,), jnp.float32).at[dest].set(w_flat[order])
    block_e = jnp.minimum(jnp.searchsorted(pad_end, jnp.arange(n_blocks) * MOE_BLOCK, side='right'),
                          N_EXPERTS - 1)
    xb = xf[row_tok].reshape(n_blocks, MOE_BLOCK, d)

    def expert_block(args):
        xblk, e = args
        hdn = jax.nn.silu(xblk @ w_gate[e]) * (xblk @ w_up[e])
        return hdn @ w_down[e]

    yb = lax.map(expert_block, (xb, block_e)).reshape(rows, d)
    y = jax.ops.segment_sum(yb * row_w[:, None].astype(yb.dtype), row_tok, num_segments=t)
    return y.reshape(b, s, d)


def reference(x, mem, w_mem_kv, a_w_in, a_conv_w, a_w_out, b_w_in, b_w_out,
              c_w_in, c_pool_w, c_pool_scale, c_w_out, ln_g, ln_b,
              ffn_w_gate, ffn_w_up, ffn_w_down, moe_router, moe_w_gate, moe_w_up, moe_w_down):
    b = x.shape[0]
    m_len = mem.shape[1]
    kv = jnp.einsum('bmd,de->bme', mem, w_mem_kv).reshape(b, m_len, 2, MEM_HEADS, HEAD_DIM)
    mem_k, mem_v = kv[:, :, 0], kv[:, :, 1]
    for i in range(DEPTH):
        kind = i % N_MIXERS
        j = i // N_MIXERS
        if kind == 0:
            h = jnp.einsum('bsd,de->bse', x, a_w_in[j])
            mix = _short_conv_mixer(h[..., :3 * MIX_WIDTH], a_conv_w[j])
            q_mem = h[..., 3 * MIX_WIDTH:]
            w_out = a_w_out[j]
        elif kind == 1:
            h = jnp.einsum('bsd,de->bse', x, b_w_in[j])
            mix = _dilated_mixer(h[..., :3 * DIL_QKV_WIDTH])
            q_mem = h[..., 3 * DIL_QKV_WIDTH:]
            w_out = b_w_out[j]
        else:
            h = jnp.einsum('bsd,de->bse', x, c_w_in[j])
            mix = _pool_mixer(h[..., :MIX_WIDTH], c_pool_w[j], c_pool_scale[j])
            q_mem = h[..., MIX_WIDTH:]
            w_out = c_w_out[j]
        mem_out = _mem_attention(q_mem, mem_k, mem_v)
        y = jnp.einsum('bse,ed->bsd', jnp.concatenate([mix, mem_out], axis=-1), w_out)
        x = _layer_norm(DEEPNORM_ALPHA * x + y, ln_g[i, 0], ln_b[i, 0])
        f = i // 2
        if i % 2 == 0:
            y = _swiglu(x, ffn_w_gate[f], ffn_w_up[f], ffn_w_down[f])
        else:
            y = _moe(x, moe_router[f], moe_w_gate[f], moe_w_up[f], moe_w_down[f])
        x = _layer_norm(DEEPNORM_ALPHA * x + y, ln_g[i, 1], ln_b[i, 1])
    return x
```

```python
import contextlib
import numpy as np
import concourse.bass as bass
import concourse.mybir as mybir
from concourse.bass_utils import run_bass_kernel_spmd

F32 = mybir.dt.float32
BF16 = mybir.dt.bfloat16
AF = mybir.ActivationFunctionType
ALU = mybir.AluOpType
AX = mybir.AxisListType

D = 1024
S = 2048
NT = 4
TS = 512
DEPTH = 4
ALPHA = float((2 * DEPTH) ** 0.25)
LN_EPS = 1e-5
MIXW = 768
DFF = 2816
DFFE = 3584
NE = 8
DIL = ((128, 1), (512, 4), (2048, 16))
POOLW = (2, 4, 8, 16)
ARENA_BYTES = 96 * 1024
MOE_MODE = "cap"
CAP = 640
NSC = CAP // 128
STILES = ((0, 512), (512, CAP - 512))


class Tk:
    __slots__ = ("name", "w", "r", "rd")

    def __init__(self, name=""):
        self.name = name
        self.w = None
        self.r = {}
        self.rd = []


class Chan:
    __slots__ = ("sem", "count", "name")

    def __init__(self, name):
        self.name = name
        self.sem = None
        self.count = 0


class Op:
    __slots__ = ("eng", "fn", "deps", "signal", "sigval", "kind", "chan", "dmaval")


ENGS = ("pe", "act", "dve", "pool", "sp")


class Sched:
    def __init__(self):
        self.ops = []
        self.extra = {e: [] for e in ENGS}
        self.last = {e: None for e in ENGS}
        self.chans = []
        self.last_dma = {}

    def chan(self, name):
        c = Chan(name)
        self.chans.append(c)
        return c

    def op(self, eng, fn, reads=(), writes=(), chan=None):
        o = Op()
        o.eng = eng
        o.fn = fn
        o.signal = False
        o.sigval = None
        o.kind = "d" if chan is not None else "c"
        o.chan = chan
        o.dmaval = None
        deps = {}
        for t in reads:
            if t.w is not None:
                deps[id(t.w)] = t.w
        for t in writes:
            if t.w is not None:
                deps[id(t.w)] = t.w
            for r in t.r.values():
                deps[id(r)] = r
            for r in t.rd:
                deps[id(r)] = r
        for d in self.extra[eng]:
            deps[id(d)] = d
        self.extra[eng] = []
        dl = []
        for d in deps.values():
            if d is o:
                continue
            if d.kind == "c" and o.kind == "c" and d.eng == "pe" and eng == "pe":
                continue
            d.signal = True
            dl.append(d)
        o.deps = dl
        if chan is not None:
            chan.count += 16
            o.dmaval = chan.count
            self.last_dma[id(chan)] = o
        for t in reads:
            if o.kind == "d":
                t.rd.append(o)
            else:
                t.r[eng] = o
        for t in writes:
            t.w = o
            t.r = {}
            t.rd = []
        if o.kind == "c":
            self.last[eng] = o
        self.ops.append(o)
        return o

    def barrier(self):
        deps = [o for o in self.last.values() if o is not None]
        deps += list(self.last_dma.values())
        for e in ENGS:
            self.extra[e] = list(deps)

    def finalize(self):
        cnt = {e: 0 for e in ENGS}
        for o in self.ops:
            if o.kind == "c" and o.signal:
                cnt[o.eng] += 1
                o.sigval = cnt[o.eng]

    def emit(self, nc, block, engsem):
        handles = {"pe": "tensor", "act": "scalar", "dve": "vector", "pool": "gpsimd", "sp": "sync"}
        final = [(c.sem, c.count) for c in self.chans if c.count > 0]

        def run(engname):
            def body(e):
                waited = {}
                for o in self.ops:
                    if o.eng != engname:
                        continue
                    for d in o.deps:
                        if d.kind == "d":
                            sem, val = d.chan.sem, d.dmaval
                        else:
                            sem, val = engsem[d.eng], d.sigval
                        k = id(sem)
                        if waited.get(k, 0) >= val:
                            continue
                        e.wait_ge(sem, val)
                        waited[k] = val
                    ins = o.fn(e)
                    if o.kind == "d":
                        ins.then_inc(o.chan.sem, 16)
                    elif o.signal:
                        ins.then_inc(engsem[o.eng], 1)
                if engname == "sp":
                    for sem, val in final:
                        e.wait_ge(sem, val)
            return body

        for en in ENGS:
            getattr(block, handles[en])(run(en))


def build_program(layers, nseq, debug=False):
    nc = bass.Bass("TRN2", target_bir_lowering=False)
    Sd = Sched()

    def dram_in(name, shape):
        return nc.dram_tensor(name, list(shape), F32, kind="ExternalInput").ap()

    xT = dram_in("xT", [nseq, D, S])
    memT = dram_in("memT", [nseq, D, 256])
    w_mem_kv = dram_in("w_mem_kv", [D, 512])
    a_w_in = dram_in("a_w_in", [2, D, 2560])
    a_w_out = dram_in("a_w_out", [2, D, D])
    b_w_in = dram_in("b_w_in", [1, D, 2560])
    b_w_out = dram_in("b_w_out", [1, 512, D])
    c_w_in = dram_in("c_w_in", [1, D, D])
    c_w_out = dram_in("c_w_out", [1, D, D])
    pwfull = dram_in("pwfull", [MIXW, MIXW])
    ffn_w_gate = dram_in("ffn_w_gate", [2, D, DFF])
    ffn_w_up = dram_in("ffn_w_up", [2, D, DFF])
    ffn_w_down = dram_in("ffn_w_down", [2, DFF, D])
    moe_router = dram_in("moe_router", [2, D, NE])
    moe_w_gate = dram_in("moe_w_gate", [2, NE, D, DFFE])
    moe_w_up = dram_in("moe_w_up", [2, NE, D, DFFE])
    moe_w_down = dram_in("moe_w_down", [2, NE, DFFE, D])
    c_lnp = dram_in("c_lnp", [128, 128])
    c_convw = dram_in("c_convw", [128, 36])
    c_pscale = dram_in("c_pscale", [128, 6])
    c_ident = dram_in("c_ident", [128, 128])
    c_expb = dram_in("c_expb", [128, 12 * 256])
    c_edge = dram_in("c_edge", [128, 64])
    c_lstrict = dram_in("c_lstrict", [128, 128])
    c_iota = dram_in("c_iota", [128, CAP])
    c_slotid = dram_in("c_slotid", [128, 8])
    yT = nc.dram_tensor("yT", [nseq, D, S], F32, kind="ExternalOutput").ap()
    coef_scr = nc.dram_tensor("coef_scr", [NE, S], F32, kind="Internal").ap()
    pos_scr = nc.dram_tensor("pos_scr", [NE, S], F32, kind="Internal").ap()
    xd_scr = nc.dram_tensor("xd_scr", [NE, D, CAP], BF16, kind="Internal").ap()
    dbg_out = None
    if debug:
        dbg_out = nc.dram_tensor("dbg", [len(layers), nseq, D, S], F32, kind="ExternalOutput").ap()

    es = contextlib.ExitStack()
    with es:
        def sb(name, shape, dt):
            return es.enter_context(nc.sbuf_tensor(name, list(shape), dt))

        xacc = sb("xacc", [128, 8, S], F32)
        xb = sb("xb", [128, 8, S], BF16)
        lnp = sb("lnp", [128, 256], F32)
        convw = sb("convw", [128, 36], F32)
        pscale = sb("pscale", [128, 6], F32)
        router = sb("router", [128, 2, 8, NE], F32)
        ident = sb("ident", [128, 128], F32)
        ones32 = sb("ones32", [128, 128], F32)
        onespad = sb("onespad", [128, 2, 128], BF16)
        kTpad = sb("kTpad", [128, 4, 256], BF16)
        vpad = sb("vpad", [128, 4, 2, 128], BF16)
        expb = sb("expb", [128, 12, 256], BF16)
        edge = sb("edge", [128, 4, 16], F32)
        slotid = sb("slotid", [128, 8], F32)
        identb = sb("identb", [128, 128], BF16)
        arena = sb("arena", [128, ARENA_BYTES // 2], BF16)
        ps = es.enter_context(nc.psum_tensor("ps", [128, 8, 512], F32))

        engsem = {e: es.enter_context(nc.semaphore("s_" + e)) for e in ENGS}

        XA = [Tk("xa%d" % t) for t in range(NT)]
        XB = [Tk("xb%d" % t) for t in range(NT)]
        PB = [Tk("ps%d" % b) for b in range(8)]
        CONST = Tk("const")
        KV = Tk("kv")
        ch_const = Sd.chan("const")
        ch_x = [Sd.chan("x%d" % t) for t in range(NT)]
        ch_out = [Sd.chan("o%d" % t) for t in range(NT)]
        ch_w = [Sd.chan("w%d" % i) for i in range(6)]
        ch_misc = [Sd.chan("m%d" % i) for i in range(4)]
        ch_coef = Sd.chan("coef")
        ch_cb = [Sd.chan("cb%d" % i) for i in range(2)]
        ch_xd = [Sd.chan("xd%d" % i) for i in range(2)]
        ch_xdl = [Sd.chan("xdl%d" % i) for i in range(2)]
        ch_pos = Sd.chan("pos")

        def tsl(tt):
            return slice(tt * TS, (tt + 1) * TS)

        class Carver:
            def __init__(self, base=None, nbytes=ARENA_BYTES):
                self.off = 0
                self.base = arena if base is None else base
                self.nbytes = nbytes

            def take(self, shape, dt):
                esz = 4 if dt == F32 else 2
                n = 1
                for s_ in shape[1:]:
                    n *= s_
                nbytes = n * esz
                assert self.off % 4 == 0
                assert self.off + nbytes <= self.nbytes, (self.off, nbytes)
                v = self.base[:, self.off // 2:(self.off + nbytes) // 2]
                if dt == F32:
                    v = v.bitcast(F32)
                self.off += nbytes
                if shape[0] != 128:
                    v = v[0:shape[0]]
                if len(shape) == 3:
                    v = v.rearrange("p (a b) -> p a b", a=shape[1])
                elif len(shape) == 4:
                    v = v.rearrange("p (a b c) -> p a b c", a=shape[1], b=shape[2])
                return v

        def dma(eng, out, in_, chan, reads=(), writes=()):
            Sd.op(eng, lambda e, o=out, i=in_: e.dma_start(out=o, in_=i), reads, writes, chan=chan)

        def mm(out, pairs, reads, writes):
            def fn(e, out=out, pairs=pairs):
                n = len(pairs)
                ins = None
                for i, (l, r) in enumerate(pairs):
                    ins = e.matmul(out, l, r, start=(i == 0), stop=(i == n - 1))
                return ins
            Sd.op("pe", fn, reads, writes)

        def act(out, in_, func, reads, writes, bias=None, scale=None):
            kw = {}
            if bias is not None:
                kw["bias"] = bias
            if scale is not None:
                kw["scale"] = scale
            Sd.op("act", lambda e, o=out, i=in_, f=func, kw=kw: e.activation(o, i, f, **kw), reads, writes)

        def tt_(eng, out, in0, in1, op, reads, writes):
            Sd.op(eng, lambda e, o=out, a=in0, b=in1, op=op: e.tensor_tensor(o, a, b, op), reads, writes)

        def ts_(eng, out, in0, s1, s2, op0, op1, reads, writes):
            if op1 is None:
                Sd.op(eng, lambda e, o=out, a=in0, s1=s1, op0=op0: e.tensor_scalar(o, a, s1, None, op0), reads, writes)
            else:
                Sd.op(eng, lambda e, o=out, a=in0, s1=s1, s2=s2, op0=op0, op1=op1:
                      e.tensor_scalar(o, a, s1, s2, op0, op1), reads, writes)

        def stt(eng, out, in0, scalar, in1, op0, op1, reads, writes):
            Sd.op(eng, lambda e, o=out, a=in0, s=scalar, b=in1, op0=op0, op1=op1:
                  e.scalar_tensor_tensor(o, a, s, b, op0, op1), reads, writes)

        def cp(eng, out, in_, reads, writes):
            Sd.op(eng, lambda e, o=out, i=in_: e.tensor_copy(o, i), reads, writes)

        def mset(eng, ap, val, writes):
            Sd.op(eng, lambda e, a=ap, v=val: e.memset(a, v), (), writes)

        dma("sp", lnp[:, 0:128], c_lnp, ch_const, (), (CONST,))
        dma("sp", convw[:], c_convw, ch_const, (), (CONST,))
        dma("sp", pscale[:], c_pscale, ch_const, (), (CONST,))
        dma("sp", ident[:], c_ident, ch_const, (), (CONST,))
        dma("pool", expb[:].rearrange("p a b -> p (a b)"), c_expb, ch_const, (), (CONST,))
        dma("sp", edge[:].rearrange("p a b -> p (a b)"), c_edge, ch_const, (), (CONST,))
        for f in range(2):
            dma("sp", router[:, f], moe_router[f].rearrange("(c p) e -> p c e", p=128), ch_const, (), (CONST,))
        dma("sp", slotid[:], c_slotid, ch_const, (), (CONST,))
        Sd.op("act", lambda e: e.mul(lnp[:, 128:256], lnp[:, 0:128], ALPHA), (CONST,), (CONST,))
        cp("dve", identb[:], ident[:], (CONST,), (CONST,))
        mset("dve", ones32[:], 1.0, (CONST,))
        mset("dve", onespad[:], 0.0, (CONST,))
        mset("dve", onespad[:, 0, 0:64], 1.0, (CONST,))
        mset("dve", onespad[:, 1, 64:128], 1.0, (CONST,))
        mset("dve", kTpad[:], 0.0, (KV,))
        mset("dve", vpad[:], 0.0, (KV,))

        def lncol(l, j, c):
            return (l * 2 + j) * 8 + c

        def layer_norm(tt, l, j, sq, tmp, SQ, TMP, final, bank_a, bank_b):
            v = xacc[:, :, tsl(tt)]
            act(sq, v, AF.Square, (XA[tt],), (SQ,))
            mm(ps[:, bank_a, :], [(ones32[:], xacc[:, c, tsl(tt)]) for c in range(8)],
               (XA[tt], CONST), (PB[bank_a],))
            mm(ps[:, bank_b, :], [(ones32[:], sq[:, c, :]) for c in range(8)], (SQ, CONST), (PB[bank_b],))
            mean, msq, var, rstd = tmp[:, 0], tmp[:, 1], tmp[:, 2], tmp[:, 3]
            Sd.op("act", lambda e: e.mul(mean, ps[:, bank_a, :], 1.0 / D), (PB[bank_a],), (TMP[0],))
            tt_("dve", msq, mean, mean, ALU.mult, (TMP[0],), (TMP[1],))
            stt("dve", var, ps[:, bank_b, :], 1.0 / D, msq, ALU.mult, ALU.subtract, (PB[bank_b], TMP[1]), (TMP[2],))
            ts_("dve", var, var, LN_EPS, None, ALU.add, None, (TMP[2],), (TMP[2],))
            act(var, var, AF.Sqrt, (TMP[2],), (TMP[2],))
            Sd.op("dve", lambda e: e.reciprocal(rstd, var), (TMP[2],), (TMP[3],))
            mb = mean.unsqueeze(1).to_broadcast([128, 8, TS])
            rb = rstd.unsqueeze(1).to_broadcast([128, 8, TS])
            tt_("dve", sq, v, mb, ALU.subtract, (XA[tt], TMP[0]), (SQ,))
            tt_("pool", sq, sq, rb, ALU.mult, (SQ, TMP[3]), (SQ,))
            for c in range(8):
                col = lncol(l, j, c)
                g, b_ = lnp[:, col:col + 1], lnp[:, 64 + col:64 + col + 1]
                ga, ba = lnp[:, 128 + col:128 + col + 1], lnp[:, 192 + col:192 + col + 1]
                if final:
                    act(xacc[:, c, tsl(tt)], sq[:, c, :], AF.Identity, (SQ, CONST), (XA[tt],), bias=b_, scale=g)
                else:
                    act(xb[:, c, tsl(tt)], sq[:, c, :], AF.Identity, (SQ, CONST), (XB[tt],), bias=b_, scale=g)
                    act(xacc[:, c, tsl(tt)], sq[:, c, :], AF.Identity, (SQ, CONST), (XA[tt],), bias=ba, scale=ga)

        def load_w_cols(slot, slot_tk, chan, src2d, colgroups):
            off = 0
            for (c0, n) in colgroups:
                dma("pool", slot[:, :, off:off + n], src2d[:, c0:c0 + n].rearrange("(c p) e -> p c e", p=128),
                    chan, (), (slot_tk,))
                off += n

        def mem_attention(qm, QM, memout, MO, pT, PT, rden, RD):
            for tt in range(NT):
                for pr in range(2):
                    bn, bd = 4 + (pr % 2) * 2, 5 + (pr % 2) * 2
                    for hh in range(2):
                        h = 2 * pr + hh
                        b0 = (hh % 2) * 2
                        for mc in range(2):
                            mm(ps[:, b0 + mc, :], [(kTpad[:, h, mc * 128:(mc + 1) * 128], qm[:, pr, tsl(tt)])],
                               (KV, QM[tt]), (PB[b0 + mc],))
                        act(pT[:, hh], ps[:, b0:b0 + 2, :], AF.Exp, (PB[b0], PB[b0 + 1]), (PT[hh],))
                    mm(ps[:, bn, :], [(vpad[:, 2 * pr + hh, mc, :], pT[:, hh, mc, :]) for hh in range(2) for mc in range(2)],
                       (KV, PT[0], PT[1]), (PB[bn],))
                    mm(ps[:, bd, :], [(onespad[:, hh, :], pT[:, hh, mc, :]) for hh in range(2) for mc in range(2)],
                       (CONST, PT[0], PT[1]), (PB[bd],))
                    Sd.op("dve", lambda e, o=rden[:, pr % 2, :], i=ps[:, bd, :]: e.reciprocal(o, i), (PB[bd],), (RD[pr % 2],))
                    tt_("dve", memout[:, pr, tsl(tt)], ps[:, bn, :], rden[:, pr % 2, :], ALU.mult,
                        (PB[bn], RD[pr % 2]), (MO[tt],))

        def outproj_ln(l, wout2d, nmix, cat_aps, CAT, car):
            kc_n = len(cat_aps)
            wo = car.take([128, kc_n, D], BF16)
            WO = Tk("wo")
            dma("pool", wo, wout2d.rearrange("(c p) e -> p c e", p=128), ch_w[0], (), (WO,))
            sq = car.take([128, 8, TS], F32)
            tmp = car.take([128, 4, TS], F32)
            SQ = Tk("sq")
            TMP = [Tk("tmp%d" % i) for i in range(4)]
            for tt in range(NT):
                for dc in range(8):
                    bank = dc % 4
                    mm(ps[:, bank, :], [(wo[:, kc, dc * 128:(dc + 1) * 128], cat_aps[kc][:, tsl(tt)]) for kc in range(kc_n)],
                       (WO,) + tuple(CAT[tt]), (PB[bank],))
                    tt_("dve", xacc[:, dc, tsl(tt)], ps[:, bank, :], xacc[:, dc, tsl(tt)], ALU.add,
                        (PB[bank], XA[tt]), (XA[tt],))
                layer_norm(tt, l, 0, sq, tmp, SQ, TMP, False, 4 + (tt % 2) * 2, 5 + (tt % 2) * 2)

        def inproj_block(slot, SLOT, ncols, consumer):
            nch = ncols // 128
            k = 0
            for tt in range(NT):
                for ec in range(nch):
                    bank = k % 4
                    k += 1
                    mm(ps[:, bank, :], [(slot[:, kc, ec * 128:(ec + 1) * 128], xb[:, kc, tsl(tt)]) for kc in range(8)],
                       (SLOT, XB[tt]), (PB[bank],))
                    consumer(tt, ec, bank)

        def mixer_a(l, jA):
            car = Carver()
            mix = car.take([128, 6, S], BF16)
            memout = car.take([128, 2, S], BF16)
            qm = car.take([128, 2, S], BF16)
            slots = [car.take([128, 8, 384], BF16) for _ in range(2)]
            SL = [Tk("sl0"), Tk("sl1")]
            gcs = car.take([128, 2, TS], F32)
            GCS = [Tk("gcs0"), Tk("gcs1")]
            z = car.take([128, S + 2], F32)
            Z = Tk("z")
            gb = car.take([128, S], BF16)
            GB = Tk("gb")
            tmpc = car.take([128, 2, S], F32)
            TC = [Tk("tc0"), Tk("tc1")]
            pT = car.take([128, 2, 2, TS], BF16)
            PT = [Tk("pt0"), Tk("pt1")]
            rden = car.take([128, 2, TS], F32)
            RD = [Tk("rd0"), Tk("rd1")]
            MIX = [Tk("mix%d" % t) for t in range(NT)]
            MO = [Tk("mo%d" % t) for t in range(NT)]
            QM = [Tk("qm%d" % t) for t in range(NT)]
            w2d = a_w_in[jA]
            mset("pool", z[:, 0:1], 0.0, (Z,))
            mset("pool", z[:, S + 1:S + 2], 0.0, (Z,))
            for c in range(7):
                si = c % 2
                if c < 6:
                    load_w_cols(slots[si], SL[si], ch_w[1 + si], w2d,
                                [(c * 128, 128), (768 + c * 128, 128), (1536 + c * 128, 128)])
                else:
                    load_w_cols(slots[si], SL[si], ch_w[1 + si], w2d, [(2304, 256)])
                if c < 6:
                    def consumer(tt, ec, bank, c=c):
                        if ec == 0:
                            act(gb[:, tsl(tt)], ps[:, bank, :], AF.Copy, (PB[bank],), (GB,))
                        elif ec == 1:
                            act(gcs[:, tt % 2, :], ps[:, bank, :], AF.Copy, (PB[bank],), (GCS[tt % 2],))
                        else:
                            tt_("dve", z[:, 1 + tt * TS:1 + (tt + 1) * TS], ps[:, bank, :], gcs[:, tt % 2, :], ALU.mult,
                                (PB[bank], GCS[tt % 2]), (Z,))
                    inproj_block(slots[si], SL[si], 384, consumer)
                    wc = lambda k, c=c: convw[:, (jA * 3 + k) * 6 + c:(jA * 3 + k) * 6 + c + 1]
                    act(tmpc[:, 0, :], z[:, 0:S], AF.Identity, (Z, CONST), (TC[0],), scale=wc(0))
                    stt("dve", tmpc[:, 1, :], z[:, 1:S + 1], wc(1), tmpc[:, 0, :], ALU.mult, ALU.add,
                        (Z, CONST, TC[0]), (TC[1],))
                    stt("dve", tmpc[:, 0, :], z[:, 2:S + 2], wc(2), tmpc[:, 1, :], ALU.mult, ALU.add,
                        (Z, CONST, TC[1]), (TC[0],))
                    tt_("pool", mix[:, c, :], tmpc[:, 0, :], gb[:], ALU.mult, (TC[0], GB), tuple(MIX))
                else:
                    def consumer(tt, ec, bank):
                        Sd.op("act", lambda e, o=qm[:, ec, tsl(tt)], i=ps[:, bank, :]: e.mul(o, i, 0.125),
                              (PB[bank],), (QM[tt],))
                    inproj_block(slots[si], SL[si], 256, consumer)
            mem_attention(qm, QM, memout, MO, pT, PT, rden, RD)
            Sd.barrier()
            car2 = Carver()
            car2.take([128, 10, S], BF16)
            cat = [mix[:, c, :] for c in range(6)] + [memout[:, c, :] for c in range(2)]
            CAT = [(MIX[t], MO[t]) for t in range(NT)]
            outproj_ln(l, a_w_out[jA], 6, cat, CAT, car2)
            Sd.barrier()

        def mixer_c(l, jC):
            car = Carver()
            mix = car.take([128, 6, S], BF16)
            memout = car.take([128, 2, S], BF16)
            qm = car.take([128, 2, S], BF16)
            slots = [car.take([128, 8, 256], BF16) for _ in range(2)]
            SL = [Tk("sl0"), Tk("sl1")]
            pw = car.take([128, 6, MIXW], BF16)
            PW = Tk("pw")
            PADW = 16
            bufs = [car.take([128, S + 2 * PADW], F32) for _ in range(3)]
            BU = [Tk("bu%d" % i) for i in range(3)]
            pT = car.take([128, 2, 2, TS], BF16)
            PT = [Tk("pt0"), Tk("pt1")]
            rden = car.take([128, 2, TS], F32)
            RD = [Tk("rd0"), Tk("rd1")]
            etmp = car.take([128, 16], F32)
            ET = Tk("et")
            MIX = [Tk("mix%d" % t) for t in range(NT)]
            MO = [Tk("mo%d" % t) for t in range(NT)]
            QM = [Tk("qm%d" % t) for t in range(NT)]
            w2d = c_w_in[jC]
            dma("pool", pw, pwfull.rearrange("(c p) e -> p c e", p=128), ch_w[3], (), (PW,))
            for i in range(3):
                mset("pool", bufs[i][:, 0:PADW], 0.0, (BU[i],))
                mset("pool", bufs[i][:, S + PADW:S + 2 * PADW], 0.0, (BU[i],))
            ub, pa, pb = bufs

            def window(prs, wi):
                w = POOLW[wi]
                P = PADW
                lo, hi = -15, S + 15
                tt_("dve", pa[prs, P + lo:P + hi], ub[prs, P + lo - 1:P + hi - 1], ub[prs, P + lo:P + hi], ALU.add,
                    (BU[0],), (BU[1],))
                cur, other, curi, othi = pa, pb, 1, 2
                step = 1
                lo_c, hi_c = lo, hi
                ww = 2
                while ww < w:
                    lo_n, hi_n = lo_c + step, hi_c - step
                    tt_("dve", other[prs, P + lo_n:P + hi_n], cur[prs, P + lo_n - step:P + hi_n - step],
                        cur[prs, P + lo_n + step:P + hi_n + step], ALU.add, (BU[curi],), (BU[othi],))
                    cur, other, curi, othi = other, cur, othi, curi
                    lo_c, hi_c = lo_n, hi_n
                    step *= 2
                    ww *= 2
                return cur, curi

            def pool_half(c, prs, wi):
                cur, curi = window(prs, wi)
                w = POOLW[wi]
                P = PADW
                tt_("pool", cur[prs, P:P + 8], cur[prs, P:P + 8], edge[prs, wi, 0:8], ALU.mult, (BU[curi], CONST), (BU[curi],))
                tt_("pool", cur[prs, P + S - 8:P + S], cur[prs, P + S - 8:P + S], edge[prs, wi, 8:16], ALU.mult,
                    (BU[curi], CONST), (BU[curi],))
                stt("dve", mix[prs, c, :], cur[prs, P:P + S], 1.0 / w, ub[prs, P:P + S], ALU.mult, ALU.subtract,
                    (BU[curi], BU[0]), tuple(MIX))

            for c in range(4):
                si = c % 2
                if c < 3:
                    load_w_cols(slots[si], SL[si], ch_w[1 + si], w2d, [(c * 256, 256)])
                    for ecl in range(2):
                        cc = c * 2 + ecl
                        for tt in range(NT):
                            bank = tt % 4
                            mm(ps[:, bank, :], [(slots[si][:, kc, ecl * 128:(ecl + 1) * 128], xb[:, kc, tsl(tt)]) for kc in range(8)],
                               (SL[si], XB[tt]), (PB[bank],))
                            act(ub[:, PADW + tt * TS:PADW + (tt + 1) * TS], ps[:, bank, :], AF.Copy, (PB[bank],), (BU[0],))
                        g0 = (cc * 128) // 192
                        g1 = (cc * 128 + 64) // 192
                        if g0 == g1:
                            pool_half(cc, slice(0, 128), g0)
                        else:
                            pool_half(cc, slice(0, 64), g0)
                            pool_half(cc, slice(64, 128), g1)
                else:
                    load_w_cols(slots[si], SL[si], ch_w[1 + si], w2d, [(768, 256)])

                    def consumer(tt, ec, bank):
                        Sd.op("act", lambda e, o=qm[:, ec, tsl(tt)], i=ps[:, bank, :]: e.mul(o, i, 0.125),
                              (PB[bank],), (QM[tt],))
                    inproj_block(slots[si], SL[si], 256, consumer)
            for tt in range(NT):
                for dc in range(6):
                    mm(ps[:, dc, :], [(pw[:, kc, dc * 128:(dc + 1) * 128], mix[:, kc, tsl(tt)]) for kc in range(6)],
                       (PW, MIX[tt]), (PB[dc],))
                for dc in range(6):
                    act(mix[:, dc, tsl(tt)], ps[:, dc, :], AF.Identity, (PB[dc], CONST), (MIX[tt],), scale=pscale[:, dc:dc + 1])
            mem_attention(qm, QM, memout, MO, pT, PT, rden, RD)
            Sd.barrier()
            car2 = Carver()
            car2.take([128, 10, S], BF16)
            cat = [mix[:, c, :] for c in range(6)] + [memout[:, c, :] for c in range(2)]
            CAT = [(MIX[t], MO[t]) for t in range(NT)]
            outproj_ln(l, c_w_out[jC], 6, cat, CAT, car2)
            Sd.barrier()

        def mixer_b(l, jB):
            car = Carver()
            mix = car.take([128, 2, S], BF16)
            memout = car.take([128, 2, S], BF16)
            qm = car.take([128, 2, S], BF16)
            slots = [car.take([128, 8, 384], BF16) for _ in range(2)]
            SL = [Tk("sl0"), Tk("sl1")]
            qs = car.take([128, S], BF16)
            QS = Tk("qs")
            kEO = car.take([128, 2, S], BF16)
            KEO = Tk("keo")
            vEO = car.take([128, 2, 16, 128], BF16)
            VEO = Tk("veo")
            nacc = car.take([128, S], F32)
            dacc = car.take([128, S], F32)
            NA, DA = Tk("na"), Tk("da")
            ebuf = car.take([128, 2, 256], F32)
            EB = [Tk("eb0"), Tk("eb1")]
            pTd = car.take([128, 2, 256], BF16)
            PTD = [Tk("ptd0"), Tk("ptd1")]
            pT = car.take([128, 2, 2, TS], BF16)
            PT = [Tk("pt0"), Tk("pt1")]
            rden = car.take([128, 2, TS], F32)
            RD = [Tk("rd0"), Tk("rd1")]
            MIX = [Tk("mix%d" % t) for t in range(NT)]
            MO = [Tk("mo%d" % t) for t in range(NT)]
            QM = [Tk("qm%d" % t) for t in range(NT)]
            w2d = b_w_in[jB]
            mset("pool", kEO[:], 0.0, (KEO,))
            mset("pool", vEO[:], 0.0, (VEO,))
            it = 0
            for pr in range(2):
                mset("pool", nacc[:], 0.0, (NA,))
                mset("pool", dacc[:], 0.0, (DA,))
                for g, (win, dil) in enumerate(DIL):
                    ci = g * 2 + pr
                    si = it % 2
                    it += 1
                    load_w_cols(slots[si], SL[si], ch_w[1 + si], w2d,
                                [(ci * 128, 128), (768 + ci * 128, 128), (1536 + ci * 128, 128)])
                    slot = slots[si]
                    for tt in range(NT):
                        b0 = (tt % 2) * 2
                        mm(ps[:, b0, :], [(slot[:, kc, 0:128], xb[:, kc, tsl(tt)]) for kc in range(8)],
                           (SL[si], XB[tt]), (PB[b0],))
                        Sd.op("act", lambda e, o=qs[:, tsl(tt)], i=ps[:, b0, :]: e.mul(o, i, 0.125), (PB[b0],), (QS,))
                        mm(ps[:, b0 + 1, :], [(slot[:, kc, 128:256], xb[:, kc, tsl(tt)]) for kc in range(8)],
                           (SL[si], XB[tt]), (PB[b0 + 1],))
                        act(kEO[0:64, 0, tsl(tt)], ps[0:64, b0 + 1, :], AF.Copy, (PB[b0 + 1],), (KEO,))
                        cp("dve", kEO[64:128, 1, tsl(tt)], ps[64:128, b0 + 1, :], (PB[b0 + 1],), (KEO,))
                    n_sub = S // dil
                    nkb = n_sub // 128
                    ti = 0
                    for r in range(dil):
                        for kb in range(nkb):
                            bank = 4 + (ti // 4) % 2
                            sub = (ti % 4) * 128
                            start = kb * 128 * dil + r
                            toks = slice(start, start + 127 * dil + 1, dil)
                            mm(ps[:, bank, sub:sub + 128], [(xb[:, kc, toks], slot[:, kc, 256:384]) for kc in range(8)],
                               (SL[si],) + tuple(XB), (PB[bank],))
                            act(vEO[:, 0, ti, 0:64], ps[:, bank, sub:sub + 64], AF.Copy, (PB[bank],), (VEO,))
                            cp("dve", vEO[:, 1, ti, 64:128], ps[:, bank, sub + 64:sub + 128], (PB[bank],), (VEO,))
                            ti += 1
                    ti = 0
                    for r in range(dil):
                        for kb in range(nkb):
                            j0 = max(0, kb * 128 - 64)
                            j1 = min(n_sub, kb * 128 + 192)
                            nq = j1 - j0
                            col0 = j0 - (kb * 128 - 64)
                            kst = kb * 128 * dil + r
                            ktoks = slice(kst, kst + 127 * dil + 1, dil)
                            qst = j0 * dil + r
                            qtoks = slice(qst, qst + (nq - 1) * dil + 1, dil)
                            for hh in range(2):
                                h = 2 * pr + hh
                                bs = hh
                                mm(ps[:, bs, 0:nq], [(kEO[:, hh, ktoks], qs[:, qtoks])], (KEO, QS), (PB[bs],))
                                act(ebuf[:, hh, 0:nq], ps[:, bs, 0:nq], AF.Exp, (PB[bs],), (EB[hh],))
                                tt_("pool", pTd[:, hh, 0:nq], ebuf[:, hh, 0:nq], expb[:, g * 4 + h, col0:col0 + nq], ALU.mult,
                                    (EB[hh], CONST), (PTD[hh],))
                            bn, bd = 2 + (ti % 2), 6 + (ti % 2)
                            mm(ps[:, bn, 0:nq], [(vEO[:, hh, ti, :], pTd[:, hh, 0:nq]) for hh in range(2)],
                               (VEO, PTD[0], PTD[1]), (PB[bn],))
                            mm(ps[:, bd, 0:nq], [(onespad[:, hh, :], pTd[:, hh, 0:nq]) for hh in range(2)],
                               (CONST, PTD[0], PTD[1]), (PB[bd],))
                            tt_("dve", nacc[:, qtoks], ps[:, bn, 0:nq], nacc[:, qtoks], ALU.add, (PB[bn], NA), (NA,))
                            tt_("dve", dacc[:, qtoks], ps[:, bd, 0:nq], dacc[:, qtoks], ALU.add, (PB[bd], DA), (DA,))
                            ti += 1
                Sd.op("dve", lambda e: e.reciprocal(dacc[:], dacc[:]), (DA,), (DA,))
                tt_("pool", mix[:, pr, :], nacc[:], dacc[:], ALU.mult, (NA, DA), tuple(MIX))
            si = it % 2
            load_w_cols(slots[si], SL[si], ch_w[1 + si], w2d, [(2304, 256)])

            def consumer(tt, ec, bank):
                Sd.op("act", lambda e, o=qm[:, ec, tsl(tt)], i=ps[:, bank, :]: e.mul(o, i, 0.125),
                      (PB[bank],), (QM[tt],))
            inproj_block(slots[si][:, :, 0:256], SL[si], 256, consumer)
            mem_attention(qm, QM, memout, MO, pT, PT, rden, RD)
            Sd.barrier()
            car2 = Carver()
            car2.take([128, 6, S], BF16)
            cat = [mix[:, c, :] for c in range(2)] + [memout[:, c, :] for c in range(2)]
            CAT = [(MIX[t], MO[t]) for t in range(NT)]
            outproj_ln(l, b_w_out[jB], 2, cat, CAT, car2)
            Sd.barrier()

        class FFNState:
            pass

        def ffn_setup():
            st = FFNState()
            car = Carver()
            st.wg = [car.take([128, 8, 512], BF16) for _ in range(2)]
            st.wu = [car.take([128, 8, 512], BF16) for _ in range(2)]
            st.wd = [car.take([128, 4, D], BF16) for _ in range(2)]
            st.WS = [Tk("ws0"), Tk("ws1")]
            st.h = car.take([128, 2, 4, TS], BF16)
            st.H = [Tk("h0"), Tk("h1")]
            st.sg = car.take([128, 2, TS], F32)
            st.SG = [Tk("sg0"), Tk("sg1")]
            st.sgc = car.take([128, 2, TS], F32)
            st.SGC = [Tk("sgc0"), Tk("sgc1")]
            st.cb = car.take([128, 2, S], F32)
            st.CB = [Tk("cb0"), Tk("cb1")]
            st.blk = 0
            st.car = car
            return st

        def ffn_pass(st, wg2d, wu2d, wd2d, dff, coef_slot):
            f0 = 0
            while f0 < dff:
                fb = min(512, dff - f0)
                nfc = fb // 128
                si = st.blk % 2
                st.blk += 1
                ch = ch_w[si * 3:(si * 3) + 3]
                dma("pool", st.wg[si][:, :, 0:fb], wg2d[:, f0:f0 + fb].rearrange("(c p) e -> p c e", p=128), ch[0], (), (st.WS[si],))
                dma("pool", st.wu[si][:, :, 0:fb], wu2d[:, f0:f0 + fb].rearrange("(c p) e -> p c e", p=128), ch[1], (), (st.WS[si],))
                dma("pool", st.wd[si][:, 0:nfc, :], wd2d[f0:f0 + fb, :].rearrange("(c p) e -> p c e", p=128), ch[2], (), (st.WS[si],))
                WSt = st.WS[si]

                def gu(tt, fc, si=si, WSt=WSt):
                    bg, bu = (fc % 2) * 2, (fc % 2) * 2 + 1
                    hp = tt % 2
                    mm(ps[:, bg, :], [(st.wg[si][:, kc, fc * 128:(fc + 1) * 128], xb[:, kc, tsl(tt)]) for kc in range(8)],
                       (WSt, XB[tt]), (PB[bg],))
                    mm(ps[:, bu, :], [(st.wu[si][:, kc, fc * 128:(fc + 1) * 128], xb[:, kc, tsl(tt)]) for kc in range(8)],
                       (WSt, XB[tt]), (PB[bu],))
                    act(st.sg[:, fc % 2, :], ps[:, bg, :], AF.Silu, (PB[bg],), (st.SG[fc % 2],))
                    if coef_slot is None:
                        tt_("dve", st.h[:, hp, fc, :], ps[:, bu, :], st.sg[:, fc % 2, :], ALU.mult,
                            (PB[bu], st.SG[fc % 2]), (st.H[hp],))
                    else:
                        tt_("pool", st.sgc[:, fc % 2, :], st.sg[:, fc % 2, :], st.cb[:, coef_slot, tsl(tt)], ALU.mult,
                            (st.SG[fc % 2], st.CB[coef_slot]), (st.SGC[fc % 2],))
                        tt_("dve", st.h[:, hp, fc, :], ps[:, bu, :], st.sgc[:, fc % 2, :], ALU.mult,
                            (PB[bu], st.SGC[fc % 2]), (st.H[hp],))

                def down(tt, half, si=si, WSt=WSt, nfc=nfc):
                    hp = tt % 2
                    for dl in range(4):
                        dc = half * 4 + dl
                        mm(ps[:, 4 + dl, :], [(st.wd[si][:, fc, dc * 128:(dc + 1) * 128], st.h[:, hp, fc, :]) for fc in range(nfc)],
                           (WSt, st.H[hp]), (PB[4 + dl],))
                    Sd.op("dve", lambda e, o=xacc[:, half * 4:half * 4 + 4, tsl(tt)], a=ps[:, 4:8, :]:
                          e.tensor_tensor(o, a, o, ALU.add), (PB[4], PB[5], PB[6], PB[7], XA[tt]), (XA[tt],))

                for tt in range(NT + 1):
                    for fc in range(nfc):
                        if tt < NT:
                            gu(tt, fc)
                        if tt > 0 and fc == (nfc // 2) - 1:
                            down(tt - 1, 0)
                    if tt > 0:
                        down(tt - 1, 1)
                f0 += fb

        def dense_ffn(l, f):
            st = ffn_setup()
            ffn_pass(st, ffn_w_gate[f], ffn_w_up[f], ffn_w_down[f], DFF, None)
            Sd.barrier()

        def moe_ffn(l, f):
            sparse = (MOE_MODE == "cap")
            car = Carver()
            posm = car.take([128, 16, NE], F32)
            lg = car.take([128, 16, NE], F32)
            m1 = car.take([128, 16], F32)
            m2 = car.take([128, 16], F32)
            mk1 = car.take([128, 16, NE], F32)
            mk2 = car.take([128, 16, NE], F32)
            l2 = car.take([128, 16, NE], F32)
            w1 = car.take([128, 16], F32)
            w2 = car.take([128, 16], F32)
            coef = car.take([128, 16, NE], F32)
            coefT = car.take([NE, S], F32)
            R = Tk("route")
            fl = lambda a: a.rearrange("p a b -> p (a b)")
            for tc_ in range(16):
                tt = tc_ // 4
                mm(ps[:, 0, tc_ * NE:(tc_ + 1) * NE],
                   [(xacc[:, kc, tc_ * 128:(tc_ + 1) * 128], router[:, f, kc, :]) for kc in range(8)],
                   (XA[tt], CONST), (PB[0],))
            Sd.op("act", lambda e: e.mul(fl(lg), ps[:, 0, 0:16 * NE], 1.0 / ALPHA), (PB[0],), (R,))
            Sd.op("dve", lambda e: e.tensor_reduce(m1[:], lg[:], AX.X, ALU.max), (R,), (R,))
            tt_("dve", mk1[:], lg[:], m1[:].unsqueeze(2).to_broadcast([128, 16, NE]), ALU.is_equal, (R,), (R,))
            stt("dve", l2[:], mk1[:], -1e30, lg[:], ALU.mult, ALU.add, (R,), (R,))
            Sd.op("dve", lambda e: e.tensor_reduce(m2[:], l2[:], AX.X, ALU.max), (R,), (R,))
            tt_("dve", mk2[:], l2[:], m2[:].unsqueeze(2).to_broadcast([128, 16, NE]), ALU.is_equal, (R,), (R,))
            if sparse:
                msk = car.take([128, 16, NE], F32)
                tot = car.take([128, 16, NE], F32)
                off = car.take([128, 16, NE], F32)
                pos = car.take([128, 16, NE], F32)
                posT = car.take([NE, S], F32)
                lst = car.take([128, 128], F32)
                dma("sp", lst, c_lstrict, ch_misc[2], (), (R,))
                tt_("dve", msk[:], mk1[:], mk2[:], ALU.add, (R,), (R,))
                mm(ps[:, 5, 0:128], [(lst, fl(msk))], (R,), (PB[5],))
                mm(ps[:, 6, 0:128], [(ones32[:], fl(msk))], (R, CONST), (PB[6],))
                Sd.op("act", lambda e: e.copy(fl(tot), ps[:, 6, 0:128]), (PB[6],), (R,))
                mset("dve", off[:, 0, :], 0.0, (R,))
                for tc_ in range(1, 16):
                    tt_("dve", off[:, tc_, :], off[:, tc_ - 1, :], tot[:, tc_ - 1, :], ALU.add, (R,), (R,))
                tt_("dve", fl(pos), ps[:, 5, 0:128], fl(off), ALU.add, (PB[5], R), (R,))
                stt("dve", posm[:], pos[:], 1.0, msk[:], ALU.add, ALU.mult, (R,), (R,))
                ts_("dve", posm[:], posm[:], -1.0, None, ALU.add, None, (R,), (R,))
                for tc_ in range(16):
                    bank = 4 + tc_ // 4
                    sub = (tc_ % 4) * 128
                    Sd.op("pe", lambda e, o=ps[0:NE, bank, sub:sub + 128], i=posm[:, tc_, :]: e.transpose(o, i, ident[:]),
                          (R, CONST), (PB[bank],))
                for q in range(4):
                    cp("dve", posT[:, q * 512:(q + 1) * 512], ps[0:NE, 4 + q, :], (PB[4 + q],), (R,))
                dma("sp", pos_scr, posT, ch_pos, (R,), (R,))
            tt_("dve", w2[:], m2[:], m1[:], ALU.subtract, (R,), (R,))
            act(w2[:], w2[:], AF.Exp, (R,), (R,))
            ts_("dve", w1[:], w2[:], 1.0, None, ALU.add, None, (R,), (R,))
            Sd.op("dve", lambda e: e.reciprocal(w1[:], w1[:]), (R,), (R,))
            ts_("dve", w2[:], w1[:], -1.0, 1.0, ALU.mult, ALU.add, (R,), (R,))
            tt_("dve", mk1[:], mk1[:], w1[:].unsqueeze(2).to_broadcast([128, 16, NE]), ALU.mult, (R,), (R,))
            tt_("dve", mk2[:], mk2[:], w2[:].unsqueeze(2).to_broadcast([128, 16, NE]), ALU.mult, (R,), (R,))
            tt_("dve", coef[:], mk1[:], mk2[:], ALU.add, (R,), (R,))
            for tc_ in range(16):
                bank = 1 + tc_ // 4
                sub = (tc_ % 4) * 128
                Sd.op("pe", lambda e, o=ps[0:NE, bank, sub:sub + 128], i=coef[:, tc_, :]: e.transpose(o, i, ident[:]),
                      (R, CONST), (PB[bank],))
            for q in range(4):
                Sd.op("act", lambda e, o=coefT[:, q * 512:(q + 1) * 512], i=ps[0:NE, 1 + q, :]: e.copy(o, i), (PB[1 + q],), (R,))
            dma("sp", coef_scr, coefT, ch_coef, (R,), (R,))
            Sd.barrier()
            if not sparse:
                st = ffn_setup()
                for ex in range(NE):
                    cs = ex % 2
                    dma("sp", st.cb[:, cs, :], coef_scr[ex:ex + 1, :].to_broadcast([128, S]), ch_cb[cs], (R,), (st.CB[cs],))
                    ffn_pass(st, moe_w_gate[f, ex], moe_w_up[f, ex], moe_w_down[f, ex], DFFE, cs)
                Sd.barrier()
                return
            carD = Carver()
            posm = carD.take([128, 16, NE], F32)
            xtm = carD.take([128, 16, D], BF16)
            Pb = [carD.take([128, 16, CAP], BF16) for _ in range(2)]
            xds = [carD.take([128, 8, CAP], BF16) for _ in range(2)]
            iot = carD.take([128, CAP], F32)
            XTM, IOT, XDSCR = Tk("xtm"), Tk("iot"), Tk("xdscr")
            PTK = [Tk("p0"), Tk("p1")]
            XDS = [Tk("xds0"), Tk("xds1")]
            dma("sp", iot, c_iota, ch_misc[3], (), (IOT,))
            for tc_ in range(16):
                bank = tc_ % 2
                psb = ps[:, bank, :].bitcast(BF16)
                for kc in range(8):
                    Sd.op("pe", lambda e, o=psb[:, kc * 128:(kc + 1) * 128], i=xb[:, kc, tc_ * 128:(tc_ + 1) * 128]:
                          e.transpose(o, i, identb[:]), (XB[tc_ // 4], CONST), (PB[bank],))
                if tc_ % 2 == 0:
                    act(xtm[:, tc_, :], psb, AF.Copy, (PB[bank],), (XTM,))
                else:
                    cp("dve", xtm[:, tc_, :], psb, (PB[bank],), (XTM,))
            for ex in range(NE):
                pi = ex % 2
                for tc_ in range(16):
                    ts_("dve", Pb[pi][:, tc_, :], iot, posm[:, tc_, ex:ex + 1], None, ALU.is_equal, None, (IOT, R), (PTK[pi],))
                for dc in range(8):
                    b0 = 2 + (dc % 3) * 2
                    for ti, (s0, sn) in enumerate(STILES):
                        mm(ps[:, b0 + ti, 0:sn],
                           [(xtm[:, tc_, dc * 128:(dc + 1) * 128], Pb[pi][:, tc_, s0:s0 + sn]) for tc_ in range(16)],
                           (XTM, PTK[pi]), (PB[b0 + ti],))
                    act(xds[pi][:, dc, 0:512], ps[:, b0, 0:512], AF.Copy, (PB[b0],), (XDS[pi],))
                    cp("dve", xds[pi][:, dc, 512:CAP], ps[:, b0 + 1, 0:CAP - 512], (PB[b0 + 1],), (XDS[pi],))
                dma("sp", xd_scr[ex].rearrange("(c p) s -> p c s", p=128), xds[pi], ch_xd[pi], (XDS[pi],), (XDSCR,))
            Sd.barrier()
            carE = Carver()
            xd = [carE.take([128, 8, CAP], BF16) for _ in range(2)]
            PT = carE.take([128, NSC, S], BF16)
            yd = carE.take([128, NSC, D], F32)
            ydb = carE.take([128, NSC, D], BF16)
            wg = [carE.take([128, 8, 256], BF16) for _ in range(2)]
            wu = [carE.take([128, 8, 256], BF16) for _ in range(2)]
            wd = [carE.take([128, 2, D], BF16) for _ in range(2)]
            carX = Carver(base=xb[:].rearrange("p a b -> p (a b)"), nbytes=32 * 1024)
            hT = carX.take([128, 2, 2, CAP], BF16)
            sg = carX.take([128, 2, CAP], F32)
            posB = carX.take([128, S], F32)
            coefB = carX.take([128, S], F32)
            XD = [Tk("xd0"), Tk("xd1")]
            WS = [Tk("ws0"), Tk("ws1")]
            HT = [Tk("ht0"), Tk("ht1")]
            SG = [Tk("sg0"), Tk("sg1")]
            PTT, YD, YDB, POSB, COEFB = Tk("pt"), Tk("yd"), Tk("ydb"), Tk("posb"), Tk("coefb")
            state = {"blk": 0, "r": 0}

            def down_part(prev, half):
                si, hp = prev
                for sc in range(NSC):
                    bank = 4 + state["r"] % 4
                    state["r"] += 1
                    mm(ps[:, bank, :],
                       [(hT[:, hp, fc, sc * 128:(sc + 1) * 128], wd[si][:, fc, half * 512:(half + 1) * 512]) for fc in range(2)],
                       (WS[si], HT[hp]), (PB[bank],))
                    ydv = yd[:, sc, half * 512:(half + 1) * 512]
                    tt_("dve", ydv, ps[:, bank, :], ydv, ALU.add, (PB[bank], YD), (YD,))

            for ex in range(NE):
                xi = ex % 2
                dma("sp", xd[xi], xd_scr[ex].rearrange("(c p) s -> p c s", p=128), ch_xdl[xi], (XDSCR,), (XD[xi],))
                dma("sp", posB, pos_scr[ex:ex + 1, :].to_broadcast([128, S]), ch_cb[0], (R,), (POSB,))
                dma("sp", coefB, coef_scr[ex:ex + 1, :].to_broadcast([128, S]), ch_cb[1], (R,), (COEFB,))
                mset("pool", yd[:].rearrange("p a b -> p (a b)"), 0.0, (YD,))
                for sc in range(NSC):
                    stt("dve", PT[:, sc, :], posB, slotid[:, sc:sc + 1], coefB, ALU.is_equal, ALU.mult,
                        (POSB, COEFB, CONST), (PTT,))
                prev = None
                for b in range(DFFE // 256):
                    si = state["blk"] % 2
                    hp = si
                    state["blk"] += 1
                    f0 = b * 256
                    ch = ch_w[si * 3:si * 3 + 3]
                    dma("pool", wg[si], moe_w_gate[f, ex][:, f0:f0 + 256].rearrange("(c p) e -> p c e", p=128), ch[0], (), (WS[si],))
                    dma("pool", wu[si], moe_w_up[f, ex][:, f0:f0 + 256].rearrange("(c p) e -> p c e", p=128), ch[1], (), (WS[si],))
                    dma("pool", wd[si], moe_w_down[f, ex][f0:f0 + 256, :].rearrange("(c p) e -> p c e", p=128), ch[2], (), (WS[si],))
                    for fc in range(2):
                        for ti, (s0, sn) in enumerate(STILES):
                            mm(ps[:, ti, 0:sn],
                               [(wg[si][:, kc, fc * 128:(fc + 1) * 128], xd[xi][:, kc, s0:s0 + sn]) for kc in range(8)],
                               (WS[si], XD[xi]), (PB[ti],))
                            mm(ps[:, 2 + ti, 0:sn],
                               [(wu[si][:, kc, fc * 128:(fc + 1) * 128], xd[xi][:, kc, s0:s0 + sn]) for kc in range(8)],
                               (WS[si], XD[xi]), (PB[2 + ti],))
                        for ti, (s0, sn) in enumerate(STILES):
                            act(sg[:, fc, s0:s0 + sn], ps[:, ti, 0:sn], AF.Silu, (PB[ti],), (SG[fc],))
                        for ti, (s0, sn) in enumerate(STILES):
                            tt_("dve", hT[:, hp, fc, s0:s0 + sn], ps[:, 2 + ti, 0:sn], sg[:, fc, s0:s0 + sn], ALU.mult,
                                (PB[2 + ti], SG[fc]), (HT[hp],))
                        if prev is not None:
                            down_part(prev, fc)
                    prev = (si, hp)
                down_part(prev, 0)
                down_part(prev, 1)
                act(ydb[:].rearrange("p a b -> p (a b)"), yd[:].rearrange("p a b -> p (a b)"), AF.Copy, (YD,), (YDB,))
                for tt in range(NT):
                    for dc in range(8):
                        bank = 4 + state["r"] % 4
                        state["r"] += 1
                        mm(ps[:, bank, :], [(ydb[:, sc, dc * 128:(dc + 1) * 128], PT[:, sc, tsl(tt)]) for sc in range(NSC)],
                           (YDB, PTT), (PB[bank],))
                        tt_("dve", xacc[:, dc, tsl(tt)], ps[:, bank, :], xacc[:, dc, tsl(tt)], ALU.add,
                            (PB[bank], XA[tt]), (XA[tt],))
            Sd.barrier()

        def ln2(l, final):
            car = Carver()
            sq = car.take([128, 8, TS], F32)
            tmp = car.take([128, 4, TS], F32)
            SQ = Tk("sq")
            TMP = [Tk("tmp%d" % i) for i in range(4)]
            for tt in range(NT):
                layer_norm(tt, l, 1, sq, tmp, SQ, TMP, final, (tt % 4) * 2, (tt % 4) * 2 + 1)
            Sd.barrier()

        for s in range(nseq):
            for tt in range(NT):
                dma("sp", xacc[:, :, tsl(tt)], xT[s][:, tsl(tt)].rearrange("(c p) t -> p c t", p=128), ch_x[tt], (), (XA[tt],))
            for tt in range(NT):
                cp("dve", xb[:, :, tsl(tt)], xacc[:, :, tsl(tt)], (XA[tt],), (XB[tt],))
                Sd.op("act", lambda e, v=xacc[:, :, tsl(tt)]: e.mul(v, v, ALPHA), (XA[tt], XB[tt]), (XA[tt],))
            car = Carver()
            memb = car.take([128, 8, 256], BF16)
            wkv = car.take([128, 8, 512], BF16)
            MB, WK = Tk("memb"), Tk("wkv")
            dma("pool", memb, memT[s].rearrange("(c p) m -> p c m", p=128), ch_misc[0], (), (MB,))
            dma("pool", wkv, w_mem_kv.rearrange("(c p) e -> p c e", p=128), ch_misc[1], (), (WK,))
            for ec in range(2):
                mm(ps[:, ec, 0:256], [(wkv[:, kc, ec * 128:(ec + 1) * 128], memb[:, kc, :]) for kc in range(8)],
                   (MB, WK), (PB[ec],))
                act(kTpad[0:64, 2 * ec, :], ps[0:64, ec, 0:256], AF.Copy, (PB[ec],), (KV,))
                cp("dve", kTpad[64:128, 2 * ec + 1, :], ps[64:128, ec, 0:256], (PB[ec],), (KV,))
            for mc in range(2):
                mm(ps[:, 2 + mc, 0:256], [(memb[:, kc, mc * 128:(mc + 1) * 128], wkv[:, kc, 256:512]) for kc in range(8)],
                   (MB, WK), (PB[2 + mc],))
                for h in range(4):
                    o = vpad[:, h, mc, (h % 2) * 64:(h % 2) * 64 + 64]
                    i = ps[:, 2 + mc, h * 64:(h + 1) * 64]
                    if h % 2 == 0:
                        act(o, i, AF.Copy, (PB[2 + mc],), (KV,))
                    else:
                        cp("dve", o, i, (PB[2 + mc],), (KV,))
            Sd.barrier()
            for li, l in enumerate(layers):
                kind, j = l % 3, l // 3
                if kind == 0:
                    mixer_a(l, j)
                elif kind == 1:
                    mixer_b(l, j)
                else:
                    mixer_c(l, j)
                if l % 2 == 0:
                    dense_ffn(l, l // 2)
                else:
                    moe_ffn(l, l // 2)
                last = (li == len(layers) - 1)
                ln2(l, last)
                if debug and not last:
                    for tt in range(NT):
                        dma("sp", dbg_out[li, s][:, tsl(tt)].rearrange("(c p) t -> p c t", p=128), xacc[:, :, tsl(tt)],
                            ch_out[tt], (XA[tt],), ())
            for tt in range(NT):
                dma("sp", yT[s][:, tsl(tt)].rearrange("(c p) t -> p c t", p=128), xacc[:, :, tsl(tt)],
                    ch_out[tt], (XA[tt],), ())

        Sd.finalize()
        for c in Sd.chans:
            if c.count > 0:
                c.sem = es.enter_context(nc.semaphore("c_" + c.name))
        with nc.Block() as block:
            Sd.emit(nc, block, engsem)
    return nc


def _alibi_slopes(n):
    return 2.0 ** (-8.0 * np.arange(1, n + 1, dtype=np.float64) / n)


def _host_consts(ln_g, ln_b, a_conv_w, c_pool_w, c_pool_scale):
    c = {}
    lnp = np.zeros((128, 128), np.float32)
    lnp[:, 0:64] = ln_g.reshape(8, 8, 128).transpose(2, 0, 1).reshape(128, 64)
    lnp[:, 64:128] = ln_b.reshape(8, 8, 128).transpose(2, 0, 1).reshape(128, 64)
    c["c_lnp"] = lnp
    c["c_convw"] = np.ascontiguousarray(a_conv_w.reshape(6, 6, 128).transpose(2, 0, 1).reshape(128, 36))
    c["c_pscale"] = np.ascontiguousarray(c_pool_scale.reshape(6, 128).T)
    c["c_ident"] = np.eye(128, dtype=np.float32)
    slopes = _alibi_slopes(12).reshape(3, 4)
    p = np.arange(128)[:, None]
    jl = np.arange(256)[None, :]
    delta = jl - 64 - p
    eb = np.zeros((128, 12, 256), np.float32)
    for g, (win, dil) in enumerate(DIL):
        for h in range(4):
            v = np.exp(-slopes[g, h] * np.abs(delta) * dil)
            eb[:, g * 4 + h, :] = np.where(np.abs(delta) <= 64, v, 0.0)
    c["c_expb"] = eb.reshape(128, 12 * 256)
    ed = np.ones((128, 4, 16), np.float32)
    for wi, w in enumerate(POOLW):
        for t in range(8):
            lo, hi = max(t - w // 2, 0), min(t + w // 2 - 1, S - 1)
            ed[:, wi, t] = w / float(hi - lo + 1)
            t2 = S - 8 + t
            lo, hi = max(t2 - w // 2, 0), min(t2 + w // 2 - 1, S - 1)
            ed[:, wi, 8 + t] = w / float(hi - lo + 1)
    c["c_edge"] = ed.reshape(128, 64)
    c["c_lstrict"] = np.triu(np.ones((128, 128), np.float32), 1)
    c["c_iota"] = np.ascontiguousarray(np.broadcast_to(np.arange(CAP, dtype=np.float32)[None, :], (128, CAP)))
    c["c_slotid"] = (np.arange(128, dtype=np.float32)[:, None] + 128.0 * np.arange(8, dtype=np.float32)[None, :])
    pw = np.zeros((MIXW, MIXW), np.float32)
    for g in range(4):
        pw[g * 192:(g + 1) * 192, g * 192:(g + 1) * 192] = c_pool_w[0, g]
    c["pwfull"] = pw
    return c


LAYERS = [0, 1, 2, 3]
NSEQ = 2
_CACHE = {}


def kernel(x, mem, w_mem_kv, a_w_in, a_conv_w, a_w_out, b_w_in, b_w_out,
           c_w_in, c_pool_w, c_pool_scale, c_w_out, ln_g, ln_b,
           ffn_w_gate, ffn_w_up, ffn_w_down, moe_router, moe_w_gate, moe_w_up, moe_w_down,
           _layers=None, _debug=False, _ncores=8):
    layers = LAYERS if _layers is None else _layers
    f32 = lambda a: np.ascontiguousarray(np.asarray(a, dtype=np.float32))
    x = f32(x)
    mem = f32(mem)
    consts = _host_consts(f32(ln_g), f32(ln_b), f32(a_conv_w), f32(c_pool_w), f32(c_pool_scale))
    shared = {
        "w_mem_kv": f32(w_mem_kv), "a_w_in": f32(a_w_in), "a_w_out": f32(a_w_out),
        "b_w_in": f32(b_w_in), "b_w_out": f32(b_w_out), "c_w_in": f32(c_w_in), "c_w_out": f32(c_w_out),
        "ffn_w_gate": f32(ffn_w_gate), "ffn_w_up": f32(ffn_w_up), "ffn_w_down": f32(ffn_w_down),
        "moe_router": f32(moe_router), "moe_w_gate": f32(moe_w_gate), "moe_w_up": f32(moe_w_up),
        "moe_w_down": f32(moe_w_down),
    }
    shared.update(consts)
    key = (tuple(layers), NSEQ, _debug)
    if key not in _CACHE:
        _CACHE[key] = build_program(layers, NSEQ, _debug)
    nc = _CACHE[key]
    in_maps = []
    for c in range(_ncores):
        m = dict(shared)
        m["xT"] = np.ascontiguousarray(x[c * NSEQ:(c + 1) * NSEQ].transpose(0, 2, 1))
        m["memT"] = np.ascontiguousarray(mem[c * NSEQ:(c + 1) * NSEQ].transpose(0, 2, 1))
        in_maps.append(m)
    res = run_bass_kernel_spmd(nc, in_maps, core_ids=list(range(_ncores)))
    out = np.empty((_ncores * NSEQ, S, D), np.float32)
    for c in range(_ncores):
        out[c * NSEQ:(c + 1) * NSEQ] = res.results[c]["yT"].transpose(0, 2, 1)
    if _debug:
        return out, [r.get("dbg") for r in res.results]
    return out
```

```python
import contextlib
import numpy as np
import concourse.bass as bass
import concourse.mybir as mybir
from concourse.bass_utils import run_bass_kernel_spmd

F32 = mybir.dt.float32
BF16 = mybir.dt.bfloat16
AF = mybir.ActivationFunctionType
ALU = mybir.AluOpType
AX = mybir.AxisListType

D = 1024
S = 2048
NT = 4
TS = 512
DEPTH = 4
ALPHA = float((2 * DEPTH) ** 0.25)
LN_EPS = 1e-5
MIXW = 768
DFF = 2816
DFFE = 3584
NE = 8
DIL = ((128, 1), (512, 4), (2048, 16))
POOLW = (2, 4, 8, 16)
ARENA_BYTES = 96 * 1024
MOE_MODE = "cap"
CAP = 640
NSC = CAP // 128
STILES = ((0, 512), (512, CAP - 512))


class Tk:
    __slots__ = ("name", "w", "r", "rd")

    def __init__(self, name=""):
        self.name = name
        self.w = None
        self.r = {}
        self.rd = []


class Chan:
    __slots__ = ("sem", "count", "name")

    def __init__(self, name):
        self.name = name
        self.sem = None
        self.count = 0


class Op:
    __slots__ = ("eng", "fn", "deps", "signal", "sigval", "kind", "chan", "dmaval")


ENGS = ("pe", "act", "dve", "pool", "sp")


class Sched:
    def __init__(self):
        self.ops = []
        self.extra = {e: [] for e in ENGS}
        self.last = {e: None for e in ENGS}
        self.chans = []
        self.last_dma = {}

    def chan(self, name):
        c = Chan(name)
        self.chans.append(c)
        return c

    def op(self, eng, fn, reads=(), writes=(), chan=None):
        o = Op()
        o.eng = eng
        o.fn = fn
        o.signal = False
        o.sigval = None
        o.kind = "d" if chan is not None else "c"
        o.chan = chan
        o.dmaval = None
        deps = {}
        for t in reads:
            if t.w is not None:
                deps[id(t.w)] = t.w
        for t in writes:
            if t.w is not None:
                deps[id(t.w)] = t.w
            for r in t.r.values():
                deps[id(r)] = r
            for r in t.rd:
                deps[id(r)] = r
        for d in self.extra[eng]:
            deps[id(d)] = d
        self.extra[eng] = []
        dl = []
        for d in deps.values():
            if d is o:
                continue
            if d.kind == "c" and o.kind == "c" and d.eng == "pe" and eng == "pe":
                continue
            d.signal = True
            dl.append(d)
        o.deps = dl
        if chan is not None:
            chan.count += 16
            o.dmaval = chan.count
            self.last_dma[id(chan)] = o
        for t in reads:
            if o.kind == "d":
                t.rd.append(o)
            else:
                t.r[eng] = o
        for t in writes:
            t.w = o
            t.r = {}
            t.rd = []
        if o.kind == "c":
            self.last[eng] = o
        self.ops.append(o)
        return o

    def barrier(self):
        deps = [o for o in self.last.values() if o is not None]
        deps += list(self.last_dma.values())
        for e in ENGS:
            self.extra[e] = list(deps)

    def finalize(self):
        cnt = {e: 0 for e in ENGS}
        for o in self.ops:
            if o.kind == "c" and o.signal:
                cnt[o.eng] += 1
                o.sigval = cnt[o.eng]

    def emit(self, nc, block, engsem):
        handles = {"pe": "tensor", "act": "scalar", "dve": "vector", "pool": "gpsimd", "sp": "sync"}
        final = [(c.sem, c.count) for c in self.chans if c.count > 0]

        def run(engname):
            def body(e):
                waited = {}
                for o in self.ops:
                    if o.eng != engname:
                        continue
                    for d in o.deps:
                        if d.kind == "d":
                            sem, val = d.chan.sem, d.dmaval
                        else:
                            sem, val = engsem[d.eng], d.sigval
                        k = id(sem)
                        if waited.get(k, 0) >= val:
                            continue
                        e.wait_ge(sem, val)
                        waited[k] = val
                    ins = o.fn(e)
                    if o.kind == "d":
                        ins.then_inc(o.chan.sem, 16)
                    elif o.signal:
                        ins.then_inc(engsem[o.eng], 1)
                if engname == "sp":
                    for sem, val in final:
                        e.wait_ge(sem, val)
            return body

        for en in ENGS:
            getattr(block, handles[en])(run(en))


def build_program(layers, nseq, debug=False):
    nc = bass.Bass("TRN2", target_bir_lowering=False)
    Sd = Sched()

    def dram_in(name, shape):
        return nc.dram_tensor(name, list(shape), F32, kind="ExternalInput").ap()

    xT = dram_in("xT", [nseq, D, S])
    memT = dram_in("memT", [nseq, D, 256])
    w_mem_kv = dram_in("w_mem_kv", [D, 512])
    a_w_in = dram_in("a_w_in", [2, D, 2560])
    a_w_out = dram_in("a_w_out", [2, D, D])
    b_w_in = dram_in("b_w_in", [1, D, 2560])
    b_w_out = dram_in("b_w_out", [1, 512, D])
    c_w_in = dram_in("c_w_in", [1, D, D])
    c_w_out = dram_in("c_w_out", [1, D, D])
    pwfull = dram_in("pwfull", [MIXW, MIXW])
    ffn_w_gate = dram_in("ffn_w_gate", [2, D, DFF])
    ffn_w_up = dram_in("ffn_w_up", [2, D, DFF])
    ffn_w_down = dram_in("ffn_w_down", [2, DFF, D])
    moe_router = dram_in("moe_router", [2, D, NE])
    moe_w_gate = dram_in("moe_w_gate", [2, NE, D, DFFE])
    moe_w_up = dram_in("moe_w_up", [2, NE, D, DFFE])
    moe_w_down = dram_in("moe_w_down", [2, NE, DFFE, D])
    c_lnp = dram_in("c_lnp", [128, 128])
    c_convw = dram_in("c_convw", [128, 36])
    c_pscale = dram_in("c_pscale", [128, 6])
    c_ident = dram_in("c_ident", [128, 128])
    c_expb = dram_in("c_expb", [128, 12 * 256])
    c_edge = dram_in("c_edge", [128, 64])
    c_lstrict = dram_in("c_lstrict", [128, 128])
    c_iota = dram_in("c_iota", [128, CAP])
    c_slotid = dram_in("c_slotid", [128, 8])
    yT = nc.dram_tensor("yT", [nseq, D, S], F32, kind="ExternalOutput").ap()
    coef_scr = nc.dram_tensor("coef_scr", [NE, S], F32, kind="Internal").ap()
    pos_scr = nc.dram_tensor("pos_scr", [NE, S], F32, kind="Internal").ap()
    xd_scr = nc.dram_tensor("xd_scr", [NE, D, CAP], BF16, kind="Internal").ap()
    dbg_out = None
    if debug:
        dbg_out = nc.dram_tensor("dbg", [len(layers), nseq, D, S], F32, kind="ExternalOutput").ap()

    es = contextlib.ExitStack()
    with es:
        def sb(name, shape, dt):
            return es.enter_context(nc.sbuf_tensor(name, list(shape), dt))

        xacc = sb("xacc", [128, 8, S], F32)
        xb = sb("xb", [128, 8, S], BF16)
        lnp = sb("lnp", [128, 256], F32)
        convw = sb("convw", [128, 36], F32)
        pscale = sb("pscale", [128, 6], F32)
        router = sb("router", [128, 2, 8, NE], F32)
        ident = sb("ident", [128, 128], F32)
        ones32 = sb("ones32", [128, 128], F32)
        onespad = sb("onespad", [128, 2, 128], BF16)
        kTpad = sb("kTpad", [128, 4, 256], BF16)
        vpad = sb("vpad", [128, 4, 2, 128], BF16)
        expb = sb("expb", [128, 12, 256], BF16)
        edge = sb("edge", [128, 4, 16], F32)
        slotid = sb("slotid", [128, 8], F32)
        identb = sb("identb", [128, 128], BF16)
        arena = sb("arena", [128, ARENA_BYTES // 2], BF16)
        ps = es.enter_context(nc.psum_tensor("ps", [128, 8, 512], F32))

        engsem = {e: es.enter_context(nc.semaphore("s_" + e)) for e in ENGS}

        XA = [Tk("xa%d" % t) for t in range(NT)]
        XB = [Tk("xb%d" % t) for t in range(NT)]
        PB = [Tk("ps%d" % b) for b in range(8)]
        CONST = Tk("const")
        KV = Tk("kv")
        ch_const = Sd.chan("const")
        ch_x = [Sd.chan("x%d" % t) for t in range(NT)]
        ch_out = [Sd.chan("o%d" % t) for t in range(NT)]
        ch_w = [Sd.chan("w%d" % i) for i in range(9)]
        ch_misc = [Sd.chan("m%d" % i) for i in range(4)]
        ch_coef = Sd.chan("coef")
        ch_cb = [Sd.chan("cb%d" % i) for i in range(2)]
        ch_xd = [Sd.chan("xd%d" % i) for i in range(2)]
        ch_xdl = [Sd.chan("xdl%d" % i) for i in range(2)]
        ch_pos = Sd.chan("pos")

        def tsl(tt):
            return slice(tt * TS, (tt + 1) * TS)

        class Carver:
            def __init__(self, base=None, nbytes=ARENA_BYTES):
                self.off = 0
                self.base = arena if base is None else base
                self.nbytes = nbytes

            def take(self, shape, dt):
                esz = 4 if dt == F32 else 2
                n = 1
                for s_ in shape[1:]:
                    n *= s_
                nbytes = n * esz
                assert self.off % 4 == 0
                assert self.off + nbytes <= self.nbytes, (self.off, nbytes)
                v = self.base[:, self.off // 2:(self.off + nbytes) // 2]
                if dt == F32:
                    v = v.bitcast(F32)
                self.off += nbytes
                if shape[0] != 128:
                    v = v[0:shape[0]]
                if len(shape) == 3:
                    v = v.rearrange("p (a b) -> p a b", a=shape[1])
                elif len(shape) == 4:
                    v = v.rearrange("p (a b c) -> p a b c", a=shape[1], b=shape[2])
                return v

        def dma(eng, out, in_, chan, reads=(), writes=()):
            Sd.op(eng, lambda e, o=out, i=in_: e.dma_start(out=o, in_=i), reads, writes, chan=chan)

        def mm(out, pairs, reads, writes):
            def fn(e, out=out, pairs=pairs):
                n = len(pairs)
                ins = None
                for i, (l, r) in enumerate(pairs):
                    ins = e.matmul(out, l, r, start=(i == 0), stop=(i == n - 1))
                return ins
            Sd.op("pe", fn, reads, writes)

        def act(out, in_, func, reads, writes, bias=None, scale=None):
            kw = {}
            if bias is not None:
                kw["bias"] = bias
            if scale is not None:
                kw["scale"] = scale
            Sd.op("act", lambda e, o=out, i=in_, f=func, kw=kw: e.activation(o, i, f, **kw), reads, writes)

        def tt_(eng, out, in0, in1, op, reads, writes):
            Sd.op(eng, lambda e, o=out, a=in0, b=in1, op=op: e.tensor_tensor(o, a, b, op), reads, writes)

        def ts_(eng, out, in0, s1, s2, op0, op1, reads, writes):
            if op1 is None:
                Sd.op(eng, lambda e, o=out, a=in0, s1=s1, op0=op0: e.tensor_scalar(o, a, s1, None, op0), reads, writes)
            else:
                Sd.op(eng, lambda e, o=out, a=in0, s1=s1, s2=s2, op0=op0, op1=op1:
                      e.tensor_scalar(o, a, s1, s2, op0, op1), reads, writes)

        def stt(eng, out, in0, scalar, in1, op0, op1, reads, writes):
            Sd.op(eng, lambda e, o=out, a=in0, s=scalar, b=in1, op0=op0, op1=op1:
                  e.scalar_tensor_tensor(o, a, s, b, op0, op1), reads, writes)

        def cp(eng, out, in_, reads, writes):
            Sd.op(eng, lambda e, o=out, i=in_: e.tensor_copy(o, i), reads, writes)

        def mset(eng, ap, val, writes):
            Sd.op(eng, lambda e, a=ap, v=val: e.memset(a, v), (), writes)

        dma("sp", lnp[:, 0:128], c_lnp, ch_const, (), (CONST,))
        dma("sp", convw[:], c_convw, ch_const, (), (CONST,))
        dma("sp", pscale[:], c_pscale, ch_const, (), (CONST,))
        dma("sp", ident[:], c_ident, ch_const, (), (CONST,))
        dma("pool", expb[:].rearrange("p a b -> p (a b)"), c_expb, ch_const, (), (CONST,))
        dma("sp", edge[:].rearrange("p a b -> p (a b)"), c_edge, ch_const, (), (CONST,))
        for f in range(2):
            dma("sp", router[:, f], moe_router[f].rearrange("(c p) e -> p c e", p=128), ch_const, (), (CONST,))
        dma("sp", slotid[:], c_slotid, ch_const, (), (CONST,))
        Sd.op("act", lambda e: e.mul(lnp[:, 128:256], lnp[:, 0:128], ALPHA), (CONST,), (CONST,))
        cp("dve", identb[:], ident[:], (CONST,), (CONST,))
        mset("dve", ones32[:], 1.0, (CONST,))
        mset("dve", onespad[:], 0.0, (CONST,))
        mset("dve", onespad[:, 0, 0:64], 1.0, (CONST,))
        mset("dve", onespad[:, 1, 64:128], 1.0, (CONST,))
        mset("dve", kTpad[:], 0.0, (KV,))
        mset("dve", vpad[:], 0.0, (KV,))

        def lncol(l, j, c):
            return (l * 2 + j) * 8 + c

        def layer_norm(tt, l, j, sq, tmp, SQ, TMP, final, bank_a, bank_b):
            v = xacc[:, :, tsl(tt)]
            act(sq, v, AF.Square, (XA[tt],), (SQ,))
            mm(ps[:, bank_a, :], [(ones32[:], xacc[:, c, tsl(tt)]) for c in range(8)],
               (XA[tt], CONST), (PB[bank_a],))
            mm(ps[:, bank_b, :], [(ones32[:], sq[:, c, :]) for c in range(8)], (SQ, CONST), (PB[bank_b],))
            mean, msq, var, rstd = tmp[:, 0], tmp[:, 1], tmp[:, 2], tmp[:, 3]
            Sd.op("act", lambda e: e.mul(mean, ps[:, bank_a, :], 1.0 / D), (PB[bank_a],), (TMP[0],))
            tt_("dve", msq, mean, mean, ALU.mult, (TMP[0],), (TMP[1],))
            stt("dve", var, ps[:, bank_b, :], 1.0 / D, msq, ALU.mult, ALU.subtract, (PB[bank_b], TMP[1]), (TMP[2],))
            ts_("dve", var, var, LN_EPS, None, ALU.add, None, (TMP[2],), (TMP[2],))
            act(var, var, AF.Sqrt, (TMP[2],), (TMP[2],))
            Sd.op("dve", lambda e: e.reciprocal(rstd, var), (TMP[2],), (TMP[3],))
            mb = mean.unsqueeze(1).to_broadcast([128, 8, TS])
            rb = rstd.unsqueeze(1).to_broadcast([128, 8, TS])
            tt_("dve", sq, v, mb, ALU.subtract, (XA[tt], TMP[0]), (SQ,))
            tt_("pool", sq, sq, rb, ALU.mult, (SQ, TMP[3]), (SQ,))
            for c in range(8):
                col = lncol(l, j, c)
                g, b_ = lnp[:, col:col + 1], lnp[:, 64 + col:64 + col + 1]
                ga, ba = lnp[:, 128 + col:128 + col + 1], lnp[:, 192 + col:192 + col + 1]
                if final:
                    act(xacc[:, c, tsl(tt)], sq[:, c, :], AF.Identity, (SQ, CONST), (XA[tt],), bias=b_, scale=g)
                else:
                    act(xb[:, c, tsl(tt)], sq[:, c, :], AF.Identity, (SQ, CONST), (XB[tt],), bias=b_, scale=g)
                    act(xacc[:, c, tsl(tt)], sq[:, c, :], AF.Identity, (SQ, CONST), (XA[tt],), bias=ba, scale=ga)

        def load_w_cols(slot, slot_tk, chan, src2d, colgroups):
            off = 0
            for (c0, n) in colgroups:
                dma("pool", slot[:, :, off:off + n], src2d[:, c0:c0 + n].rearrange("(c p) e -> p c e", p=128),
                    chan, (), (slot_tk,))
                off += n

        def mem_attention(qm, QM, memout, MO, pT, PT, rden, RD):
            for tt in range(NT):
                for pr in range(2):
                    bn, bd = 4 + (pr % 2) * 2, 5 + (pr % 2) * 2
                    for hh in range(2):
                        h = 2 * pr + hh
                        b0 = (hh % 2) * 2
                        for mc in range(2):
                            mm(ps[:, b0 + mc, :], [(kTpad[:, h, mc * 128:(mc + 1) * 128], qm[:, pr, tsl(tt)])],
                               (KV, QM[tt]), (PB[b0 + mc],))
                        act(pT[:, hh], ps[:, b0:b0 + 2, :], AF.Exp, (PB[b0], PB[b0 + 1]), (PT[hh],))
                    mm(ps[:, bn, :], [(vpad[:, 2 * pr + hh, mc, :], pT[:, hh, mc, :]) for hh in range(2) for mc in range(2)],
                       (KV, PT[0], PT[1]), (PB[bn],))
                    mm(ps[:, bd, :], [(onespad[:, hh, :], pT[:, hh, mc, :]) for hh in range(2) for mc in range(2)],
                       (CONST, PT[0], PT[1]), (PB[bd],))
                    Sd.op("dve", lambda e, o=rden[:, pr % 2, :], i=ps[:, bd, :]: e.reciprocal(o, i), (PB[bd],), (RD[pr % 2],))
                    tt_("dve", memout[:, pr, tsl(tt)], ps[:, bn, :], rden[:, pr % 2, :], ALU.mult,
                        (PB[bn], RD[pr % 2]), (MO[tt],))

        def outproj_ln(l, wout2d, nmix, cat_aps, CAT, car):
            kc_n = len(cat_aps)
            wo = car.take([128, kc_n, D], BF16)
            WO = Tk("wo")
            dma("pool", wo, wout2d.rearrange("(c p) e -> p c e", p=128), ch_w[0], (), (WO,))
            sq = [car.take([128, 8, TS], F32) for _ in range(2)]
            tmp = [car.take([128, 4, TS], F32) for _ in range(2)]
            SQ = [Tk("sq0"), Tk("sq1")]
            TMP = [[Tk("tmp%d" % i) for i in range(4)] for _ in range(2)]
            for tt in range(NT):
                for dc in range(8):
                    bank = dc % 4
                    mm(ps[:, bank, :], [(wo[:, kc, dc * 128:(dc + 1) * 128], cat_aps[kc][:, tsl(tt)]) for kc in range(kc_n)],
                       (WO,) + tuple(CAT[tt]), (PB[bank],))
                    tt_("dve", xacc[:, dc, tsl(tt)], ps[:, bank, :], xacc[:, dc, tsl(tt)], ALU.add,
                        (PB[bank], XA[tt]), (XA[tt],))
                layer_norm(tt, l, 0, sq[tt % 2], tmp[tt % 2], SQ[tt % 2], TMP[tt % 2], False, 4 + (tt % 2) * 2, 5 + (tt % 2) * 2)

        def inproj_block(slot, SLOT, ncols, consumer):
            nch = ncols // 128
            k = 0
            for tt in range(NT):
                for ec in range(nch):
                    bank = k % 4
                    k += 1
                    mm(ps[:, bank, :], [(slot[:, kc, ec * 128:(ec + 1) * 128], xb[:, kc, tsl(tt)]) for kc in range(8)],
                       (SLOT, XB[tt]), (PB[bank],))
                    consumer(tt, ec, bank)

        def mixer_a(l, jA):
            car = Carver()
            mix = car.take([128, 6, S], BF16)
            memout = car.take([128, 2, S], BF16)
            qm = car.take([128, 2, S], BF16)
            slots = [car.take([128, 8, 384], BF16) for _ in range(2)]
            SL = [Tk("sl0"), Tk("sl1")]
            gcs = car.take([128, 2, TS], F32)
            GCS = [Tk("gcs0"), Tk("gcs1")]
            z = car.take([128, S + 2], F32)
            Z = Tk("z")
            gb = car.take([128, S], BF16)
            GB = Tk("gb")
            tmpc = car.take([128, 2, S], F32)
            TC = [Tk("tc0"), Tk("tc1")]
            pT = car.take([128, 2, 2, TS], BF16)
            PT = [Tk("pt0"), Tk("pt1")]
            rden = car.take([128, 2, TS], F32)
            RD = [Tk("rd0"), Tk("rd1")]
            MIX = [Tk("mix%d" % t) for t in range(NT)]
            MO = [Tk("mo%d" % t) for t in range(NT)]
            QM = [Tk("qm%d" % t) for t in range(NT)]
            w2d = a_w_in[jA]
            mset("pool", z[:, 0:1], 0.0, (Z,))
            mset("pool", z[:, S + 1:S + 2], 0.0, (Z,))
            for c in range(7):
                si = c % 2
                if c < 6:
                    load_w_cols(slots[si], SL[si], ch_w[1 + si], w2d,
                                [(c * 128, 128), (768 + c * 128, 128), (1536 + c * 128, 128)])
                else:
                    load_w_cols(slots[si], SL[si], ch_w[1 + si], w2d, [(2304, 256)])
                if c < 6:
                    def consumer(tt, ec, bank, c=c):
                        if ec == 0:
                            act(gb[:, tsl(tt)], ps[:, bank, :], AF.Copy, (PB[bank],), (GB,))
                        elif ec == 1:
                            act(gcs[:, tt % 2, :], ps[:, bank, :], AF.Copy, (PB[bank],), (GCS[tt % 2],))
                        else:
                            tt_("dve", z[:, 1 + tt * TS:1 + (tt + 1) * TS], ps[:, bank, :], gcs[:, tt % 2, :], ALU.mult,
                                (PB[bank], GCS[tt % 2]), (Z,))
                    inproj_block(slots[si], SL[si], 384, consumer)
                    wc = lambda k, c=c: convw[:, (jA * 3 + k) * 6 + c:(jA * 3 + k) * 6 + c + 1]
                    act(tmpc[:, 0, :], z[:, 0:S], AF.Identity, (Z, CONST), (TC[0],), scale=wc(0))
                    stt("dve", tmpc[:, 1, :], z[:, 1:S + 1], wc(1), tmpc[:, 0, :], ALU.mult, ALU.add,
                        (Z, CONST, TC[0]), (TC[1],))
                    stt("dve", tmpc[:, 0, :], z[:, 2:S + 2], wc(2), tmpc[:, 1, :], ALU.mult, ALU.add,
                        (Z, CONST, TC[1]), (TC[0],))
                    tt_("pool", mix[:, c, :], tmpc[:, 0, :], gb[:], ALU.mult, (TC[0], GB), tuple(MIX))
                else:
                    def consumer(tt, ec, bank):
                        Sd.op("act", lambda e, o=qm[:, ec, tsl(tt)], i=ps[:, bank, :]: e.mul(o, i, 0.125),
                              (PB[bank],), (QM[tt],))
                    inproj_block(slots[si], SL[si], 256, consumer)
            mem_attention(qm, QM, memout, MO, pT, PT, rden, RD)
            Sd.barrier()
            car2 = Carver()
            car2.take([128, 8, S], BF16)
            cat = [mix[:, c, :] for c in range(6)] + [memout[:, c, :] for c in range(2)]
            CAT = [(MIX[t], MO[t]) for t in range(NT)]
            outproj_ln(l, a_w_out[jA], 6, cat, CAT, car2)
            Sd.barrier()

        def mixer_c(l, jC):
            car = Carver()
            mix = car.take([128, 6, S], BF16)
            memout = car.take([128, 2, S], BF16)
            qm = car.take([128, 2, S], BF16)
            slots = [car.take([128, 8, 256], BF16) for _ in range(2)]
            SL = [Tk("sl0"), Tk("sl1")]
            pw = car.take([128, 6, MIXW], BF16)
            PW = Tk("pw")
            PADW = 16
            bufs = [car.take([128, S + 2 * PADW], F32) for _ in range(3)]
            BU = [Tk("bu%d" % i) for i in range(3)]
            pT = car.take([128, 2, 2, TS], BF16)
            PT = [Tk("pt0"), Tk("pt1")]
            rden = car.take([128, 2, TS], F32)
            RD = [Tk("rd0"), Tk("rd1")]
            etmp = car.take([128, 16], F32)
            ET = Tk("et")
            MIX = [Tk("mix%d" % t) for t in range(NT)]
            MO = [Tk("mo%d" % t) for t in range(NT)]
            QM = [Tk("qm%d" % t) for t in range(NT)]
            w2d = c_w_in[jC]
            dma("pool", pw, pwfull.rearrange("(c p) e -> p c e", p=128), ch_w[3], (), (PW,))
            for i in range(3):
                mset("pool", bufs[i][:, 0:PADW], 0.0, (BU[i],))
                mset("pool", bufs[i][:, S + PADW:S + 2 * PADW], 0.0, (BU[i],))
            ub, pa, pb = bufs

            def window(prs, wi):
                w = POOLW[wi]
                P = PADW
                lo, hi = -15, S + 15
                tt_("dve", pa[prs, P + lo:P + hi], ub[prs, P + lo - 1:P + hi - 1], ub[prs, P + lo:P + hi], ALU.add,
                    (BU[0],), (BU[1],))
                cur, other, curi, othi = pa, pb, 1, 2
                step = 1
                lo_c, hi_c = lo, hi
                ww = 2
                while ww < w:
                    lo_n, hi_n = lo_c + step, hi_c - step
                    tt_("dve", other[prs, P + lo_n:P + hi_n], cur[prs, P + lo_n - step:P + hi_n - step],
                        cur[prs, P + lo_n + step:P + hi_n + step], ALU.add, (BU[curi],), (BU[othi],))
                    cur, other, curi, othi = other, cur, othi, curi
                    lo_c, hi_c = lo_n, hi_n
                    step *= 2
                    ww *= 2
                return cur, curi

            def pool_half(c, prs, wi):
                cur, curi = window(prs, wi)
                w = POOLW[wi]
                P = PADW
                tt_("pool", cur[prs, P:P + 8], cur[prs, P:P + 8], edge[prs, wi, 0:8], ALU.mult, (BU[curi], CONST), (BU[curi],))
                tt_("pool", cur[prs, P + S - 8:P + S], cur[prs, P + S - 8:P + S], edge[prs, wi, 8:16], ALU.mult,
                    (BU[curi], CONST), (BU[curi],))
                stt("dve", mix[prs, c, :], cur[prs, P:P + S], 1.0 / w, ub[prs, P:P + S], ALU.mult, ALU.subtract,
                    (BU[curi], BU[0]), tuple(MIX))

            for c in range(4):
                si = c % 2
                if c < 3:
                    load_w_cols(slots[si], SL[si], ch_w[1 + si], w2d, [(c * 256, 256)])
                    for ecl in range(2):
                        cc = c * 2 + ecl
                        for tt in range(NT):
                            bank = tt % 4
                            mm(ps[:, bank, :], [(slots[si][:, kc, ecl * 128:(ecl + 1) * 128], xb[:, kc, tsl(tt)]) for kc in range(8)],
                               (SL[si], XB[tt]), (PB[bank],))
                            act(ub[:, PADW + tt * TS:PADW + (tt + 1) * TS], ps[:, bank, :], AF.Copy, (PB[bank],), (BU[0],))
                        g0 = (cc * 128) // 192
                        g1 = (cc * 128 + 64) // 192
                        if g0 == g1:
                            pool_half(cc, slice(0, 128), g0)
                        else:
                            pool_half(cc, slice(0, 64), g0)
                            pool_half(cc, slice(64, 128), g1)
                else:
                    load_w_cols(slots[si], SL[si], ch_w[1 + si], w2d, [(768, 256)])

                    def consumer(tt, ec, bank):
                        Sd.op("act", lambda e, o=qm[:, ec, tsl(tt)], i=ps[:, bank, :]: e.mul(o, i, 0.125),
                              (PB[bank],), (QM[tt],))
                    inproj_block(slots[si], SL[si], 256, consumer)
            for tt in range(NT):
                for dc in range(6):
                    mm(ps[:, dc, :], [(pw[:, kc, dc * 128:(dc + 1) * 128], mix[:, kc, tsl(tt)]) for kc in range(6)],
                       (PW, MIX[tt]), (PB[dc],))
                for dc in range(6):
                    act(mix[:, dc, tsl(tt)], ps[:, dc, :], AF.Identity, (PB[dc], CONST), (MIX[tt],), scale=pscale[:, dc:dc + 1])
            mem_attention(qm, QM, memout, MO, pT, PT, rden, RD)
            Sd.barrier()
            car2 = Carver()
            car2.take([128, 8, S], BF16)
            cat = [mix[:, c, :] for c in range(6)] + [memout[:, c, :] for c in range(2)]
            CAT = [(MIX[t], MO[t]) for t in range(NT)]
            outproj_ln(l, c_w_out[jC], 6, cat, CAT, car2)
            Sd.barrier()

        def mixer_b(l, jB):
            car = Carver()
            mix = car.take([128, 2, S], BF16)
            memout = car.take([128, 2, S], BF16)
            qm = car.take([128, 2, S], BF16)
            slots = [car.take([128, 8, 384], BF16) for _ in range(2)]
            SL = [Tk("sl0"), Tk("sl1")]
            qs = car.take([128, S], BF16)
            QS = Tk("qs")
            kEO = car.take([128, 2, S], BF16)
            KEO = Tk("keo")
            vEO = car.take([128, 2, 16, 128], BF16)
            VEO = Tk("veo")
            nacc = car.take([128, S], F32)
            dacc = car.take([128, S], F32)
            NA, DA = Tk("na"), Tk("da")
            ebuf = car.take([128, 2, 2, 256], F32)
            EB = [Tk("eb0"), Tk("eb1")]
            pTd = car.take([128, 2, 2, 256], BF16)
            PTD = [Tk("ptd0"), Tk("ptd1")]
            pT = car.take([128, 2, 2, TS], BF16)
            PT = [Tk("pt0"), Tk("pt1")]
            rden = car.take([128, 2, TS], F32)
            RD = [Tk("rd0"), Tk("rd1")]
            MIX = [Tk("mix%d" % t) for t in range(NT)]
            MO = [Tk("mo%d" % t) for t in range(NT)]
            QM = [Tk("qm%d" % t) for t in range(NT)]
            w2d = b_w_in[jB]
            mset("pool", kEO[:], 0.0, (KEO,))
            mset("pool", vEO[:], 0.0, (VEO,))
            it = 0
            for pr in range(2):
                mset("pool", nacc[:], 0.0, (NA,))
                mset("pool", dacc[:], 0.0, (DA,))
                for g, (win, dil) in enumerate(DIL):
                    ci = g * 2 + pr
                    si = it % 2
                    it += 1
                    load_w_cols(slots[si], SL[si], ch_w[1 + si], w2d,
                                [(ci * 128, 128), (768 + ci * 128, 128), (1536 + ci * 128, 128)])
                    slot = slots[si]
                    for tt in range(NT):
                        b0 = (tt % 2) * 2
                        mm(ps[:, b0, :], [(slot[:, kc, 0:128], xb[:, kc, tsl(tt)]) for kc in range(8)],
                           (SL[si], XB[tt]), (PB[b0],))
                        Sd.op("act", lambda e, o=qs[:, tsl(tt)], i=ps[:, b0, :]: e.mul(o, i, 0.125), (PB[b0],), (QS,))
                        mm(ps[:, b0 + 1, :], [(slot[:, kc, 128:256], xb[:, kc, tsl(tt)]) for kc in range(8)],
                           (SL[si], XB[tt]), (PB[b0 + 1],))
                        act(kEO[0:64, 0, tsl(tt)], ps[0:64, b0 + 1, :], AF.Copy, (PB[b0 + 1],), (KEO,))
                        cp("dve", kEO[64:128, 1, tsl(tt)], ps[64:128, b0 + 1, :], (PB[b0 + 1],), (KEO,))
                    n_sub = S // dil
                    nkb = n_sub // 128
                    ti = 0
                    for r in range(dil):
                        for kb in range(nkb):
                            bank = 4 + (ti // 4) % 2
                            sub = (ti % 4) * 128
                            start = kb * 128 * dil + r
                            toks = slice(start, start + 127 * dil + 1, dil)
                            mm(ps[:, bank, sub:sub + 128], [(xb[:, kc, toks], slot[:, kc, 256:384]) for kc in range(8)],
                               (SL[si],) + tuple(XB), (PB[bank],))
                            act(vEO[:, 0, ti, 0:64], ps[:, bank, sub:sub + 64], AF.Copy, (PB[bank],), (VEO,))
                            cp("dve", vEO[:, 1, ti, 64:128], ps[:, bank, sub + 64:sub + 128], (PB[bank],), (VEO,))
                            ti += 1
                    ti = 0
                    for r in range(dil):
                        for kb in range(nkb):
                            j0 = max(0, kb * 128 - 64)
                            j1 = min(n_sub, kb * 128 + 192)
                            nq = j1 - j0
                            col0 = j0 - (kb * 128 - 64)
                            kst = kb * 128 * dil + r
                            ktoks = slice(kst, kst + 127 * dil + 1, dil)
                            qst = j0 * dil + r
                            qtoks = slice(qst, qst + (nq - 1) * dil + 1, dil)
                            bp = ti % 2
                            for hh in range(2):
                                mm(ps[:, 2 * bp + hh, 0:nq], [(kEO[:, hh, ktoks], qs[:, qtoks])], (KEO, QS), (PB[2 * bp + hh],))
                            act(ebuf[:, bp, :, 0:nq], ps[:, 2 * bp:2 * bp + 2, 0:nq], AF.Exp, (PB[2 * bp], PB[2 * bp + 1]), (EB[bp],))
                            tt_("pool", pTd[:, bp, :, 0:nq], ebuf[:, bp, :, 0:nq],
                                expb[:, g * 4 + 2 * pr:g * 4 + 2 * pr + 2, col0:col0 + nq], ALU.mult,
                                (EB[bp], CONST), (PTD[bp],))
                            bn, bd = 4 + bp, 6 + bp
                            mm(ps[:, bn, 0:nq], [(vEO[:, hh, ti, :], pTd[:, bp, hh, 0:nq]) for hh in range(2)],
                               (VEO, PTD[bp]), (PB[bn],))
                            mm(ps[:, bd, 0:nq], [(onespad[:, hh, :], pTd[:, bp, hh, 0:nq]) for hh in range(2)],
                               (CONST, PTD[bp]), (PB[bd],))
                            tt_("dve", nacc[:, qtoks], ps[:, bn, 0:nq], nacc[:, qtoks], ALU.add, (PB[bn], NA), (NA,))
                            tt_("dve", dacc[:, qtoks], ps[:, bd, 0:nq], dacc[:, qtoks], ALU.add, (PB[bd], DA), (DA,))
                            ti += 1
                Sd.op("dve", lambda e: e.reciprocal(dacc[:], dacc[:]), (DA,), (DA,))
                tt_("pool", mix[:, pr, :], nacc[:], dacc[:], ALU.mult, (NA, DA), tuple(MIX))
            si = it % 2
            load_w_cols(slots[si], SL[si], ch_w[1 + si], w2d, [(2304, 256)])

            def consumer(tt, ec, bank):
                Sd.op("act", lambda e, o=qm[:, ec, tsl(tt)], i=ps[:, bank, :]: e.mul(o, i, 0.125),
                      (PB[bank],), (QM[tt],))
            inproj_block(slots[si][:, :, 0:256], SL[si], 256, consumer)
            mem_attention(qm, QM, memout, MO, pT, PT, rden, RD)
            Sd.barrier()
            car2 = Carver()
            car2.take([128, 4, S], BF16)
            cat = [mix[:, c, :] for c in range(2)] + [memout[:, c, :] for c in range(2)]
            CAT = [(MIX[t], MO[t]) for t in range(NT)]
            outproj_ln(l, b_w_out[jB], 2, cat, CAT, car2)
            Sd.barrier()

        class FFNState:
            pass

        def ffn_setup():
            st = FFNState()
            car = Carver()
            st.wg = [car.take([128, 8, 512], BF16) for _ in range(2)]
            st.wu = [car.take([128, 8, 512], BF16) for _ in range(2)]
            st.wd = [car.take([128, 4, D], BF16) for _ in range(2)]
            st.WS = [Tk("ws0"), Tk("ws1")]
            st.h = car.take([128, 2, 4, TS], BF16)
            st.H = [Tk("h0"), Tk("h1")]
            st.sg = car.take([128, 2, TS], F32)
            st.SG = [Tk("sg0"), Tk("sg1")]
            st.sgc = car.take([128, 2, TS], F32)
            st.SGC = [Tk("sgc0"), Tk("sgc1")]
            st.cb = car.take([128, 2, S], F32)
            st.CB = [Tk("cb0"), Tk("cb1")]
            st.blk = 0
            st.car = car
            return st

        def ffn_pass(st, wg2d, wu2d, wd2d, dff, coef_slot):
            f0 = 0
            while f0 < dff:
                fb = min(512, dff - f0)
                nfc = fb // 128
                si = st.blk % 2
                st.blk += 1
                ch = ch_w[si * 3:(si * 3) + 3]
                dma("pool", st.wg[si][:, :, 0:fb], wg2d[:, f0:f0 + fb].rearrange("(c p) e -> p c e", p=128), ch[0], (), (st.WS[si],))
                dma("pool", st.wu[si][:, :, 0:fb], wu2d[:, f0:f0 + fb].rearrange("(c p) e -> p c e", p=128), ch[1], (), (st.WS[si],))
                dma("pool", st.wd[si][:, 0:nfc, :], wd2d[f0:f0 + fb, :].rearrange("(c p) e -> p c e", p=128), ch[2], (), (st.WS[si],))
                WSt = st.WS[si]

                def gu(tt, fc, si=si, WSt=WSt):
                    bg, bu = (fc % 2) * 2, (fc % 2) * 2 + 1
                    hp = tt % 2
                    mm(ps[:, bg, :], [(st.wg[si][:, kc, fc * 128:(fc + 1) * 128], xb[:, kc, tsl(tt)]) for kc in range(8)],
                       (WSt, XB[tt]), (PB[bg],))
                    mm(ps[:, bu, :], [(st.wu[si][:, kc, fc * 128:(fc + 1) * 128], xb[:, kc, tsl(tt)]) for kc in range(8)],
                       (WSt, XB[tt]), (PB[bu],))
                    act(st.sg[:, fc % 2, :], ps[:, bg, :], AF.Silu, (PB[bg],), (st.SG[fc % 2],))
                    if coef_slot is None:
                        tt_("dve", st.h[:, hp, fc, :], ps[:, bu, :], st.sg[:, fc % 2, :], ALU.mult,
                            (PB[bu], st.SG[fc % 2]), (st.H[hp],))
                    else:
                        tt_("pool", st.sgc[:, fc % 2, :], st.sg[:, fc % 2, :], st.cb[:, coef_slot, tsl(tt)], ALU.mult,
                            (st.SG[fc % 2], st.CB[coef_slot]), (st.SGC[fc % 2],))
                        tt_("dve", st.h[:, hp, fc, :], ps[:, bu, :], st.sgc[:, fc % 2, :], ALU.mult,
                            (PB[bu], st.SGC[fc % 2]), (st.H[hp],))

                def down(tt, half, si=si, WSt=WSt, nfc=nfc):
                    hp = tt % 2
                    for dl in range(4):
                        dc = half * 4 + dl
                        mm(ps[:, 4 + dl, :], [(st.wd[si][:, fc, dc * 128:(dc + 1) * 128], st.h[:, hp, fc, :]) for fc in range(nfc)],
                           (WSt, st.H[hp]), (PB[4 + dl],))
                    Sd.op("dve", lambda e, o=xacc[:, half * 4:half * 4 + 4, tsl(tt)], a=ps[:, 4:8, :]:
                          e.tensor_tensor(o, a, o, ALU.add), (PB[4], PB[5], PB[6], PB[7], XA[tt]), (XA[tt],))

                for tt in range(NT + 1):
                    for fc in range(nfc):
                        if tt < NT:
                            gu(tt, fc)
                        if tt > 0 and fc == (nfc // 2) - 1:
                            down(tt - 1, 0)
                    if tt > 0:
                        down(tt - 1, 1)
                f0 += fb

        def dense_ffn(l, f):
            st = ffn_setup()
            ffn_pass(st, ffn_w_gate[f], ffn_w_up[f], ffn_w_down[f], DFF, None)
            Sd.barrier()

        def moe_ffn(l, f):
            sparse = (MOE_MODE == "cap")
            car = Carver()
            posm = car.take([128, 16, NE], F32)
            lg = car.take([128, 16, NE], F32)
            m1 = car.take([128, 16], F32)
            m2 = car.take([128, 16], F32)
            mk1 = car.take([128, 16, NE], F32)
            mk2 = car.take([128, 16, NE], F32)
            l2 = car.take([128, 16, NE], F32)
            w1 = car.take([128, 16], F32)
            w2 = car.take([128, 16], F32)
            coef = car.take([128, 16, NE], F32)
            coefT = car.take([NE, S], F32)
            R = Tk("route")
            fl = lambda a: a.rearrange("p a b -> p (a b)")
            for tc_ in range(16):
                tt = tc_ // 4
                mm(ps[:, 0, tc_ * NE:(tc_ + 1) * NE],
                   [(xacc[:, kc, tc_ * 128:(tc_ + 1) * 128], router[:, f, kc, :]) for kc in range(8)],
                   (XA[tt], CONST), (PB[0],))
            Sd.op("act", lambda e: e.mul(fl(lg), ps[:, 0, 0:16 * NE], 1.0 / ALPHA), (PB[0],), (R,))
            Sd.op("dve", lambda e: e.tensor_reduce(m1[:], lg[:], AX.X, ALU.max), (R,), (R,))
            tt_("dve", mk1[:], lg[:], m1[:].unsqueeze(2).to_broadcast([128, 16, NE]), ALU.is_equal, (R,), (R,))
            stt("dve", l2[:], mk1[:], -1e30, lg[:], ALU.mult, ALU.add, (R,), (R,))
            Sd.op("dve", lambda e: e.tensor_reduce(m2[:], l2[:], AX.X, ALU.max), (R,), (R,))
            tt_("dve", mk2[:], l2[:], m2[:].unsqueeze(2).to_broadcast([128, 16, NE]), ALU.is_equal, (R,), (R,))
            if sparse:
                msk = car.take([128, 16, NE], F32)
                tot = car.take([128, 16, NE], F32)
                off = car.take([128, 16, NE], F32)
                pos = car.take([128, 16, NE], F32)
                posT = car.take([NE, S], F32)
                lst = car.take([128, 128], F32)
                dma("sp", lst, c_lstrict, ch_misc[2], (), (R,))
                tt_("dve", msk[:], mk1[:], mk2[:], ALU.add, (R,), (R,))
                mm(ps[:, 5, 0:128], [(lst, fl(msk))], (R,), (PB[5],))
                mm(ps[:, 6, 0:128], [(ones32[:], fl(msk))], (R, CONST), (PB[6],))
                Sd.op("act", lambda e: e.copy(fl(tot), ps[:, 6, 0:128]), (PB[6],), (R,))
                mset("dve", off[:, 0, :], 0.0, (R,))
                for tc_ in range(1, 16):
                    tt_("dve", off[:, tc_, :], off[:, tc_ - 1, :], tot[:, tc_ - 1, :], ALU.add, (R,), (R,))
                tt_("dve", fl(pos), ps[:, 5, 0:128], fl(off), ALU.add, (PB[5], R), (R,))
                stt("dve", posm[:], pos[:], 1.0, msk[:], ALU.add, ALU.mult, (R,), (R,))
                ts_("dve", posm[:], posm[:], -1.0, None, ALU.add, None, (R,), (R,))
                for tc_ in range(16):
                    bank = 4 + tc_ // 4
                    sub = (tc_ % 4) * 128
                    Sd.op("pe", lambda e, o=ps[0:NE, bank, sub:sub + 128], i=posm[:, tc_, :]: e.transpose(o, i, ident[:]),
                          (R, CONST), (PB[bank],))
                for q in range(4):
                    cp("dve", posT[:, q * 512:(q + 1) * 512], ps[0:NE, 4 + q, :], (PB[4 + q],), (R,))
                dma("sp", pos_scr, posT, ch_pos, (R,), (R,))
            tt_("dve", w2[:], m2[:], m1[:], ALU.subtract, (R,), (R,))
            act(w2[:], w2[:], AF.Exp, (R,), (R,))
            ts_("dve", w1[:], w2[:], 1.0, None, ALU.add, None, (R,), (R,))
            Sd.op("dve", lambda e: e.reciprocal(w1[:], w1[:]), (R,), (R,))
            ts_("dve", w2[:], w1[:], -1.0, 1.0, ALU.mult, ALU.add, (R,), (R,))
            tt_("dve", mk1[:], mk1[:], w1[:].unsqueeze(2).to_broadcast([128, 16, NE]), ALU.mult, (R,), (R,))
            tt_("dve", mk2[:], mk2[:], w2[:].unsqueeze(2).to_broadcast([128, 16, NE]), ALU.mult, (R,), (R,))
            tt_("dve", coef[:], mk1[:], mk2[:], ALU.add, (R,), (R,))
            for tc_ in range(16):
                bank = 1 + tc_ // 4
                sub = (tc_ % 4) * 128
                Sd.op("pe", lambda e, o=ps[0:NE, bank, sub:sub + 128], i=coef[:, tc_, :]: e.transpose(o, i, ident[:]),
                      (R, CONST), (PB[bank],))
            for q in range(4):
                Sd.op("act", lambda e, o=coefT[:, q * 512:(q + 1) * 512], i=ps[0:NE, 1 + q, :]: e.copy(o, i), (PB[1 + q],), (R,))
            dma("sp", coef_scr, coefT, ch_coef, (R,), (R,))
            Sd.barrier()
            if not sparse:
                st = ffn_setup()
                for ex in range(NE):
                    cs = ex % 2
                    dma("sp", st.cb[:, cs, :], coef_scr[ex:ex + 1, :].to_broadcast([128, S]), ch_cb[cs], (R,), (st.CB[cs],))
                    ffn_pass(st, moe_w_gate[f, ex], moe_w_up[f, ex], moe_w_down[f, ex], DFFE, cs)
                Sd.barrier()
                return
            carD = Carver()
            posm = carD.take([128, 16, NE], F32)
            xtm = carD.take([128, 16, D], BF16)
            Pb = [carD.take([128, 16, CAP], BF16) for _ in range(2)]
            xds = [carD.take([128, 8, CAP], BF16) for _ in range(2)]
            iot = carD.take([128, CAP], F32)
            XTM, IOT, XDSCR = Tk("xtm"), Tk("iot"), Tk("xdscr")
            PTK = [Tk("p0"), Tk("p1")]
            XDS = [Tk("xds0"), Tk("xds1")]
            dma("sp", iot, c_iota, ch_misc[3], (), (IOT,))
            for tc_ in range(16):
                bank = tc_ % 2
                psb = ps[:, bank, :].bitcast(BF16)
                for kc in range(8):
                    Sd.op("pe", lambda e, o=psb[:, kc * 128:(kc + 1) * 128], i=xb[:, kc, tc_ * 128:(tc_ + 1) * 128]:
                          e.transpose(o, i, identb[:]), (XB[tc_ // 4], CONST), (PB[bank],))
                if tc_ % 2 == 0:
                    act(xtm[:, tc_, :], psb, AF.Copy, (PB[bank],), (XTM,))
                else:
                    cp("dve", xtm[:, tc_, :], psb, (PB[bank],), (XTM,))
            for ex in range(NE):
                pi = ex % 2
                for tc_ in range(16):
                    ts_("dve", Pb[pi][:, tc_, :], iot, posm[:, tc_, ex:ex + 1], None, ALU.is_equal, None, (IOT, R), (PTK[pi],))
                for dc in range(8):
                    b0 = 2 + (dc % 3) * 2
                    for ti, (s0, sn) in enumerate(STILES):
                        mm(ps[:, b0 + ti, 0:sn],
                           [(xtm[:, tc_, dc * 128:(dc + 1) * 128], Pb[pi][:, tc_, s0:s0 + sn]) for tc_ in range(16)],
                           (XTM, PTK[pi]), (PB[b0 + ti],))
                    act(xds[pi][:, dc, 0:512], ps[:, b0, 0:512], AF.Copy, (PB[b0],), (XDS[pi],))
                    cp("dve", xds[pi][:, dc, 512:CAP], ps[:, b0 + 1, 0:CAP - 512], (PB[b0 + 1],), (XDS[pi],))
                dma("sp", xd_scr[ex].rearrange("(c p) s -> p c s", p=128), xds[pi], ch_xd[pi], (XDS[pi],), (XDSCR,))
            Sd.barrier()
            carE = Carver()
            xd = [carE.take([128, 8, CAP], BF16) for _ in range(2)]
            PT = carE.take([128, NSC, S], BF16)
            yd = carE.take([128, NSC, D], F32)
            ydb = carE.take([128, NSC, D], BF16)
            wg = [carE.take([128, 8, 256], BF16) for _ in range(2)]
            wu = [carE.take([128, 8, 256], BF16) for _ in range(2)]
            wd = [carE.take([128, 2, D], BF16) for _ in range(2)]
            carX = Carver(base=xb[:].rearrange("p a b -> p (a b)"), nbytes=32 * 1024)
            hT = carX.take([128, 2, 2, CAP], BF16)
            sg = carX.take([128, 2, CAP], F32)
            pcb = carX.take([128, S], F32)
            wg.append(carX.take([128, 8, 256], BF16))
            wu.append(carX.take([128, 8, 256], BF16))
            wd.append(carX.take([128, 2, D], BF16))
            XD = [Tk("xd0"), Tk("xd1")]
            WS = [Tk("ws0"), Tk("ws1"), Tk("ws2")]
            HT = [Tk("ht0"), Tk("ht1")]
            SG = [Tk("sg0"), Tk("sg1")]
            PTT, YD, YDB, PCB = Tk("pt"), Tk("yd"), Tk("ydb"), Tk("pcb")
            state = {"blk": 0, "r": 0}

            def down_part(prev, half):
                si, hp = prev
                for sc in range(NSC):
                    bank = 4 + state["r"] % 4
                    state["r"] += 1
                    mm(ps[:, bank, :],
                       [(hT[:, hp, fc, sc * 128:(sc + 1) * 128], wd[si][:, fc, half * 512:(half + 1) * 512]) for fc in range(2)],
                       (WS[si], HT[hp]), (PB[bank],))
                    ydv = yd[:, sc, half * 512:(half + 1) * 512]
                    tt_("dve", ydv, ps[:, bank, :], ydv, ALU.add, (PB[bank], YD), (YD,))

            for ex in range(NE):
                xi = ex % 2
                dma("sp", xd[xi], xd_scr[ex].rearrange("(c p) s -> p c s", p=128), ch_xdl[xi], (XDSCR,), (XD[xi],))
                dma("sp", pcb, pos_scr[ex:ex + 1, :].to_broadcast([128, S]), ch_cb[0], (R,), (PCB,))
                mset("pool", yd[:].rearrange("p a b -> p (a b)"), 0.0, (YD,))
                for sc in range(NSC):
                    ts_("dve", PT[:, sc, :], pcb, slotid[:, sc:sc + 1], None, ALU.is_equal, None, (PCB, CONST), (PTT,))
                dma("sp", pcb, coef_scr[ex:ex + 1, :].to_broadcast([128, S]), ch_cb[1], (R,), (PCB,))
                for sc in range(NSC):
                    tt_("pool", PT[:, sc, :], PT[:, sc, :], pcb, ALU.mult, (PCB, PTT), (PTT,))
                prev = None
                for b in range(DFFE // 256):
                    si = state["blk"] % 3
                    hp = state["blk"] % 2
                    state["blk"] += 1
                    f0 = b * 256
                    ch = ch_w[si * 3:si * 3 + 3]
                    dma("pool", wg[si], moe_w_gate[f, ex][:, f0:f0 + 256].rearrange("(c p) e -> p c e", p=128), ch[0], (), (WS[si],))
                    dma("pool", wu[si], moe_w_up[f, ex][:, f0:f0 + 256].rearrange("(c p) e -> p c e", p=128), ch[1], (), (WS[si],))
                    dma("pool", wd[si], moe_w_down[f, ex][f0:f0 + 256, :].rearrange("(c p) e -> p c e", p=128), ch[2], (), (WS[si],))
                    for fc in range(2):
                        for ti, (s0, sn) in enumerate(STILES):
                            mm(ps[:, ti, 0:sn],
                               [(wg[si][:, kc, fc * 128:(fc + 1) * 128], xd[xi][:, kc, s0:s0 + sn]) for kc in range(8)],
                               (WS[si], XD[xi]), (PB[ti],))
                            mm(ps[:, 2 + ti, 0:sn],
                               [(wu[si][:, kc, fc * 128:(fc + 1) * 128], xd[xi][:, kc, s0:s0 + sn]) for kc in range(8)],
                               (WS[si], XD[xi]), (PB[2 + ti],))
                        for ti, (s0, sn) in enumerate(STILES):
                            act(sg[:, fc, s0:s0 + sn], ps[:, ti, 0:sn], AF.Silu, (PB[ti],), (SG[fc],))
                        for ti, (s0, sn) in enumerate(STILES):
                            tt_("dve", hT[:, hp, fc, s0:s0 + sn], ps[:, 2 + ti, 0:sn], sg[:, fc, s0:s0 + sn], ALU.mult,
                                (PB[2 + ti], SG[fc]), (HT[hp],))
                        if prev is not None:
                            down_part(prev, fc)
                    prev = (si, hp)
                down_part(prev, 0)
                down_part(prev, 1)
                act(ydb[:].rearrange("p a b -> p (a b)"), yd[:].rearrange("p a b -> p (a b)"), AF.Copy, (YD,), (YDB,))
                for tt in range(NT):
                    for dc in range(8):
                        bank = 4 + state["r"] % 4
                        state["r"] += 1
                        mm(ps[:, bank, :], [(ydb[:, sc, dc * 128:(dc + 1) * 128], PT[:, sc, tsl(tt)]) for sc in range(NSC)],
                           (YDB, PTT), (PB[bank],))
                        tt_("dve", xacc[:, dc, tsl(tt)], ps[:, bank, :], xacc[:, dc, tsl(tt)], ALU.add,
                            (PB[bank], XA[tt]), (XA[tt],))
            Sd.barrier()

        def ln2(l, final):
            car = Carver()
            sq = [car.take([128, 8, TS], F32) for _ in range(2)]
            tmp = [car.take([128, 4, TS], F32) for _ in range(2)]
            SQ = [Tk("sq0"), Tk("sq1")]
            TMP = [[Tk("tmp%d" % i) for i in range(4)] for _ in range(2)]
            for tt in range(NT):
                layer_norm(tt, l, 1, sq[tt % 2], tmp[tt % 2], SQ[tt % 2], TMP[tt % 2], final, (tt % 4) * 2, (tt % 4) * 2 + 1)
            Sd.barrier()

        for s in range(nseq):
            for tt in range(NT):
                dma("sp", xacc[:, :, tsl(tt)], xT[s][:, tsl(tt)].rearrange("(c p) t -> p c t", p=128), ch_x[tt], (), (XA[tt],))
            for tt in range(NT):
                cp("dve", xb[:, :, tsl(tt)], xacc[:, :, tsl(tt)], (XA[tt],), (XB[tt],))
                Sd.op("act", lambda e, v=xacc[:, :, tsl(tt)]: e.mul(v, v, ALPHA), (XA[tt], XB[tt]), (XA[tt],))
            car = Carver()
            memb = car.take([128, 8, 256], BF16)
            wkv = car.take([128, 8, 512], BF16)
            MB, WK = Tk("memb"), Tk("wkv")
            dma("pool", memb, memT[s].rearrange("(c p) m -> p c m", p=128), ch_misc[0], (), (MB,))
            dma("pool", wkv, w_mem_kv.rearrange("(c p) e -> p c e", p=128), ch_misc[1], (), (WK,))
            for ec in range(2):
                mm(ps[:, ec, 0:256], [(wkv[:, kc, ec * 128:(ec + 1) * 128], memb[:, kc, :]) for kc in range(8)],
                   (MB, WK), (PB[ec],))
                act(kTpad[0:64, 2 * ec, :], ps[0:64, ec, 0:256], AF.Copy, (PB[ec],), (KV,))
                cp("dve", kTpad[64:128, 2 * ec + 1, :], ps[64:128, ec, 0:256], (PB[ec],), (KV,))
            for mc in range(2):
                mm(ps[:, 2 + mc, 0:256], [(memb[:, kc, mc * 128:(mc + 1) * 128], wkv[:, kc, 256:512]) for kc in range(8)],
                   (MB, WK), (PB[2 + mc],))
                for h in range(4):
                    o = vpad[:, h, mc, (h % 2) * 64:(h % 2) * 64 + 64]
                    i = ps[:, 2 + mc, h * 64:(h + 1) * 64]
                    if h % 2 == 0:
                        act(o, i, AF.Copy, (PB[2 + mc],), (KV,))
                    else:
                        cp("dve", o, i, (PB[2 + mc],), (KV,))
            Sd.barrier()
            for li, l in enumerate(layers):
                kind, j = l % 3, l // 3
                if kind == 0:
                    mixer_a(l, j)
                elif kind == 1:
                    mixer_b(l, j)
                else:
                    mixer_c(l, j)
                if l % 2 == 0:
                    dense_ffn(l, l // 2)
                else:
                    moe_ffn(l, l // 2)
                last = (li == len(layers) - 1)
                ln2(l, last)
                if debug and not last:
                    for tt in range(NT):
                        dma("sp", dbg_out[li, s][:, tsl(tt)].rearrange("(c p) t -> p c t", p=128), xacc[:, :, tsl(tt)],
                            ch_out[tt], (XA[tt],), ())
            for tt in range(NT):
                dma("sp", yT[s][:, tsl(tt)].rearrange("(c p) t -> p c t", p=128), xacc[:, :, tsl(tt)],
                    ch_out[tt], (XA[tt],), ())

        Sd.finalize()
        for c in Sd.chans:
            if c.count > 0:
                c.sem = es.enter_context(nc.semaphore("c_" + c.name))
        with nc.Block() as block:
            Sd.emit(nc, block, engsem)
    return nc


def _alibi_slopes(n):
    return 2.0 ** (-8.0 * np.arange(1, n + 1, dtype=np.float64) / n)


def _host_consts(ln_g, ln_b, a_conv_w, c_pool_w, c_pool_scale):
    c = {}
    lnp = np.zeros((128, 128), np.float32)
    lnp[:, 0:64] = ln_g.reshape(8, 8, 128).transpose(2, 0, 1).reshape(128, 64)
    lnp[:, 64:128] = ln_b.reshape(8, 8, 128).transpose(2, 0, 1).reshape(128, 64)
    c["c_lnp"] = lnp
    c["c_convw"] = np.ascontiguousarray(a_conv_w.reshape(6, 6, 128).transpose(2, 0, 1).reshape(128, 36))
    c["c_pscale"] = np.ascontiguousarray(c_pool_scale.reshape(6, 128).T)
    c["c_ident"] = np.eye(128, dtype=np.float32)
    slopes = _alibi_slopes(12).reshape(3, 4)
    p = np.arange(128)[:, None]
    jl = np.arange(256)[None, :]
    delta = jl - 64 - p
    eb = np.zeros((128, 12, 256), np.float32)
    for g, (win, dil) in enumerate(DIL):
        for h in range(4):
            v = np.exp(-slopes[g, h] * np.abs(delta) * dil)
            eb[:, g * 4 + h, :] = np.where(np.abs(delta) <= 64, v, 0.0)
    c["c_expb"] = eb.reshape(128, 12 * 256)
    ed = np.ones((128, 4, 16), np.float32)
    for wi, w in enumerate(POOLW):
        for t in range(8):
            lo, hi = max(t - w // 2, 0), min(t + w // 2 - 1, S - 1)
            ed[:, wi, t] = w / float(hi - lo + 1)
            t2 = S - 8 + t
            lo, hi = max(t2 - w // 2, 0), min(t2 + w // 2 - 1, S - 1)
            ed[:, wi, 8 + t] = w / float(hi - lo + 1)
    c["c_edge"] = ed.reshape(128, 64)
    c["c_lstrict"] = np.triu(np.ones((128, 128), np.float32), 1)
    c["c_iota"] = np.ascontiguousarray(np.broadcast_to(np.arange(CAP, dtype=np.float32)[None, :], (128, CAP)))
    c["c_slotid"] = (np.arange(128, dtype=np.float32)[:, None] + 128.0 * np.arange(8, dtype=np.float32)[None, :])
    pw = np.zeros((MIXW, MIXW), np.float32)
    for g in range(4):
        pw[g * 192:(g + 1) * 192, g * 192:(g + 1) * 192] = c_pool_w[0, g]
    c["pwfull"] = pw
    return c


LAYERS = [0, 1, 2, 3]
NSEQ = 2
_CACHE = {}


def kernel(x, mem, w_mem_kv, a_w_in, a_conv_w, a_w_out, b_w_in, b_w_out,
           c_w_in, c_pool_w, c_pool_scale, c_w_out, ln_g, ln_b,
           ffn_w_gate, ffn_w_up, ffn_w_down, moe_router, moe_w_gate, moe_w_up, moe_w_down,
           _layers=None, _debug=False, _ncores=8):
    layers = LAYERS if _layers is None else _layers
    f32 = lambda a: np.ascontiguousarray(np.asarray(a, dtype=np.float32))
    x = f32(x)
    mem = f32(mem)
    consts = _host_consts(f32(ln_g), f32(ln_b), f32(a_conv_w), f32(c_pool_w), f32(c_pool_scale))
    shared = {
        "w_mem_kv": f32(w_mem_kv), "a_w_in": f32(a_w_in), "a_w_out": f32(a_w_out),
        "b_w_in": f32(b_w_in), "b_w_out": f32(b_w_out), "c_w_in": f32(c_w_in), "c_w_out": f32(c_w_out),
        "ffn_w_gate": f32(ffn_w_gate), "ffn_w_up": f32(ffn_w_up), "ffn_w_down": f32(ffn_w_down),
        "moe_router": f32(moe_router), "moe_w_gate": f32(moe_w_gate), "moe_w_up": f32(moe_w_up),
        "moe_w_down": f32(moe_w_down),
    }
    shared.update(consts)
    key = (tuple(layers), NSEQ, _debug)
    if key not in _CACHE:
        _CACHE[key] = build_program(layers, NSEQ, _debug)
    nc = _CACHE[key]
    in_maps = []
    for c in range(_ncores):
        m = dict(shared)
        m["xT"] = np.ascontiguousarray(x[c * NSEQ:(c + 1) * NSEQ].transpose(0, 2, 1))
        m["memT"] = np.ascontiguousarray(mem[c * NSEQ:(c + 1) * NSEQ].transpose(0, 2, 1))
        in_maps.append(m)
    res = run_bass_kernel_spmd(nc, in_maps, core_ids=list(range(_ncores)))
    out = np.empty((_ncores * NSEQ, S, D), np.float32)
    for c in range(_ncores):
        out[c * NSEQ:(c + 1) * NSEQ] = res.results[c]["yT"].transpose(0, 2, 1)
    if _debug:
        return out, [r.get("dbg") for r in res.results]
    return out
```

```python
import contextlib
import numpy as np
import concourse.bass as bass
import concourse.mybir as mybir
from concourse.bass_utils import run_bass_kernel_spmd

F32 = mybir.dt.float32
BF16 = mybir.dt.bfloat16
AF = mybir.ActivationFunctionType
ALU = mybir.AluOpType
AX = mybir.AxisListType

D = 1024
S = 2048
NT = 4
TS = 512
DEPTH = 4
ALPHA = float((2 * DEPTH) ** 0.25)
LN_EPS = 1e-5
MIXW = 768
DFF = 2816
DFFE = 3584
NE = 8
DIL = ((128, 1), (512, 4), (2048, 16))
POOLW = (2, 4, 8, 16)
ARENA_BYTES = 96 * 1024
MOE_MODE = "cap"
CAP = 640
NSC = CAP // 128
STILES = ((0, 512), (512, CAP - 512))


class Tk:
    __slots__ = ("name", "w", "r", "rd")

    def __init__(self, name=""):
        self.name = name
        self.w = None
        self.r = {}
        self.rd = []


class Chan:
    __slots__ = ("sem", "count", "name")

    def __init__(self, name):
        self.name = name
        self.sem = None
        self.count = 0


class Op:
    __slots__ = ("eng", "fn", "deps", "signal", "sigval", "kind", "chan", "dmaval")


ENGS = ("pe", "act", "dve", "pool", "sp")


class Sched:
    def __init__(self):
        self.ops = []
        self.extra = {e: [] for e in ENGS}
        self.last = {e: None for e in ENGS}
        self.chans = []
        self.last_dma = {}

    def chan(self, name):
        c = Chan(name)
        self.chans.append(c)
        return c

    def op(self, eng, fn, reads=(), writes=(), chan=None):
        o = Op()
        o.eng = eng
        o.fn = fn
        o.signal = False
        o.sigval = None
        o.kind = "d" if chan is not None else "c"
        o.chan = chan
        o.dmaval = None
        deps = {}
        for t in reads:
            if t.w is not None:
                deps[id(t.w)] = t.w
        for t in writes:
            if t.w is not None:
                deps[id(t.w)] = t.w
            for r in t.r.values():
                deps[id(r)] = r
            for r in t.rd:
                deps[id(r)] = r
        for d in self.extra[eng]:
            deps[id(d)] = d
        self.extra[eng] = []
        dl = []
        for d in deps.values():
            if d is o:
                continue
            if d.kind == "c" and o.kind == "c" and d.eng == "pe" and eng == "pe":
                continue
            d.signal = True
            dl.append(d)
        o.deps = dl
        if chan is not None:
            chan.count += 16
            o.dmaval = chan.count
            self.last_dma[id(chan)] = o
        for t in reads:
            if o.kind == "d":
                t.rd.append(o)
            else:
                t.r[eng] = o
        for t in writes:
            t.w = o
            t.r = {}
            t.rd = []
        if o.kind == "c":
            self.last[eng] = o
        self.ops.append(o)
        return o

    def barrier(self):
        deps = [o for o in self.last.values() if o is not None]
        deps += list(self.last_dma.values())
        for e in ENGS:
            self.extra[e] = list(deps)

    def finalize(self):
        cnt = {e: 0 for e in ENGS}
        for o in self.ops:
            if o.kind == "c" and o.signal:
                cnt[o.eng] += 1
                o.sigval = cnt[o.eng]
        self.sigcounts = cnt

    def emit(self, nc, block, engsem):
        handles = {"pe": "tensor", "act": "scalar", "dve": "vector", "pool": "gpsimd", "sp": "sync"}
        final = [(c.sem, c.count) for c in self.chans if c.count > 0]

        def run(engname):
            def body(e):
                waited = {}
                for o in self.ops:
                    if o.eng != engname:
                        continue
                    for d in o.deps:
                        if d.kind == "d":
                            sem, val = d.chan.sem, d.dmaval
                        else:
                            sem, val = engsem[d.eng], d.sigval
                        k = id(sem)
                        if waited.get(k, 0) >= val:
                            continue
                        e.wait_ge(sem, val)
                        waited[k] = val
                    ins = o.fn(e)
                    if o.kind == "d":
                        ins.then_inc(o.chan.sem, 16)
                    elif o.signal:
                        ins.then_inc(engsem[o.eng], 1)
                if engname == "sp":
                    for sem, val in final:
                        e.wait_ge(sem, val)
            return body

        for en in ENGS:
            getattr(block, handles[en])(run(en))


def build_program(layers, nseq, debug=False):
    nc = bass.Bass("TRN2", target_bir_lowering=False)
    Sd = Sched()

    def dram_in(name, shape):
        return nc.dram_tensor(name, list(shape), F32, kind="ExternalInput").ap()

    xT = dram_in("xT", [nseq, D, S])
    memT = dram_in("memT", [nseq, D, 256])
    w_mem_kv = dram_in("w_mem_kv", [D, 512])
    a_w_in = dram_in("a_w_in", [2, D, 2560])
    a_w_out = dram_in("a_w_out", [2, D, D])
    b_w_in = dram_in("b_w_in", [1, D, 2560])
    b_w_out = dram_in("b_w_out", [1, 512, D])
    c_w_in = dram_in("c_w_in", [1, D, D])
    c_w_out = dram_in("c_w_out", [1, D, D])
    pwfull = dram_in("pwfull", [MIXW, MIXW])
    ffn_w_gate = dram_in("ffn_w_gate", [2, D, DFF])
    ffn_w_up = dram_in("ffn_w_up", [2, D, DFF])
    ffn_w_down = dram_in("ffn_w_down", [2, DFF, D])
    moe_router = dram_in("moe_router", [2, D, NE])
    moe_w_gate = dram_in("moe_w_gate", [2, NE, D, DFFE])
    moe_w_up = dram_in("moe_w_up", [2, NE, D, DFFE])
    moe_w_down = dram_in("moe_w_down", [2, NE, DFFE, D])
    c_lnp = dram_in("c_lnp", [128, 128])
    c_convw = dram_in("c_convw", [128, 36])
    c_pscale = dram_in("c_pscale", [128, 6])
    c_ident = dram_in("c_ident", [128, 128])
    c_expb = dram_in("c_expb", [128, 12 * 256])
    c_edge = dram_in("c_edge", [128, 64])
    c_lstrict = dram_in("c_lstrict", [128, 128])
    c_iota = dram_in("c_iota", [128, CAP])
    c_slotid = dram_in("c_slotid", [128, 8])
    yT = nc.dram_tensor("yT", [nseq, D, S], F32, kind="ExternalOutput").ap()
    coef_scr = nc.dram_tensor("coef_scr", [NE, S], F32, kind="Internal").ap()
    pos_scr = nc.dram_tensor("pos_scr", [NE, S], F32, kind="Internal").ap()
    xd_scr = nc.dram_tensor("xd_scr", [NE, D, CAP], BF16, kind="Internal").ap()
    dbg_out = None
    if debug:
        dbg_out = nc.dram_tensor("dbg", [len(layers), nseq, D, S], F32, kind="ExternalOutput").ap()

    es = contextlib.ExitStack()
    with es:
        def sb(name, shape, dt):
            return es.enter_context(nc.sbuf_tensor(name, list(shape), dt))

        xacc = sb("xacc", [128, 8, S], F32)
        xb = sb("xb", [128, 8, S], BF16)
        lnp = sb("lnp", [128, 256], F32)
        convw = sb("convw", [128, 36], F32)
        pscale = sb("pscale", [128, 6], F32)
        router = sb("router", [128, 2, 8, NE], F32)
        ident = sb("ident", [128, 128], F32)
        ones32 = sb("ones32", [128, 128], F32)
        onespad = sb("onespad", [128, 2, 128], BF16)
        kTpad = sb("kTpad", [128, 4, 256], BF16)
        vpad = sb("vpad", [128, 4, 2, 128], BF16)
        expb = sb("expb", [128, 12, 256], BF16)
        edge = sb("edge", [128, 4, 16], F32)
        slotid = sb("slotid", [128, 8], F32)
        identb = sb("identb", [128, 128], BF16)
        arena = sb("arena", [128, ARENA_BYTES // 2], BF16)
        ps = es.enter_context(nc.psum_tensor("ps", [128, 8, 512], F32))

        engsem = {e: es.enter_context(nc.semaphore("s_" + e)) for e in ENGS}

        XA = [Tk("xa%d" % t) for t in range(NT)]
        XB = [Tk("xb%d" % t) for t in range(NT)]
        PB = [Tk("ps%d" % b) for b in range(8)]
        CONST = Tk("const")
        KV = Tk("kv")
        ch_const = Sd.chan("const")
        ch_x = [Sd.chan("x%d" % t) for t in range(NT)]
        ch_out = [Sd.chan("o%d" % t) for t in range(NT)]
        ch_w = [Sd.chan("w%d" % i) for i in range(9)]
        ch_misc = [Sd.chan("m%d" % i) for i in range(4)]
        ch_coef = Sd.chan("coef")
        ch_cb = [Sd.chan("cb%d" % i) for i in range(2)]
        ch_xd = [Sd.chan("xd%d" % i) for i in range(2)]
        ch_xdl = [Sd.chan("xdl%d" % i) for i in range(2)]
        ch_pos = Sd.chan("pos")

        def tsl(tt):
            return slice(tt * TS, (tt + 1) * TS)

        class Carver:
            def __init__(self, base=None, nbytes=ARENA_BYTES):
                self.off = 0
                self.base = arena if base is None else base
                self.nbytes = nbytes

            def take(self, shape, dt):
                esz = 4 if dt == F32 else 2
                n = 1
                for s_ in shape[1:]:
                    n *= s_
                nbytes = n * esz
                assert self.off % 4 == 0
                assert self.off + nbytes <= self.nbytes, (self.off, nbytes)
                v = self.base[:, self.off // 2:(self.off + nbytes) // 2]
                if dt == F32:
                    v = v.bitcast(F32)
                self.off += nbytes
                if shape[0] != 128:
                    v = v[0:shape[0]]
                if len(shape) == 3:
                    v = v.rearrange("p (a b) -> p a b", a=shape[1])
                elif len(shape) == 4:
                    v = v.rearrange("p (a b c) -> p a b c", a=shape[1], b=shape[2])
                return v

        def dma(eng, out, in_, chan, reads=(), writes=()):
            Sd.op(eng, lambda e, o=out, i=in_: e.dma_start(out=o, in_=i), reads, writes, chan=chan)

        def mm(out, pairs, reads, writes):
            def fn(e, out=out, pairs=pairs):
                n = len(pairs)
                ins = None
                for i, (l, r) in enumerate(pairs):
                    ins = e.matmul(out, l, r, start=(i == 0), stop=(i == n - 1))
                return ins
            Sd.op("pe", fn, reads, writes)

        def act(out, in_, func, reads, writes, bias=None, scale=None):
            kw = {}
            if bias is not None:
                kw["bias"] = bias
            if scale is not None:
                kw["scale"] = scale
            Sd.op("act", lambda e, o=out, i=in_, f=func, kw=kw: e.activation(o, i, f, **kw), reads, writes)

        def tt_(eng, out, in0, in1, op, reads, writes):
            Sd.op(eng, lambda e, o=out, a=in0, b=in1, op=op: e.tensor_tensor(o, a, b, op), reads, writes)

        def ts_(eng, out, in0, s1, s2, op0, op1, reads, writes):
            if op1 is None:
                Sd.op(eng, lambda e, o=out, a=in0, s1=s1, op0=op0: e.tensor_scalar(o, a, s1, None, op0), reads, writes)
            else:
                Sd.op(eng, lambda e, o=out, a=in0, s1=s1, s2=s2, op0=op0, op1=op1:
                      e.tensor_scalar(o, a, s1, s2, op0, op1), reads, writes)

        def stt(eng, out, in0, scalar, in1, op0, op1, reads, writes):
            Sd.op(eng, lambda e, o=out, a=in0, s=scalar, b=in1, op0=op0, op1=op1:
                  e.scalar_tensor_tensor(o, a, s, b, op0, op1), reads, writes)

        def cp(eng, out, in_, reads, writes):
            Sd.op(eng, lambda e, o=out, i=in_: e.tensor_copy(o, i), reads, writes)

        def mset(eng, ap, val, writes):
            Sd.op(eng, lambda e, a=ap, v=val: e.memset(a, v), (), writes)

        dma("sp", lnp[:, 0:128], c_lnp, ch_const, (), (CONST,))
        dma("sp", convw[:], c_convw, ch_const, (), (CONST,))
        dma("sp", pscale[:], c_pscale, ch_const, (), (CONST,))
        dma("sp", ident[:], c_ident, ch_const, (), (CONST,))
        dma("pool", expb[:].rearrange("p a b -> p (a b)"), c_expb, ch_const, (), (CONST,))
        dma("sp", edge[:].rearrange("p a b -> p (a b)"), c_edge, ch_const, (), (CONST,))
        for f in range(2):
            dma("sp", router[:, f], moe_router[f].rearrange("(c p) e -> p c e", p=128), ch_const, (), (CONST,))
        dma("sp", slotid[:], c_slotid, ch_const, (), (CONST,))
        Sd.op("act", lambda e: e.mul(lnp[:, 128:256], lnp[:, 0:128], ALPHA), (CONST,), (CONST,))
        cp("dve", identb[:], ident[:], (CONST,), (CONST,))
        mset("dve", ones32[:], 1.0, (CONST,))
        mset("dve", onespad[:], 0.0, (CONST,))
        mset("dve", onespad[:, 0, 0:64], 1.0, (CONST,))
        mset("dve", onespad[:, 1, 64:128], 1.0, (CONST,))
        mset("dve", kTpad[:], 0.0, (KV,))
        mset("dve", vpad[:], 0.0, (KV,))

        def lncol(l, j, c):
            return (l * 2 + j) * 8 + c

        def layer_norm(tt, l, j, sq, tmp, SQ, TMP, final, bank_a, bank_b):
            v = xacc[:, :, tsl(tt)]
            act(sq, v, AF.Square, (XA[tt],), (SQ,))
            mm(ps[:, bank_a, :], [(ones32[:], xacc[:, c, tsl(tt)]) for c in range(8)],
               (XA[tt], CONST), (PB[bank_a],))
            mm(ps[:, bank_b, :], [(ones32[:], sq[:, c, :]) for c in range(8)], (SQ, CONST), (PB[bank_b],))
            mean, msq, var, rstd = tmp[:, 0], tmp[:, 1], tmp[:, 2], tmp[:, 3]
            Sd.op("act", lambda e: e.mul(mean, ps[:, bank_a, :], 1.0 / D), (PB[bank_a],), (TMP[0],))
            tt_("dve", msq, mean, mean, ALU.mult, (TMP[0],), (TMP[1],))
            stt("dve", var, ps[:, bank_b, :], 1.0 / D, msq, ALU.mult, ALU.subtract, (PB[bank_b], TMP[1]), (TMP[2],))
            ts_("dve", var, var, LN_EPS, None, ALU.add, None, (TMP[2],), (TMP[2],))
            act(var, var, AF.Sqrt, (TMP[2],), (TMP[2],))
            Sd.op("dve", lambda e: e.reciprocal(rstd, var), (TMP[2],), (TMP[3],))
            mb = mean.unsqueeze(1).to_broadcast([128, 8, TS])
            rb = rstd.unsqueeze(1).to_broadcast([128, 8, TS])
            tt_("dve", sq, v, mb, ALU.subtract, (XA[tt], TMP[0]), (SQ,))
            tt_("pool", sq, sq, rb, ALU.mult, (SQ, TMP[3]), (SQ,))
            for c in range(8):
                col = lncol(l, j, c)
                g, b_ = lnp[:, col:col + 1], lnp[:, 64 + col:64 + col + 1]
                ga, ba = lnp[:, 128 + col:128 + col + 1], lnp[:, 192 + col:192 + col + 1]
                if final:
                    act(xacc[:, c, tsl(tt)], sq[:, c, :], AF.Identity, (SQ, CONST), (XA[tt],), bias=b_, scale=g)
                else:
                    act(xb[:, c, tsl(tt)], sq[:, c, :], AF.Identity, (SQ, CONST), (XB[tt],), bias=b_, scale=g)
                    act(xacc[:, c, tsl(tt)], sq[:, c, :], AF.Identity, (SQ, CONST), (XA[tt],), bias=ba, scale=ga)

        def load_w_cols(slot, slot_tk, chan, src2d, colgroups):
            off = 0
            for (c0, n) in colgroups:
                dma("pool", slot[:, :, off:off + n], src2d[:, c0:c0 + n].rearrange("(c p) e -> p c e", p=128),
                    chan, (), (slot_tk,))
                off += n

        def mem_attention(qm, QM, memout, MO, pT, PT, rden, RD):
            for tt in range(NT):
                for pr in range(2):
                    bn, bd = 4 + (pr % 2) * 2, 5 + (pr % 2) * 2
                    for hh in range(2):
                        h = 2 * pr + hh
                        b0 = (hh % 2) * 2
                        for mc in range(2):
                            mm(ps[:, b0 + mc, :], [(kTpad[:, h, mc * 128:(mc + 1) * 128], qm[:, pr, tsl(tt)])],
                               (KV, QM[tt]), (PB[b0 + mc],))
                        act(pT[:, hh], ps[:, b0:b0 + 2, :], AF.Exp, (PB[b0], PB[b0 + 1]), (PT[hh],))
                    mm(ps[:, bn, :], [(vpad[:, 2 * pr + hh, mc, :], pT[:, hh, mc, :]) for hh in range(2) for mc in range(2)],
                       (KV, PT[0], PT[1]), (PB[bn],))
                    mm(ps[:, bd, :], [(onespad[:, hh, :], pT[:, hh, mc, :]) for hh in range(2) for mc in range(2)],
                       (CONST, PT[0], PT[1]), (PB[bd],))
                    Sd.op("dve", lambda e, o=rden[:, pr % 2, :], i=ps[:, bd, :]: e.reciprocal(o, i), (PB[bd],), (RD[pr % 2],))
                    tt_("dve", memout[:, pr, tsl(tt)], ps[:, bn, :], rden[:, pr % 2, :], ALU.mult,
                        (PB[bn], RD[pr % 2]), (MO[tt],))

        def outproj_ln(l, wout2d, nmix, cat_aps, CAT, car):
            kc_n = len(cat_aps)
            wo = car.take([128, kc_n, D], BF16)
            WO = Tk("wo")
            dma("pool", wo, wout2d.rearrange("(c p) e -> p c e", p=128), ch_w[0], (), (WO,))
            sq = [car.take([128, 8, TS], F32) for _ in range(2)]
            tmp = [car.take([128, 4, TS], F32) for _ in range(2)]
            SQ = [Tk("sq0"), Tk("sq1")]
            TMP = [[Tk("tmp%d" % i) for i in range(4)] for _ in range(2)]
            for tt in range(NT):
                for dc in range(8):
                    bank = dc % 4
                    mm(ps[:, bank, :], [(wo[:, kc, dc * 128:(dc + 1) * 128], cat_aps[kc][:, tsl(tt)]) for kc in range(kc_n)],
                       (WO,) + tuple(CAT[tt]), (PB[bank],))
                    tt_("dve", xacc[:, dc, tsl(tt)], ps[:, bank, :], xacc[:, dc, tsl(tt)], ALU.add,
                        (PB[bank], XA[tt]), (XA[tt],))
                layer_norm(tt, l, 0, sq[tt % 2], tmp[tt % 2], SQ[tt % 2], TMP[tt % 2], False, 4 + (tt % 2) * 2, 5 + (tt % 2) * 2)

        def inproj_block(slot, SLOT, ncols, consumer):
            nch = ncols // 128
            k = 0
            for tt in range(NT):
                for ec in range(nch):
                    bank = k % 4
                    k += 1
                    mm(ps[:, bank, :], [(slot[:, kc, ec * 128:(ec + 1) * 128], xb[:, kc, tsl(tt)]) for kc in range(8)],
                       (SLOT, XB[tt]), (PB[bank],))
                    consumer(tt, ec, bank)

        def mixer_a(l, jA):
            car = Carver()
            mix = car.take([128, 6, S], BF16)
            memout = car.take([128, 2, S], BF16)
            qm = car.take([128, 2, S], BF16)
            slots = [car.take([128, 8, 384], BF16) for _ in range(2)]
            SL = [Tk("sl0"), Tk("sl1")]
            gcs = car.take([128, 2, TS], F32)
            GCS = [Tk("gcs0"), Tk("gcs1")]
            z = car.take([128, S + 2], F32)
            Z = Tk("z")
            gb = car.take([128, S], BF16)
            GB = Tk("gb")
            tmpc = car.take([128, 2, S], F32)
            TC = [Tk("tc0"), Tk("tc1")]
            pT = car.take([128, 2, 2, TS], BF16)
            PT = [Tk("pt0"), Tk("pt1")]
            rden = car.take([128, 2, TS], F32)
            RD = [Tk("rd0"), Tk("rd1")]
            MIX = [Tk("mix%d" % t) for t in range(NT)]
            MO = [Tk("mo%d" % t) for t in range(NT)]
            QM = [Tk("qm%d" % t) for t in range(NT)]
            w2d = a_w_in[jA]
            mset("pool", z[:, 0:1], 0.0, (Z,))
            mset("pool", z[:, S + 1:S + 2], 0.0, (Z,))
            for c in range(7):
                si = c % 2
                if c < 6:
                    load_w_cols(slots[si], SL[si], ch_w[1 + si], w2d,
                                [(c * 128, 128), (768 + c * 128, 128), (1536 + c * 128, 128)])
                else:
                    load_w_cols(slots[si], SL[si], ch_w[1 + si], w2d, [(2304, 256)])
                if c < 6:
                    def consumer(tt, ec, bank, c=c):
                        if ec == 0:
                            act(gb[:, tsl(tt)], ps[:, bank, :], AF.Copy, (PB[bank],), (GB,))
                        elif ec == 1:
                            act(gcs[:, tt % 2, :], ps[:, bank, :], AF.Copy, (PB[bank],), (GCS[tt % 2],))
                        else:
                            tt_("dve", z[:, 1 + tt * TS:1 + (tt + 1) * TS], ps[:, bank, :], gcs[:, tt % 2, :], ALU.mult,
                                (PB[bank], GCS[tt % 2]), (Z,))
                    inproj_block(slots[si], SL[si], 384, consumer)
                    wc = lambda k, c=c: convw[:, (jA * 3 + k) * 6 + c:(jA * 3 + k) * 6 + c + 1]
                    act(tmpc[:, 0, :], z[:, 0:S], AF.Identity, (Z, CONST), (TC[0],), scale=wc(0))
                    stt("dve", tmpc[:, 1, :], z[:, 1:S + 1], wc(1), tmpc[:, 0, :], ALU.mult, ALU.add,
                        (Z, CONST, TC[0]), (TC[1],))
                    stt("dve", tmpc[:, 0, :], z[:, 2:S + 2], wc(2), tmpc[:, 1, :], ALU.mult, ALU.add,
                        (Z, CONST, TC[1]), (TC[0],))
                    tt_("pool", mix[:, c, :], tmpc[:, 0, :], gb[:], ALU.mult, (TC[0], GB), tuple(MIX))
                else:
                    def consumer(tt, ec, bank):
                        Sd.op("act", lambda e, o=qm[:, ec, tsl(tt)], i=ps[:, bank, :]: e.mul(o, i, 0.125),
                              (PB[bank],), (QM[tt],))
                    inproj_block(slots[si], SL[si], 256, consumer)
            mem_attention(qm, QM, memout, MO, pT, PT, rden, RD)
            Sd.barrier()
            car2 = Carver()
            car2.take([128, 8, S], BF16)
            cat = [mix[:, c, :] for c in range(6)] + [memout[:, c, :] for c in range(2)]
            CAT = [(MIX[t], MO[t]) for t in range(NT)]
            outproj_ln(l, a_w_out[jA], 6, cat, CAT, car2)
            Sd.barrier()

        def mixer_c(l, jC):
            car = Carver()
            mix = car.take([128, 6, S], BF16)
            memout = car.take([128, 2, S], BF16)
            qm = car.take([128, 2, S], BF16)
            slots = [car.take([128, 8, 256], BF16) for _ in range(2)]
            SL = [Tk("sl0"), Tk("sl1")]
            pw = car.take([128, 6, MIXW], BF16)
            PW = Tk("pw")
            PADW = 16
            bufs = [car.take([128, S + 2 * PADW], F32) for _ in range(3)]
            BU = [Tk("bu%d" % i) for i in range(3)]
            pT = car.take([128, 2, 2, TS], BF16)
            PT = [Tk("pt0"), Tk("pt1")]
            rden = car.take([128, 2, TS], F32)
            RD = [Tk("rd0"), Tk("rd1")]
            etmp = car.take([128, 16], F32)
            ET = Tk("et")
            MIX = [Tk("mix%d" % t) for t in range(NT)]
            MO = [Tk("mo%d" % t) for t in range(NT)]
            QM = [Tk("qm%d" % t) for t in range(NT)]
            w2d = c_w_in[jC]
            dma("pool", pw, pwfull.rearrange("(c p) e -> p c e", p=128), ch_w[3], (), (PW,))
            for i in range(3):
                mset("pool", bufs[i][:, 0:PADW], 0.0, (BU[i],))
                mset("pool", bufs[i][:, S + PADW:S + 2 * PADW], 0.0, (BU[i],))
            ub, pa, pb = bufs

            def window(prs, wi):
                w = POOLW[wi]
                P = PADW
                lo, hi = -15, S + 15
                tt_("dve", pa[prs, P + lo:P + hi], ub[prs, P + lo - 1:P + hi - 1], ub[prs, P + lo:P + hi], ALU.add,
                    (BU[0],), (BU[1],))
                cur, other, curi, othi = pa, pb, 1, 2
                step = 1
                lo_c, hi_c = lo, hi
                ww = 2
                while ww < w:
                    lo_n, hi_n = lo_c + step, hi_c - step
                    tt_("dve", other[prs, P + lo_n:P + hi_n], cur[prs, P + lo_n - step:P + hi_n - step],
                        cur[prs, P + lo_n + step:P + hi_n + step], ALU.add, (BU[curi],), (BU[othi],))
                    cur, other, curi, othi = other, cur, othi, curi
                    lo_c, hi_c = lo_n, hi_n
                    step *= 2
                    ww *= 2
                return cur, curi

            def pool_half(c, prs, wi):
                cur, curi = window(prs, wi)
                w = POOLW[wi]
                P = PADW
                tt_("pool", cur[prs, P:P + 8], cur[prs, P:P + 8], edge[prs, wi, 0:8], ALU.mult, (BU[curi], CONST), (BU[curi],))
                tt_("pool", cur[prs, P + S - 8:P + S], cur[prs, P + S - 8:P + S], edge[prs, wi, 8:16], ALU.mult,
                    (BU[curi], CONST), (BU[curi],))
                stt("dve", mix[prs, c, :], cur[prs, P:P + S], 1.0 / w, ub[prs, P:P + S], ALU.mult, ALU.subtract,
                    (BU[curi], BU[0]), tuple(MIX))

            for c in range(4):
                si = c % 2
                if c < 3:
                    load_w_cols(slots[si], SL[si], ch_w[1 + si], w2d, [(c * 256, 256)])
                    for ecl in range(2):
                        cc = c * 2 + ecl
                        for tt in range(NT):
                            bank = tt % 4
                            mm(ps[:, bank, :], [(slots[si][:, kc, ecl * 128:(ecl + 1) * 128], xb[:, kc, tsl(tt)]) for kc in range(8)],
                               (SL[si], XB[tt]), (PB[bank],))
                            act(ub[:, PADW + tt * TS:PADW + (tt + 1) * TS], ps[:, bank, :], AF.Copy, (PB[bank],), (BU[0],))
                        g0 = (cc * 128) // 192
                        g1 = (cc * 128 + 64) // 192
                        if g0 == g1:
                            pool_half(cc, slice(0, 128), g0)
                        else:
                            pool_half(cc, slice(0, 64), g0)
                            pool_half(cc, slice(64, 128), g1)
                else:
                    load_w_cols(slots[si], SL[si], ch_w[1 + si], w2d, [(768, 256)])

                    def consumer(tt, ec, bank):
                        Sd.op("act", lambda e, o=qm[:, ec, tsl(tt)], i=ps[:, bank, :]: e.mul(o, i, 0.125),
                              (PB[bank],), (QM[tt],))
                    inproj_block(slots[si], SL[si], 256, consumer)
            for tt in range(NT):
                for dc in range(6):
                    mm(ps[:, dc, :], [(pw[:, kc, dc * 128:(dc + 1) * 128], mix[:, kc, tsl(tt)]) for kc in range(6)],
                       (PW, MIX[tt]), (PB[dc],))
                for dc in range(6):
                    act(mix[:, dc, tsl(tt)], ps[:, dc, :], AF.Identity, (PB[dc], CONST), (MIX[tt],), scale=pscale[:, dc:dc + 1])
            mem_attention(qm, QM, memout, MO, pT, PT, rden, RD)
            Sd.barrier()
            car2 = Carver()
            car2.take([128, 8, S], BF16)
            cat = [mix[:, c, :] for c in range(6)] + [memout[:, c, :] for c in range(2)]
            CAT = [(MIX[t], MO[t]) for t in range(NT)]
            outproj_ln(l, c_w_out[jC], 6, cat, CAT, car2)
            Sd.barrier()

        def mixer_b(l, jB):
            car = Carver()
            mix = car.take([128, 2, S], BF16)
            memout = car.take([128, 2, S], BF16)
            qm = car.take([128, 2, S], BF16)
            slots = [car.take([128, 8, 384], BF16) for _ in range(2)]
            SL = [Tk("sl0"), Tk("sl1")]
            qs = car.take([128, S], BF16)
            QS = Tk("qs")
            kEO = car.take([128, 2, S], BF16)
            KEO = Tk("keo")
            vEO = car.take([128, 2, 16, 128], BF16)
            VEO = Tk("veo")
            nacc = car.take([128, S], F32)
            dacc = car.take([128, S], F32)
            NA, DA = Tk("na"), Tk("da")
            ebuf = car.take([128, 2, 2, 256], F32)
            EB = [Tk("eb0"), Tk("eb1")]
            pTd = car.take([128, 2, 2, 256], BF16)
            PTD = [Tk("ptd0"), Tk("ptd1")]
            pT = car.take([128, 2, 2, TS], BF16)
            PT = [Tk("pt0"), Tk("pt1")]
            rden = car.take([128, 2, TS], F32)
            RD = [Tk("rd0"), Tk("rd1")]
            MIX = [Tk("mix%d" % t) for t in range(NT)]
            MO = [Tk("mo%d" % t) for t in range(NT)]
            QM = [Tk("qm%d" % t) for t in range(NT)]
            w2d = b_w_in[jB]
            mset("pool", kEO[:], 0.0, (KEO,))
            mset("pool", vEO[:], 0.0, (VEO,))
            it = 0
            for pr in range(2):
                mset("pool", nacc[:], 0.0, (NA,))
                mset("pool", dacc[:], 0.0, (DA,))
                for g, (win, dil) in enumerate(DIL):
                    ci = g * 2 + pr
                    si = it % 2
                    it += 1
                    load_w_cols(slots[si], SL[si], ch_w[1 + si], w2d,
                                [(ci * 128, 128), (768 + ci * 128, 128), (1536 + ci * 128, 128)])
                    slot = slots[si]
                    for tt in range(NT):
                        b0 = (tt % 2) * 2
                        mm(ps[:, b0, :], [(slot[:, kc, 0:128], xb[:, kc, tsl(tt)]) for kc in range(8)],
                           (SL[si], XB[tt]), (PB[b0],))
                        Sd.op("act", lambda e, o=qs[:, tsl(tt)], i=ps[:, b0, :]: e.mul(o, i, 0.125), (PB[b0],), (QS,))
                        mm(ps[:, b0 + 1, :], [(slot[:, kc, 128:256], xb[:, kc, tsl(tt)]) for kc in range(8)],
                           (SL[si], XB[tt]), (PB[b0 + 1],))
                        act(kEO[0:64, 0, tsl(tt)], ps[0:64, b0 + 1, :], AF.Copy, (PB[b0 + 1],), (KEO,))
                        cp("dve", kEO[64:128, 1, tsl(tt)], ps[64:128, b0 + 1, :], (PB[b0 + 1],), (KEO,))
                    n_sub = S // dil
                    nkb = n_sub // 128
                    ti = 0
                    for r in range(dil):
                        for kb in range(nkb):
                            bank = 4 + (ti // 4) % 2
                            sub = (ti % 4) * 128
                            start = kb * 128 * dil + r
                            toks = slice(start, start + 127 * dil + 1, dil)
                            mm(ps[:, bank, sub:sub + 128], [(xb[:, kc, toks], slot[:, kc, 256:384]) for kc in range(8)],
                               (SL[si],) + tuple(XB), (PB[bank],))
                            act(vEO[:, 0, ti, 0:64], ps[:, bank, sub:sub + 64], AF.Copy, (PB[bank],), (VEO,))
                            cp("dve", vEO[:, 1, ti, 64:128], ps[:, bank, sub + 64:sub + 128], (PB[bank],), (VEO,))
                            ti += 1
                    ti = 0
                    for r in range(dil):
                        for kb in range(nkb):
                            j0 = max(0, kb * 128 - 64)
                            j1 = min(n_sub, kb * 128 + 192)
                            nq = j1 - j0
                            col0 = j0 - (kb * 128 - 64)
                            kst = kb * 128 * dil + r
                            ktoks = slice(kst, kst + 127 * dil + 1, dil)
                            qst = j0 * dil + r
                            qtoks = slice(qst, qst + (nq - 1) * dil + 1, dil)
                            bp = ti % 2
                            for hh in range(2):
                                mm(ps[:, 2 * bp + hh, 0:nq], [(kEO[:, hh, ktoks], qs[:, qtoks])], (KEO, QS), (PB[2 * bp + hh],))
                            act(ebuf[:, bp, :, 0:nq], ps[:, 2 * bp:2 * bp + 2, 0:nq], AF.Exp, (PB[2 * bp], PB[2 * bp + 1]), (EB[bp],))
                            tt_("pool", pTd[:, bp, :, 0:nq], ebuf[:, bp, :, 0:nq],
                                expb[:, g * 4 + 2 * pr:g * 4 + 2 * pr + 2, col0:col0 + nq], ALU.mult,
                                (EB[bp], CONST), (PTD[bp],))
                            bn, bd = 4 + bp, 6 + bp
                            mm(ps[:, bn, 0:nq], [(vEO[:, hh, ti, :], pTd[:, bp, hh, 0:nq]) for hh in range(2)],
                               (VEO, PTD[bp]), (PB[bn],))
                            mm(ps[:, bd, 0:nq], [(onespad[:, hh, :], pTd[:, bp, hh, 0:nq]) for hh in range(2)],
                               (CONST, PTD[bp]), (PB[bd],))
                            tt_("dve", nacc[:, qtoks], ps[:, bn, 0:nq], nacc[:, qtoks], ALU.add, (PB[bn], NA), (NA,))
                            tt_("dve", dacc[:, qtoks], ps[:, bd, 0:nq], dacc[:, qtoks], ALU.add, (PB[bd], DA), (DA,))
                            ti += 1
                Sd.op("dve", lambda e: e.reciprocal(dacc[:], dacc[:]), (DA,), (DA,))
                tt_("pool", mix[:, pr, :], nacc[:], dacc[:], ALU.mult, (NA, DA), tuple(MIX))
            si = it % 2
            load_w_cols(slots[si], SL[si], ch_w[1 + si], w2d, [(2304, 256)])

            def consumer(tt, ec, bank):
                Sd.op("act", lambda e, o=qm[:, ec, tsl(tt)], i=ps[:, bank, :]: e.mul(o, i, 0.125),
                      (PB[bank],), (QM[tt],))
            inproj_block(slots[si][:, :, 0:256], SL[si], 256, consumer)
            mem_attention(qm, QM, memout, MO, pT, PT, rden, RD)
            Sd.barrier()
            car2 = Carver()
            car2.take([128, 4, S], BF16)
            cat = [mix[:, c, :] for c in range(2)] + [memout[:, c, :] for c in range(2)]
            CAT = [(MIX[t], MO[t]) for t in range(NT)]
            outproj_ln(l, b_w_out[jB], 2, cat, CAT, car2)
            Sd.barrier()

        class FFNState:
            pass

        def ffn_setup():
            st = FFNState()
            car = Carver()
            st.wg = [car.take([128, 8, 512], BF16) for _ in range(2)]
            st.wu = [car.take([128, 8, 512], BF16) for _ in range(2)]
            st.wd = [car.take([128, 4, D], BF16) for _ in range(2)]
            st.WS = [Tk("ws0"), Tk("ws1")]
            st.h = car.take([128, 2, 4, TS], BF16)
            st.H = [Tk("h0"), Tk("h1")]
            st.sg = car.take([128, 2, TS], F32)
            st.SG = [Tk("sg0"), Tk("sg1")]
            st.sgc = car.take([128, 2, TS], F32)
            st.SGC = [Tk("sgc0"), Tk("sgc1")]
            st.cb = car.take([128, 2, S], F32)
            st.CB = [Tk("cb0"), Tk("cb1")]
            st.blk = 0
            st.car = car
            return st

        def ffn_pass(st, wg2d, wu2d, wd2d, dff, coef_slot):
            f0 = 0
            while f0 < dff:
                fb = min(512, dff - f0)
                nfc = fb // 128
                si = st.blk % 2
                st.blk += 1
                ch = ch_w[si * 3:(si * 3) + 3]
                dma("pool", st.wg[si][:, :, 0:fb], wg2d[:, f0:f0 + fb].rearrange("(c p) e -> p c e", p=128), ch[0], (), (st.WS[si],))
                dma("pool", st.wu[si][:, :, 0:fb], wu2d[:, f0:f0 + fb].rearrange("(c p) e -> p c e", p=128), ch[1], (), (st.WS[si],))
                dma("pool", st.wd[si][:, 0:nfc, :], wd2d[f0:f0 + fb, :].rearrange("(c p) e -> p c e", p=128), ch[2], (), (st.WS[si],))
                WSt = st.WS[si]

                def gu(tt, fc, si=si, WSt=WSt):
                    bg, bu = (fc % 2) * 2, (fc % 2) * 2 + 1
                    hp = tt % 2
                    mm(ps[:, bg, :], [(st.wg[si][:, kc, fc * 128:(fc + 1) * 128], xb[:, kc, tsl(tt)]) for kc in range(8)],
                       (WSt, XB[tt]), (PB[bg],))
                    mm(ps[:, bu, :], [(st.wu[si][:, kc, fc * 128:(fc + 1) * 128], xb[:, kc, tsl(tt)]) for kc in range(8)],
                       (WSt, XB[tt]), (PB[bu],))
                    act(st.sg[:, fc % 2, :], ps[:, bg, :], AF.Silu, (PB[bg],), (st.SG[fc % 2],))
                    if coef_slot is None:
                        tt_("dve", st.h[:, hp, fc, :], ps[:, bu, :], st.sg[:, fc % 2, :], ALU.mult,
                            (PB[bu], st.SG[fc % 2]), (st.H[hp],))
                    else:
                        tt_("pool", st.sgc[:, fc % 2, :], st.sg[:, fc % 2, :], st.cb[:, coef_slot, tsl(tt)], ALU.mult,
                            (st.SG[fc % 2], st.CB[coef_slot]), (st.SGC[fc % 2],))
                        tt_("dve", st.h[:, hp, fc, :], ps[:, bu, :], st.sgc[:, fc % 2, :], ALU.mult,
                            (PB[bu], st.SGC[fc % 2]), (st.H[hp],))

                def down(tt, half, si=si, WSt=WSt, nfc=nfc):
                    hp = tt % 2
                    for dl in range(4):
                        dc = half * 4 + dl
                        mm(ps[:, 4 + dl, :], [(st.wd[si][:, fc, dc * 128:(dc + 1) * 128], st.h[:, hp, fc, :]) for fc in range(nfc)],
                           (WSt, st.H[hp]), (PB[4 + dl],))
                    Sd.op("dve", lambda e, o=xacc[:, half * 4:half * 4 + 4, tsl(tt)], a=ps[:, 4:8, :]:
                          e.tensor_tensor(o, a, o, ALU.add), (PB[4], PB[5], PB[6], PB[7], XA[tt]), (XA[tt],))

                for tt in range(NT + 1):
                    for fc in range(nfc):
                        if tt < NT:
                            gu(tt, fc)
                        if tt > 0 and fc == (nfc // 2) - 1:
                            down(tt - 1, 0)
                    if tt > 0:
                        down(tt - 1, 1)
                f0 += fb

        def dense_ffn(l, f):
            st = ffn_setup()
            ffn_pass(st, ffn_w_gate[f], ffn_w_up[f], ffn_w_down[f], DFF, None)
            Sd.barrier()

        def moe_ffn(l, f):
            sparse = (MOE_MODE == "cap")
            car = Carver()
            posm = car.take([128, 16, NE], F32)
            lg = car.take([128, 16, NE], F32)
            m1 = car.take([128, 16], F32)
            m2 = car.take([128, 16], F32)
            mk1 = car.take([128, 16, NE], F32)
            mk2 = car.take([128, 16, NE], F32)
            l2 = car.take([128, 16, NE], F32)
            w1 = car.take([128, 16], F32)
            w2 = car.take([128, 16], F32)
            coef = car.take([128, 16, NE], F32)
            coefT = car.take([NE, S], F32)
            R = Tk("route")
            fl = lambda a: a.rearrange("p a b -> p (a b)")
            for tc_ in range(16):
                tt = tc_ // 4
                mm(ps[:, 0, tc_ * NE:(tc_ + 1) * NE],
                   [(xacc[:, kc, tc_ * 128:(tc_ + 1) * 128], router[:, f, kc, :]) for kc in range(8)],
                   (XA[tt], CONST), (PB[0],))
            Sd.op("act", lambda e: e.mul(fl(lg), ps[:, 0, 0:16 * NE], 1.0 / ALPHA), (PB[0],), (R,))
            Sd.op("dve", lambda e: e.tensor_reduce(m1[:], lg[:], AX.X, ALU.max), (R,), (R,))
            tt_("dve", mk1[:], lg[:], m1[:].unsqueeze(2).to_broadcast([128, 16, NE]), ALU.is_equal, (R,), (R,))
            stt("dve", l2[:], mk1[:], -1e30, lg[:], ALU.mult, ALU.add, (R,), (R,))
            Sd.op("dve", lambda e: e.tensor_reduce(m2[:], l2[:], AX.X, ALU.max), (R,), (R,))
            tt_("dve", mk2[:], l2[:], m2[:].unsqueeze(2).to_broadcast([128, 16, NE]), ALU.is_equal, (R,), (R,))
            if sparse:
                msk = car.take([128, 16, NE], F32)
                tot = car.take([128, 16, NE], F32)
                off = car.take([128, 16, NE], F32)
                pos = car.take([128, 16, NE], F32)
                posT = car.take([NE, S], F32)
                lst = car.take([128, 128], F32)
                dma("sp", lst, c_lstrict, ch_misc[2], (), (R,))
                tt_("dve", msk[:], mk1[:], mk2[:], ALU.add, (R,), (R,))
                mm(ps[:, 5, 0:128], [(lst, fl(msk))], (R,), (PB[5],))
                mm(ps[:, 6, 0:128], [(ones32[:], fl(msk))], (R, CONST), (PB[6],))
                Sd.op("act", lambda e: e.copy(fl(tot), ps[:, 6, 0:128]), (PB[6],), (R,))
                mset("dve", off[:, 0, :], 0.0, (R,))
                for tc_ in range(1, 16):
                    tt_("dve", off[:, tc_, :], off[:, tc_ - 1, :], tot[:, tc_ - 1, :], ALU.add, (R,), (R,))
                tt_("dve", fl(pos), ps[:, 5, 0:128], fl(off), ALU.add, (PB[5], R), (R,))
                stt("dve", posm[:], pos[:], 1.0, msk[:], ALU.add, ALU.mult, (R,), (R,))
                ts_("dve", posm[:], posm[:], -1.0, None, ALU.add, None, (R,), (R,))
                for tc_ in range(16):
                    bank = 4 + tc_ // 4
                    sub = (tc_ % 4) * 128
                    Sd.op("pe", lambda e, o=ps[0:NE, bank, sub:sub + 128], i=posm[:, tc_, :]: e.transpose(o, i, ident[:]),
                          (R, CONST), (PB[bank],))
                for q in range(4):
                    cp("dve", posT[:, q * 512:(q + 1) * 512], ps[0:NE, 4 + q, :], (PB[4 + q],), (R,))
                dma("sp", pos_scr, posT, ch_pos, (R,), (R,))
            tt_("dve", w2[:], m2[:], m1[:], ALU.subtract, (R,), (R,))
            act(w2[:], w2[:], AF.Exp, (R,), (R,))
            ts_("dve", w1[:], w2[:], 1.0, None, ALU.add, None, (R,), (R,))
            Sd.op("dve", lambda e: e.reciprocal(w1[:], w1[:]), (R,), (R,))
            ts_("dve", w2[:], w1[:], -1.0, 1.0, ALU.mult, ALU.add, (R,), (R,))
            tt_("dve", mk1[:], mk1[:], w1[:].unsqueeze(2).to_broadcast([128, 16, NE]), ALU.mult, (R,), (R,))
            tt_("dve", mk2[:], mk2[:], w2[:].unsqueeze(2).to_broadcast([128, 16, NE]), ALU.mult, (R,), (R,))
            tt_("dve", coef[:], mk1[:], mk2[:], ALU.add, (R,), (R,))
            for tc_ in range(16):
                bank = 1 + tc_ // 4
                sub = (tc_ % 4) * 128
                Sd.op("pe", lambda e, o=ps[0:NE, bank, sub:sub + 128], i=coef[:, tc_, :]: e.transpose(o, i, ident[:]),
                      (R, CONST), (PB[bank],))
            for q in range(4):
                Sd.op("act", lambda e, o=coefT[:, q * 512:(q + 1) * 512], i=ps[0:NE, 1 + q, :]: e.copy(o, i), (PB[1 + q],), (R,))
            dma("sp", coef_scr, coefT, ch_coef, (R,), (R,))
            Sd.barrier()
            if not sparse:
                st = ffn_setup()
                for ex in range(NE):
                    cs = ex % 2
                    dma("sp", st.cb[:, cs, :], coef_scr[ex:ex + 1, :].to_broadcast([128, S]), ch_cb[cs], (R,), (st.CB[cs],))
                    ffn_pass(st, moe_w_gate[f, ex], moe_w_up[f, ex], moe_w_down[f, ex], DFFE, cs)
                Sd.barrier()
                return
            carD = Carver()
            posm = carD.take([128, 16, NE], F32)
            xtm = carD.take([128, 16, D], BF16)
            Pb = [carD.take([128, 16, CAP], BF16) for _ in range(2)]
            xds = [carD.take([128, 8, CAP], BF16) for _ in range(2)]
            iot = carD.take([128, CAP], F32)
            XTM, IOT, XDSCR = Tk("xtm"), Tk("iot"), Tk("xdscr")
            PTK = [Tk("p0"), Tk("p1")]
            XDS = [Tk("xds0"), Tk("xds1")]
            dma("sp", iot, c_iota, ch_misc[3], (), (IOT,))
            for tc_ in range(16):
                bank = tc_ % 2
                psb = ps[:, bank, :].bitcast(BF16)
                for kc in range(8):
                    Sd.op("pe", lambda e, o=psb[:, kc * 128:(kc + 1) * 128], i=xb[:, kc, tc_ * 128:(tc_ + 1) * 128]:
                          e.transpose(o, i, identb[:]), (XB[tc_ // 4], CONST), (PB[bank],))
                if tc_ % 2 == 0:
                    act(xtm[:, tc_, :], psb, AF.Copy, (PB[bank],), (XTM,))
                else:
                    cp("dve", xtm[:, tc_, :], psb, (PB[bank],), (XTM,))
            for ex in range(NE):
                pi = ex % 2
                for tc_ in range(16):
                    ts_("dve", Pb[pi][:, tc_, :], iot, posm[:, tc_, ex:ex + 1], None, ALU.is_equal, None, (IOT, R), (PTK[pi],))
                for dc in range(8):
                    b0 = 2 + (dc % 3) * 2
                    for ti, (s0, sn) in enumerate(STILES):
                        mm(ps[:, b0 + ti, 0:sn],
                           [(xtm[:, tc_, dc * 128:(dc + 1) * 128], Pb[pi][:, tc_, s0:s0 + sn]) for tc_ in range(16)],
                           (XTM, PTK[pi]), (PB[b0 + ti],))
                    act(xds[pi][:, dc, 0:512], ps[:, b0, 0:512], AF.Copy, (PB[b0],), (XDS[pi],))
                    cp("dve", xds[pi][:, dc, 512:CAP], ps[:, b0 + 1, 0:CAP - 512], (PB[b0 + 1],), (XDS[pi],))
                dma("sp", xd_scr[ex].rearrange("(c p) s -> p c s", p=128), xds[pi], ch_xd[pi], (XDS[pi],), (XDSCR,))
            Sd.barrier()
            carE = Carver()
            xd = [carE.take([128, 8, CAP], BF16) for _ in range(2)]
            PT = carE.take([128, NSC, S], BF16)
            yd = carE.take([128, NSC, D], F32)
            ydb = carE.take([128, NSC, D], BF16)
            wg = [carE.take([128, 8, 256], BF16) for _ in range(2)]
            wu = [carE.take([128, 8, 256], BF16) for _ in range(2)]
            wd = [carE.take([128, 2, D], BF16) for _ in range(2)]
            carX = Carver(base=xb[:].rearrange("p a b -> p (a b)"), nbytes=32 * 1024)
            hT = carX.take([128, 2, 2, CAP], BF16)
            sg = carX.take([128, 2, CAP], F32)
            pcb = carX.take([128, S], F32)
            wg.append(carX.take([128, 8, 256], BF16))
            wu.append(carX.take([128, 8, 256], BF16))
            wd.append(carX.take([128, 2, D], BF16))
            XD = [Tk("xd0"), Tk("xd1")]
            WG = [Tk("wg%d" % i) for i in range(3)]
            WU = [Tk("wu%d" % i) for i in range(3)]
            WD = [Tk("wd%d" % i) for i in range(3)]
            HT = [Tk("ht0"), Tk("ht1")]
            SG = [Tk("sg0"), Tk("sg1")]
            PTT, YD, YDB, PCB = Tk("pt"), Tk("yd"), Tk("ydb"), Tk("pcb")
            state = {"blk": 0, "r": 0}

            def down_part(prev, half):
                si, hp = prev
                for sc in range(NSC):
                    bank = 4 + state["r"] % 4
                    state["r"] += 1
                    mm(ps[:, bank, :],
                       [(hT[:, hp, fc, sc * 128:(sc + 1) * 128], wd[si][:, fc, half * 512:(half + 1) * 512]) for fc in range(2)],
                       (WD[si], HT[hp]), (PB[bank],))
                    ydv = yd[:, sc, half * 512:(half + 1) * 512]
                    tt_("dve", ydv, ps[:, bank, :], ydv, ALU.add, (PB[bank], YD), (YD,))

            for ex in range(NE):
                xi = ex % 2
                dma("sp", xd[xi], xd_scr[ex].rearrange("(c p) s -> p c s", p=128), ch_xdl[xi], (XDSCR,), (XD[xi],))
                dma("sp", pcb, pos_scr[ex:ex + 1, :].to_broadcast([128, S]), ch_cb[0], (R,), (PCB,))
                mset("pool", yd[:].rearrange("p a b -> p (a b)"), 0.0, (YD,))
                for sc in range(NSC):
                    ts_("dve", PT[:, sc, :], pcb, slotid[:, sc:sc + 1], None, ALU.is_equal, None, (PCB, CONST), (PTT,))
                dma("sp", pcb, coef_scr[ex:ex + 1, :].to_broadcast([128, S]), ch_cb[1], (R,), (PCB,))
                for sc in range(NSC):
                    tt_("pool", PT[:, sc, :], PT[:, sc, :], pcb, ALU.mult, (PCB, PTT), (PTT,))
                prev = None
                for b in range(DFFE // 256):
                    si = state["blk"] % 3
                    hp = state["blk"] % 2
                    state["blk"] += 1
                    f0 = b * 256
                    ch = ch_w[si * 3:si * 3 + 3]
                    dma("pool", wg[si], moe_w_gate[f, ex][:, f0:f0 + 256].rearrange("(c p) e -> p c e", p=128), ch[0], (), (WG[si],))
                    dma("pool", wu[si], moe_w_up[f, ex][:, f0:f0 + 256].rearrange("(c p) e -> p c e", p=128), ch[1], (), (WU[si],))
                    dma("pool", wd[si], moe_w_down[f, ex][f0:f0 + 256, :].rearrange("(c p) e -> p c e", p=128), ch[2], (), (WD[si],))
                    for fc in range(2):
                        for ti, (s0, sn) in enumerate(STILES):
                            mm(ps[:, ti, 0:sn],
                               [(wg[si][:, kc, fc * 128:(fc + 1) * 128], xd[xi][:, kc, s0:s0 + sn]) for kc in range(8)],
                               (WG[si], XD[xi]), (PB[ti],))
                            mm(ps[:, 2 + ti, 0:sn],
                               [(wu[si][:, kc, fc * 128:(fc + 1) * 128], xd[xi][:, kc, s0:s0 + sn]) for kc in range(8)],
                               (WU[si], XD[xi]), (PB[2 + ti],))
                        for ti, (s0, sn) in enumerate(STILES):
                            act(sg[:, fc, s0:s0 + sn], ps[:, ti, 0:sn], AF.Silu, (PB[ti],), (SG[fc],))
                        for ti, (s0, sn) in enumerate(STILES):
                            tt_("dve", hT[:, hp, fc, s0:s0 + sn], ps[:, 2 + ti, 0:sn], sg[:, fc, s0:s0 + sn], ALU.mult,
                                (PB[2 + ti], SG[fc]), (HT[hp],))
                        if prev is not None:
                            down_part(prev, fc)
                    prev = (si, hp)
                down_part(prev, 0)
                down_part(prev, 1)
                act(ydb[:].rearrange("p a b -> p (a b)"), yd[:].rearrange("p a b -> p (a b)"), AF.Copy, (YD,), (YDB,))
                for tt in range(NT):
                    for dc in range(8):
                        bank = 4 + state["r"] % 4
                        state["r"] += 1
                        mm(ps[:, bank, :], [(ydb[:, sc, dc * 128:(dc + 1) * 128], PT[:, sc, tsl(tt)]) for sc in range(NSC)],
                           (YDB, PTT), (PB[bank],))
                        tt_("dve", xacc[:, dc, tsl(tt)], ps[:, bank, :], xacc[:, dc, tsl(tt)], ALU.add,
                            (PB[bank], XA[tt]), (XA[tt],))
            Sd.barrier()

        def ln2(l, final):
            car = Carver()
            sq = [car.take([128, 8, TS], F32) for _ in range(2)]
            tmp = [car.take([128, 4, TS], F32) for _ in range(2)]
            SQ = [Tk("sq0"), Tk("sq1")]
            TMP = [[Tk("tmp%d" % i) for i in range(4)] for _ in range(2)]
            for tt in range(NT):
                layer_norm(tt, l, 1, sq[tt % 2], tmp[tt % 2], SQ[tt % 2], TMP[tt % 2], final, (tt % 4) * 2, (tt % 4) * 2 + 1)
            Sd.barrier()

        for s in range(nseq):
            for tt in range(NT):
                dma("sp", xacc[:, :, tsl(tt)], xT[s][:, tsl(tt)].rearrange("(c p) t -> p c t", p=128), ch_x[tt], (), (XA[tt],))
            for tt in range(NT):
                cp("dve", xb[:, :, tsl(tt)], xacc[:, :, tsl(tt)], (XA[tt],), (XB[tt],))
                Sd.op("act", lambda e, v=xacc[:, :, tsl(tt)]: e.mul(v, v, ALPHA), (XA[tt], XB[tt]), (XA[tt],))
            car = Carver()
            memb = car.take([128, 8, 256], BF16)
            wkv = car.take([128, 8, 512], BF16)
            MB, WK = Tk("memb"), Tk("wkv")
            dma("pool", memb, memT[s].rearrange("(c p) m -> p c m", p=128), ch_misc[0], (), (MB,))
            dma("pool", wkv, w_mem_kv.rearrange("(c p) e -> p c e", p=128), ch_misc[1], (), (WK,))
            for ec in range(2):
                mm(ps[:, ec, 0:256], [(wkv[:, kc, ec * 128:(ec + 1) * 128], memb[:, kc, :]) for kc in range(8)],
                   (MB, WK), (PB[ec],))
                act(kTpad[0:64, 2 * ec, :], ps[0:64, ec, 0:256], AF.Copy, (PB[ec],), (KV,))
                cp("dve", kTpad[64:128, 2 * ec + 1, :], ps[64:128, ec, 0:256], (PB[ec],), (KV,))
            for mc in range(2):
                mm(ps[:, 2 + mc, 0:256], [(memb[:, kc, mc * 128:(mc + 1) * 128], wkv[:, kc, 256:512]) for kc in range(8)],
                   (MB, WK), (PB[2 + mc],))
                for h in range(4):
                    o = vpad[:, h, mc, (h % 2) * 64:(h % 2) * 64 + 64]
                    i = ps[:, 2 + mc, h * 64:(h + 1) * 64]
                    if h % 2 == 0:
                        act(o, i, AF.Copy, (PB[2 + mc],), (KV,))
                    else:
                        cp("dve", o, i, (PB[2 + mc],), (KV,))
            Sd.barrier()
            for li, l in enumerate(layers):
                kind, j = l % 3, l // 3
                if kind == 0:
                    mixer_a(l, j)
                elif kind == 1:
                    mixer_b(l, j)
                else:
                    mixer_c(l, j)
                if l % 2 == 0:
                    dense_ffn(l, l // 2)
                else:
                    moe_ffn(l, l // 2)
                last = (li == len(layers) - 1)
                ln2(l, last)
                if debug and not last:
                    for tt in range(NT):
                        dma("sp", dbg_out[li, s][:, tsl(tt)].rearrange("(c p) t -> p c t", p=128), xacc[:, :, tsl(tt)],
                            ch_out[tt], (XA[tt],), ())
            for tt in range(NT):
                dma("sp", yT[s][:, tsl(tt)].rearrange("(c p) t -> p c t", p=128), xacc[:, :, tsl(tt)],
                    ch_out[tt], (XA[tt],), ())

        Sd.finalize()
        for c in Sd.chans:
            if c.count > 0:
                c.sem = es.enter_context(nc.semaphore("c_" + c.name))
        with nc.Block() as block:
            Sd.emit(nc, block, engsem)
    return nc


def _alibi_slopes(n):
    return 2.0 ** (-8.0 * np.arange(1, n + 1, dtype=np.float64) / n)


def _host_consts(ln_g, ln_b, a_conv_w, c_pool_w, c_pool_scale):
    c = {}
    lnp = np.zeros((128, 128), np.float32)
    lnp[:, 0:64] = ln_g.reshape(8, 8, 128).transpose(2, 0, 1).reshape(128, 64)
    lnp[:, 64:128] = ln_b.reshape(8, 8, 128).transpose(2, 0, 1).reshape(128, 64)
    c["c_lnp"] = lnp
    c["c_convw"] = np.ascontiguousarray(a_conv_w.reshape(6, 6, 128).transpose(2, 0, 1).reshape(128, 36))
    c["c_pscale"] = np.ascontiguousarray(c_pool_scale.reshape(6, 128).T)
    c["c_ident"] = np.eye(128, dtype=np.float32)
    slopes = _alibi_slopes(12).reshape(3, 4)
    p = np.arange(128)[:, None]
    jl = np.arange(256)[None, :]
    delta = jl - 64 - p
    eb = np.zeros((128, 12, 256), np.float32)
    for g, (win, dil) in enumerate(DIL):
        for h in range(4):
            v = np.exp(-slopes[g, h] * np.abs(delta) * dil)
            eb[:, g * 4 + h, :] = np.where(np.abs(delta) <= 64, v, 0.0)
    c["c_expb"] = eb.reshape(128, 12 * 256)
    ed = np.ones((128, 4, 16), np.float32)
    for wi, w in enumerate(POOLW):
        for t in range(8):
            lo, hi = max(t - w // 2, 0), min(t + w // 2 - 1, S - 1)
            ed[:, wi, t] = w / float(hi - lo + 1)
            t2 = S - 8 + t
            lo, hi = max(t2 - w // 2, 0), min(t2 + w // 2 - 1, S - 1)
            ed[:, wi, 8 + t] = w / float(hi - lo + 1)
    c["c_edge"] = ed.reshape(128, 64)
    c["c_lstrict"] = np.triu(np.ones((128, 128), np.float32), 1)
    c["c_iota"] = np.ascontiguousarray(np.broadcast_to(np.arange(CAP, dtype=np.float32)[None, :], (128, CAP)))
    c["c_slotid"] = (np.arange(128, dtype=np.float32)[:, None] + 128.0 * np.arange(8, dtype=np.float32)[None, :])
    pw = np.zeros((MIXW, MIXW), np.float32)
    for g in range(4):
        pw[g * 192:(g + 1) * 192, g * 192:(g + 1) * 192] = c_pool_w[0, g]
    c["pwfull"] = pw
    return c


LAYERS = [0, 1, 2, 3]
NSEQ = 2
_CACHE = {}


def kernel(x, mem, w_mem_kv, a_w_in, a_conv_w, a_w_out, b_w_in, b_w_out,
           c_w_in, c_pool_w, c_pool_scale, c_w_out, ln_g, ln_b,
           ffn_w_gate, ffn_w_up, ffn_w_down, moe_router, moe_w_gate, moe_w_up, moe_w_down,
           _layers=None, _debug=False, _ncores=8):
    layers = LAYERS if _layers is None else _layers
    f32 = lambda a: np.ascontiguousarray(np.asarray(a, dtype=np.float32))
    x = f32(x)
    mem = f32(mem)
    consts = _host_consts(f32(ln_g), f32(ln_b), f32(a_conv_w), f32(c_pool_w), f32(c_pool_scale))
    shared = {
        "w_mem_kv": f32(w_mem_kv), "a_w_in": f32(a_w_in), "a_w_out": f32(a_w_out),
        "b_w_in": f32(b_w_in), "b_w_out": f32(b_w_out), "c_w_in": f32(c_w_in), "c_w_out": f32(c_w_out),
        "ffn_w_gate": f32(ffn_w_gate), "ffn_w_up": f32(ffn_w_up), "ffn_w_down": f32(ffn_w_down),
        "moe_router": f32(moe_router), "moe_w_gate": f32(moe_w_gate), "moe_w_up": f32(moe_w_up),
        "moe_w_down": f32(moe_w_down),
    }
    shared.update(consts)
    key = (tuple(layers), NSEQ, _debug)
    if key not in _CACHE:
        _CACHE[key] = build_program(layers, NSEQ, _debug)
    nc = _CACHE[key]
    in_maps = []
    for c in range(_ncores):
        m = dict(shared)
        m["xT"] = np.ascontiguousarray(x[c * NSEQ:(c + 1) * NSEQ].transpose(0, 2, 1))
        m["memT"] = np.ascontiguousarray(mem[c * NSEQ:(c + 1) * NSEQ].transpose(0, 2, 1))
        in_maps.append(m)
    res = run_bass_kernel_spmd(nc, in_maps, core_ids=list(range(_ncores)))
    out = np.empty((_ncores * NSEQ, S, D), np.float32)
    for c in range(_ncores):
        out[c * NSEQ:(c + 1) * NSEQ] = res.results[c]["yT"].transpose(0, 2, 1)
    if _debug:
        return out, [r.get("dbg") for r in res.results]
    return out
```

```python
import contextlib
import numpy as np
import concourse.bass as bass
import concourse.mybir as mybir
from concourse.bass_utils import run_bass_kernel_spmd

F32 = mybir.dt.float32
BF16 = mybir.dt.bfloat16
AF = mybir.ActivationFunctionType
ALU = mybir.AluOpType
AX = mybir.AxisListType

D = 1024
S = 2048
NT = 4
TS = 512
DEPTH = 4
ALPHA = float((2 * DEPTH) ** 0.25)
LN_EPS = 1e-5
MIXW = 768
DFF = 2816
DFFE = 3584
NE = 8
DIL = ((128, 1), (512, 4), (2048, 16))
POOLW = (2, 4, 8, 16)
ARENA_BYTES = 96 * 1024
MOE_MODE = "cap"
CAP = 640
NSC = CAP // 128
STILES = ((0, 512), (512, CAP - 512))


class Tk:
    __slots__ = ("name", "w", "r", "rd")

    def __init__(self, name=""):
        self.name = name
        self.w = None
        self.r = {}
        self.rd = []


class Chan:
    __slots__ = ("sem", "count", "name")

    def __init__(self, name):
        self.name = name
        self.sem = None
        self.count = 0


class Op:
    __slots__ = ("eng", "fn", "deps", "signal", "sigval", "kind", "chan", "dmaval")


ENGS = ("pe", "act", "dve", "pool", "sp")


class Sched:
    def __init__(self):
        self.ops = []
        self.extra = {e: [] for e in ENGS}
        self.last = {e: None for e in ENGS}
        self.chans = []
        self.last_dma = {}

    def chan(self, name):
        c = Chan(name)
        self.chans.append(c)
        return c

    def op(self, eng, fn, reads=(), writes=(), chan=None):
        o = Op()
        o.eng = eng
        o.fn = fn
        o.signal = False
        o.sigval = None
        o.kind = "d" if chan is not None else "c"
        o.chan = chan
        o.dmaval = None
        deps = {}
        for t in reads:
            if t.w is not None:
                deps[id(t.w)] = t.w
        for t in writes:
            if t.w is not None:
                deps[id(t.w)] = t.w
            for r in t.r.values():
                deps[id(r)] = r
            for r in t.rd:
                deps[id(r)] = r
        for d in self.extra[eng]:
            deps[id(d)] = d
        self.extra[eng] = []
        dl = []
        for d in deps.values():
            if d is o:
                continue
            if d.kind == "c" and o.kind == "c" and d.eng == "pe" and eng == "pe":
                continue
            d.signal = True
            dl.append(d)
        o.deps = dl
        if chan is not None:
            chan.count += 16
            o.dmaval = chan.count
            self.last_dma[id(chan)] = o
        for t in reads:
            if o.kind == "d":
                t.rd.append(o)
            else:
                t.r[eng] = o
        for t in writes:
            t.w = o
            t.r = {}
            t.rd = []
        if o.kind == "c":
            self.last[eng] = o
        self.ops.append(o)
        return o

    def barrier(self):
        deps = [o for o in self.last.values() if o is not None]
        deps += list(self.last_dma.values())
        for e in ENGS:
            self.extra[e] = list(deps)

    def finalize(self):
        cnt = {e: 0 for e in ENGS}
        for o in self.ops:
            if o.kind == "c" and o.signal:
                cnt[o.eng] += 1
                o.sigval = cnt[o.eng]
        self.sigcounts = cnt

    def emit(self, nc, block, engsem):
        handles = {"pe": "tensor", "act": "scalar", "dve": "vector", "pool": "gpsimd", "sp": "sync"}
        final = [(c.sem, c.count) for c in self.chans if c.count > 0]

        def run(engname):
            def body(e):
                waited = {}
                for o in self.ops:
                    if o.eng != engname:
                        continue
                    for d in o.deps:
                        if d.kind == "d":
                            sem, val = d.chan.sem, d.dmaval
                        else:
                            sem, val = engsem[d.eng], d.sigval
                        k = id(sem)
                        if waited.get(k, 0) >= val:
                            continue
                        e.wait_ge(sem, val)
                        waited[k] = val
                    ins = o.fn(e)
                    if o.kind == "d":
                        ins.then_inc(o.chan.sem, 16)
                    elif o.signal:
                        ins.then_inc(engsem[o.eng], 1)
                if engname == "sp":
                    for sem, val in final:
                        e.wait_ge(sem, val)
            return body

        for en in ENGS:
            getattr(block, handles[en])(run(en))


def build_program(layers, nseq, debug=False):
    nc = bass.Bass("TRN2", target_bir_lowering=False)
    Sd = Sched()

    def dram_in(name, shape):
        return nc.dram_tensor(name, list(shape), F32, kind="ExternalInput").ap()

    xT = dram_in("xT", [nseq, D, S])
    memT = dram_in("memT", [nseq, D, 256])
    w_mem_kv = dram_in("w_mem_kv", [D, 512])
    a_w_in = dram_in("a_w_in", [2, D, 2560])
    a_w_out = dram_in("a_w_out", [2, D, D])
    b_w_in = dram_in("b_w_in", [1, D, 2560])
    b_w_out = dram_in("b_w_out", [1, 512, D])
    c_w_in = dram_in("c_w_in", [1, D, D])
    c_w_out = dram_in("c_w_out", [1, D, D])
    pwfull = dram_in("pwfull", [MIXW, MIXW])
    ffn_w_gate = dram_in("ffn_w_gate", [2, D, DFF])
    ffn_w_up = dram_in("ffn_w_up", [2, D, DFF])
    ffn_w_down = dram_in("ffn_w_down", [2, DFF, D])
    moe_router = dram_in("moe_router", [2, D, NE])
    moe_w_gate = dram_in("moe_w_gate", [2, NE, D, DFFE])
    moe_w_up = dram_in("moe_w_up", [2, NE, D, DFFE])
    moe_w_down = dram_in("moe_w_down", [2, NE, DFFE, D])
    c_lnp = dram_in("c_lnp", [128, 128])
    c_convw = dram_in("c_convw", [128, 36])
    c_pscale = dram_in("c_pscale", [128, 6])
    c_ident = dram_in("c_ident", [128, 128])
    c_expb = dram_in("c_expb", [128, 12 * 256])
    c_edge = dram_in("c_edge", [128, 64])
    c_lstrict = dram_in("c_lstrict", [128, 128])
    c_iota = dram_in("c_iota", [128, CAP])
    c_slotid = dram_in("c_slotid", [128, 8])
    yT = nc.dram_tensor("yT", [nseq, D, S], F32, kind="ExternalOutput").ap()
    coef_scr = nc.dram_tensor("coef_scr", [NE, S], F32, kind="Internal").ap()
    pos_scr = nc.dram_tensor("pos_scr", [NE, S], F32, kind="Internal").ap()
    xd_scr = nc.dram_tensor("xd_scr", [NE, D, CAP], BF16, kind="Internal").ap()
    dbg_out = None
    if debug:
        dbg_out = nc.dram_tensor("dbg", [len(layers), nseq, D, S], F32, kind="ExternalOutput").ap()

    es = contextlib.ExitStack()
    with es:
        def sb(name, shape, dt):
            return es.enter_context(nc.sbuf_tensor(name, list(shape), dt))

        xacc = sb("xacc", [128, 8, S], F32)
        xb = sb("xb", [128, 8, S], BF16)
        lnp = sb("lnp", [128, 256], F32)
        convw = sb("convw", [128, 36], F32)
        pscale = sb("pscale", [128, 6], F32)
        router = sb("router", [128, 2, 8, NE], F32)
        ident = sb("ident", [128, 128], F32)
        ones32 = sb("ones32", [128, 128], F32)
        onespad = sb("onespad", [128, 2, 128], BF16)
        kTpad = sb("kTpad", [128, 4, 256], BF16)
        vpad = sb("vpad", [128, 4, 2, 128], BF16)
        expb = sb("expb", [128, 12, 256], BF16)
        edge = sb("edge", [128, 4, 16], F32)
        slotid = sb("slotid", [128, 8], F32)
        identb = sb("identb", [128, 128], BF16)
        arena = sb("arena", [128, ARENA_BYTES // 2], BF16)
        ps = es.enter_context(nc.psum_tensor("ps", [128, 8, 512], F32))

        engsem = {e: es.enter_context(nc.semaphore("s_" + e)) for e in ENGS}

        XA = [Tk("xa%d" % t) for t in range(NT)]
        XB = [Tk("xb%d" % t) for t in range(NT)]
        PB = [Tk("ps%d" % b) for b in range(8)]
        CONST = Tk("const")
        KV = Tk("kv")
        ch_const = Sd.chan("const")
        ch_x = [Sd.chan("x%d" % t) for t in range(NT)]
        ch_out = [Sd.chan("o%d" % t) for t in range(NT)]
        ch_w = [Sd.chan("w%d" % i) for i in range(9)]
        ch_misc = [Sd.chan("m%d" % i) for i in range(4)]
        ch_coef = Sd.chan("coef")
        ch_cb = [Sd.chan("cb%d" % i) for i in range(2)]
        ch_xd = [Sd.chan("xd%d" % i) for i in range(2)]
        ch_xdl = [Sd.chan("xdl%d" % i) for i in range(2)]
        ch_pos = Sd.chan("pos")

        def tsl(tt):
            return slice(tt * TS, (tt + 1) * TS)

        class Carver:
            def __init__(self, base=None, nbytes=ARENA_BYTES):
                self.off = 0
                self.base = arena if base is None else base
                self.nbytes = nbytes

            def take(self, shape, dt):
                esz = 4 if dt == F32 else 2
                n = 1
                for s_ in shape[1:]:
                    n *= s_
                nbytes = n * esz
                assert self.off % 4 == 0
                assert self.off + nbytes <= self.nbytes, (self.off, nbytes)
                v = self.base[:, self.off // 2:(self.off + nbytes) // 2]
                if dt == F32:
                    v = v.bitcast(F32)
                self.off += nbytes
                if shape[0] != 128:
                    v = v[0:shape[0]]
                if len(shape) == 3:
                    v = v.rearrange("p (a b) -> p a b", a=shape[1])
                elif len(shape) == 4:
                    v = v.rearrange("p (a b c) -> p a b c", a=shape[1], b=shape[2])
                return v

        def dma(eng, out, in_, chan, reads=(), writes=()):
            Sd.op(eng, lambda e, o=out, i=in_: e.dma_start(out=o, in_=i), reads, writes, chan=chan)

        def mm(out, pairs, reads, writes):
            def fn(e, out=out, pairs=pairs):
                n = len(pairs)
                ins = None
                for i, (l, r) in enumerate(pairs):
                    ins = e.matmul(out, l, r, start=(i == 0), stop=(i == n - 1))
                return ins
            Sd.op("pe", fn, reads, writes)

        def act(out, in_, func, reads, writes, bias=None, scale=None):
            kw = {}
            if bias is not None:
                kw["bias"] = bias
            if scale is not None:
                kw["scale"] = scale
            Sd.op("act", lambda e, o=out, i=in_, f=func, kw=kw: e.activation(o, i, f, **kw), reads, writes)

        def tt_(eng, out, in0, in1, op, reads, writes):
            Sd.op(eng, lambda e, o=out, a=in0, b=in1, op=op: e.tensor_tensor(o, a, b, op), reads, writes)

        def ts_(eng, out, in0, s1, s2, op0, op1, reads, writes):
            if op1 is None:
                Sd.op(eng, lambda e, o=out, a=in0, s1=s1, op0=op0: e.tensor_scalar(o, a, s1, None, op0), reads, writes)
            else:
                Sd.op(eng, lambda e, o=out, a=in0, s1=s1, s2=s2, op0=op0, op1=op1:
                      e.tensor_scalar(o, a, s1, s2, op0, op1), reads, writes)

        def stt(eng, out, in0, scalar, in1, op0, op1, reads, writes):
            Sd.op(eng, lambda e, o=out, a=in0, s=scalar, b=in1, op0=op0, op1=op1:
                  e.scalar_tensor_tensor(o, a, s, b, op0, op1), reads, writes)

        def cp(eng, out, in_, reads, writes):
            Sd.op(eng, lambda e, o=out, i=in_: e.tensor_copy(o, i), reads, writes)

        def mset(eng, ap, val, writes):
            Sd.op(eng, lambda e, a=ap, v=val: e.memset(a, v), (), writes)

        dma("sp", lnp[:, 0:128], c_lnp, ch_const, (), (CONST,))
        dma("sp", convw[:], c_convw, ch_const, (), (CONST,))
        dma("sp", pscale[:], c_pscale, ch_const, (), (CONST,))
        dma("sp", ident[:], c_ident, ch_const, (), (CONST,))
        dma("pool", expb[:].rearrange("p a b -> p (a b)"), c_expb, ch_const, (), (CONST,))
        dma("sp", edge[:].rearrange("p a b -> p (a b)"), c_edge, ch_const, (), (CONST,))
        for f in range(2):
            dma("sp", router[:, f], moe_router[f].rearrange("(c p) e -> p c e", p=128), ch_const, (), (CONST,))
        dma("sp", slotid[:], c_slotid, ch_const, (), (CONST,))
        Sd.op("act", lambda e: e.mul(lnp[:, 128:256], lnp[:, 0:128], ALPHA), (CONST,), (CONST,))
        cp("dve", identb[:], ident[:], (CONST,), (CONST,))
        mset("dve", ones32[:], 1.0, (CONST,))
        mset("dve", onespad[:], 0.0, (CONST,))
        mset("dve", onespad[:, 0, 0:64], 1.0, (CONST,))
        mset("dve", onespad[:, 1, 64:128], 1.0, (CONST,))
        mset("dve", kTpad[:], 0.0, (KV,))
        mset("dve", vpad[:], 0.0, (KV,))

        def lncol(l, j, c):
            return (l * 2 + j) * 8 + c

        def layer_norm(tt, l, j, sq, tmp, SQ, TMP, final, bank_a, bank_b):
            v = xacc[:, :, tsl(tt)]
            act(sq, v, AF.Square, (XA[tt],), (SQ,))
            mm(ps[:, bank_a, :], [(ones32[:], xacc[:, c, tsl(tt)]) for c in range(8)],
               (XA[tt], CONST), (PB[bank_a],))
            mm(ps[:, bank_b, :], [(ones32[:], sq[:, c, :]) for c in range(8)], (SQ, CONST), (PB[bank_b],))
            mean, msq, var, rstd = tmp[:, 0], tmp[:, 1], tmp[:, 2], tmp[:, 3]
            Sd.op("act", lambda e: e.mul(mean, ps[:, bank_a, :], 1.0 / D), (PB[bank_a],), (TMP[0],))
            tt_("dve", msq, mean, mean, ALU.mult, (TMP[0],), (TMP[1],))
            stt("dve", var, ps[:, bank_b, :], 1.0 / D, msq, ALU.mult, ALU.subtract, (PB[bank_b], TMP[1]), (TMP[2],))
            ts_("dve", var, var, LN_EPS, None, ALU.add, None, (TMP[2],), (TMP[2],))
            act(var, var, AF.Sqrt, (TMP[2],), (TMP[2],))
            Sd.op("dve", lambda e: e.reciprocal(rstd, var), (TMP[2],), (TMP[3],))
            mb = mean.unsqueeze(1).to_broadcast([128, 8, TS])
            rb = rstd.unsqueeze(1).to_broadcast([128, 8, TS])
            tt_("dve", sq, v, mb, ALU.subtract, (XA[tt], TMP[0]), (SQ,))
            tt_("pool", sq, sq, rb, ALU.mult, (SQ, TMP[3]), (SQ,))
            for c in range(8):
                col = lncol(l, j, c)
                g, b_ = lnp[:, col:col + 1], lnp[:, 64 + col:64 + col + 1]
                ga, ba = lnp[:, 128 + col:128 + col + 1], lnp[:, 192 + col:192 + col + 1]
                if final:
                    act(xacc[:, c, tsl(tt)], sq[:, c, :], AF.Identity, (SQ, CONST), (XA[tt],), bias=b_, scale=g)
                else:
                    act(xb[:, c, tsl(tt)], sq[:, c, :], AF.Identity, (SQ, CONST), (XB[tt],), bias=b_, scale=g)
                    act(xacc[:, c, tsl(tt)], sq[:, c, :], AF.Identity, (SQ, CONST), (XA[tt],), bias=ba, scale=ga)

        def load_w_cols(slot, slot_tk, chan, src2d, colgroups):
            off = 0
            for (c0, n) in colgroups:
                dma("pool", slot[:, :, off:off + n], src2d[:, c0:c0 + n].rearrange("(c p) e -> p c e", p=128),
                    chan, (), (slot_tk,))
                off += n

        def mem_attention(qm, QM, memout, MO, pT, PT, rden, RD):
            for tt in range(NT):
                for pr in range(2):
                    bn, bd = 4 + (pr % 2) * 2, 5 + (pr % 2) * 2
                    for hh in range(2):
                        h = 2 * pr + hh
                        b0 = (hh % 2) * 2
                        for mc in range(2):
                            mm(ps[:, b0 + mc, :], [(kTpad[:, h, mc * 128:(mc + 1) * 128], qm[:, pr, tsl(tt)])],
                               (KV, QM[tt]), (PB[b0 + mc],))
                        act(pT[:, hh], ps[:, b0:b0 + 2, :], AF.Exp, (PB[b0], PB[b0 + 1]), (PT[hh],))
                    mm(ps[:, bn, :], [(vpad[:, 2 * pr + hh, mc, :], pT[:, hh, mc, :]) for hh in range(2) for mc in range(2)],
                       (KV, PT[0], PT[1]), (PB[bn],))
                    mm(ps[:, bd, :], [(onespad[:, hh, :], pT[:, hh, mc, :]) for hh in range(2) for mc in range(2)],
                       (CONST, PT[0], PT[1]), (PB[bd],))
                    Sd.op("dve", lambda e, o=rden[:, pr % 2, :], i=ps[:, bd, :]: e.reciprocal(o, i), (PB[bd],), (RD[pr % 2],))
                    tt_("dve", memout[:, pr, tsl(tt)], ps[:, bn, :], rden[:, pr % 2, :], ALU.mult,
                        (PB[bn], RD[pr % 2]), (MO[tt],))

        def outproj_ln(l, wout2d, nmix, cat_aps, CAT, car):
            kc_n = len(cat_aps)
            wo = car.take([128, kc_n, D], BF16)
            WO = Tk("wo")
            dma("pool", wo, wout2d.rearrange("(c p) e -> p c e", p=128), ch_w[0], (), (WO,))
            sq = [car.take([128, 8, TS], F32) for _ in range(2)]
            tmp = [car.take([128, 4, TS], F32) for _ in range(2)]
            SQ = [Tk("sq0"), Tk("sq1")]
            TMP = [[Tk("tmp%d" % i) for i in range(4)] for _ in range(2)]
            for tt in range(NT):
                for dc in range(8):
                    bank = dc % 4
                    mm(ps[:, bank, :], [(wo[:, kc, dc * 128:(dc + 1) * 128], cat_aps[kc][:, tsl(tt)]) for kc in range(kc_n)],
                       (WO,) + tuple(CAT[tt]), (PB[bank],))
                    tt_("dve", xacc[:, dc, tsl(tt)], ps[:, bank, :], xacc[:, dc, tsl(tt)], ALU.add,
                        (PB[bank], XA[tt]), (XA[tt],))
                layer_norm(tt, l, 0, sq[tt % 2], tmp[tt % 2], SQ[tt % 2], TMP[tt % 2], False, 4 + (tt % 2) * 2, 5 + (tt % 2) * 2)

        def inproj_block(slot, SLOT, ncols, consumer):
            nch = ncols // 128
            k = 0
            for tt in range(NT):
                for ec in range(nch):
                    bank = k % 4
                    k += 1
                    mm(ps[:, bank, :], [(slot[:, kc, ec * 128:(ec + 1) * 128], xb[:, kc, tsl(tt)]) for kc in range(8)],
                       (SLOT, XB[tt]), (PB[bank],))
                    consumer(tt, ec, bank)

        def mixer_a(l, jA):
            car = Carver()
            mix = car.take([128, 6, S], BF16)
            memout = car.take([128, 2, S], BF16)
            qm = car.take([128, 2, S], BF16)
            slots = [car.take([128, 8, 384], BF16) for _ in range(2)]
            SL = [Tk("sl0"), Tk("sl1")]
            gcs = car.take([128, 2, TS], F32)
            GCS = [Tk("gcs0"), Tk("gcs1")]
            z = car.take([128, S + 2], F32)
            Z = Tk("z")
            gb = car.take([128, S], BF16)
            GB = Tk("gb")
            tmpc = car.take([128, 2, S], F32)
            TC = [Tk("tc0"), Tk("tc1")]
            pT = car.take([128, 2, 2, TS], BF16)
            PT = [Tk("pt0"), Tk("pt1")]
            rden = car.take([128, 2, TS], F32)
            RD = [Tk("rd0"), Tk("rd1")]
            MIX = [Tk("mix%d" % t) for t in range(NT)]
            MO = [Tk("mo%d" % t) for t in range(NT)]
            QM = [Tk("qm%d" % t) for t in range(NT)]
            w2d = a_w_in[jA]
            mset("pool", z[:, 0:1], 0.0, (Z,))
            mset("pool", z[:, S + 1:S + 2], 0.0, (Z,))
            for c in range(7):
                si = c % 2
                if c < 6:
                    load_w_cols(slots[si], SL[si], ch_w[1 + si], w2d,
                                [(c * 128, 128), (768 + c * 128, 128), (1536 + c * 128, 128)])
                else:
                    load_w_cols(slots[si], SL[si], ch_w[1 + si], w2d, [(2304, 256)])
                if c < 6:
                    def consumer(tt, ec, bank, c=c):
                        if ec == 0:
                            act(gb[:, tsl(tt)], ps[:, bank, :], AF.Copy, (PB[bank],), (GB,))
                        elif ec == 1:
                            act(gcs[:, tt % 2, :], ps[:, bank, :], AF.Copy, (PB[bank],), (GCS[tt % 2],))
                        else:
                            tt_("dve", z[:, 1 + tt * TS:1 + (tt + 1) * TS], ps[:, bank, :], gcs[:, tt % 2, :], ALU.mult,
                                (PB[bank], GCS[tt % 2]), (Z,))
                    inproj_block(slots[si], SL[si], 384, consumer)
                    wc = lambda k, c=c: convw[:, (jA * 3 + k) * 6 + c:(jA * 3 + k) * 6 + c + 1]
                    act(tmpc[:, 0, :], z[:, 0:S], AF.Identity, (Z, CONST), (TC[0],), scale=wc(0))
                    stt("dve", tmpc[:, 1, :], z[:, 1:S + 1], wc(1), tmpc[:, 0, :], ALU.mult, ALU.add,
                        (Z, CONST, TC[0]), (TC[1],))
                    stt("dve", tmpc[:, 0, :], z[:, 2:S + 2], wc(2), tmpc[:, 1, :], ALU.mult, ALU.add,
                        (Z, CONST, TC[1]), (TC[0],))
                    tt_("pool", mix[:, c, :], tmpc[:, 0, :], gb[:], ALU.mult, (TC[0], GB), tuple(MIX))
                else:
                    def consumer(tt, ec, bank):
                        Sd.op("act", lambda e, o=qm[:, ec, tsl(tt)], i=ps[:, bank, :]: e.mul(o, i, 0.125),
                              (PB[bank],), (QM[tt],))
                    inproj_block(slots[si], SL[si], 256, consumer)
            mem_attention(qm, QM, memout, MO, pT, PT, rden, RD)
            Sd.barrier()
            car2 = Carver()
            car2.take([128, 8, S], BF16)
            cat = [mix[:, c, :] for c in range(6)] + [memout[:, c, :] for c in range(2)]
            CAT = [(MIX[t], MO[t]) for t in range(NT)]
            outproj_ln(l, a_w_out[jA], 6, cat, CAT, car2)
            Sd.barrier()

        def mixer_c(l, jC):
            car = Carver()
            mix = car.take([128, 6, S], BF16)
            memout = car.take([128, 2, S], BF16)
            qm = car.take([128, 2, S], BF16)
            slots = [car.take([128, 8, 256], BF16) for _ in range(2)]
            SL = [Tk("sl0"), Tk("sl1")]
            pw = car.take([128, 6, MIXW], BF16)
            PW = Tk("pw")
            PADW = 16
            bufs = [car.take([128, S + 2 * PADW], F32) for _ in range(3)]
            BU = [Tk("bu%d" % i) for i in range(3)]
            pT = car.take([128, 2, 2, TS], BF16)
            PT = [Tk("pt0"), Tk("pt1")]
            rden = car.take([128, 2, TS], F32)
            RD = [Tk("rd0"), Tk("rd1")]
            etmp = car.take([128, 16], F32)
            ET = Tk("et")
            MIX = [Tk("mix%d" % t) for t in range(NT)]
            MO = [Tk("mo%d" % t) for t in range(NT)]
            QM = [Tk("qm%d" % t) for t in range(NT)]
            w2d = c_w_in[jC]
            dma("pool", pw, pwfull.rearrange("(c p) e -> p c e", p=128), ch_w[3], (), (PW,))
            for i in range(3):
                mset("pool", bufs[i][:, 0:PADW], 0.0, (BU[i],))
                mset("pool", bufs[i][:, S + PADW:S + 2 * PADW], 0.0, (BU[i],))
            ub, pa, pb = bufs

            def window(prs, wi):
                w = POOLW[wi]
                P = PADW
                lo, hi = -15, S + 15
                tt_("dve", pa[prs, P + lo:P + hi], ub[prs, P + lo - 1:P + hi - 1], ub[prs, P + lo:P + hi], ALU.add,
                    (BU[0],), (BU[1],))
                cur, other, curi, othi = pa, pb, 1, 2
                step = 1
                lo_c, hi_c = lo, hi
                ww = 2
                while ww < w:
                    lo_n, hi_n = lo_c + step, hi_c - step
                    tt_("dve", other[prs, P + lo_n:P + hi_n], cur[prs, P + lo_n - step:P + hi_n - step],
                        cur[prs, P + lo_n + step:P + hi_n + step], ALU.add, (BU[curi],), (BU[othi],))
                    cur, other, curi, othi = other, cur, othi, curi
                    lo_c, hi_c = lo_n, hi_n
                    step *= 2
                    ww *= 2
                return cur, curi

            def pool_half(c, prs, wi):
                cur, curi = window(prs, wi)
                w = POOLW[wi]
                P = PADW
                tt_("pool", cur[prs, P:P + 8], cur[prs, P:P + 8], edge[prs, wi, 0:8], ALU.mult, (BU[curi], CONST), (BU[curi],))
                tt_("pool", cur[prs, P + S - 8:P + S], cur[prs, P + S - 8:P + S], edge[prs, wi, 8:16], ALU.mult,
                    (BU[curi], CONST), (BU[curi],))
                stt("dve", mix[prs, c, :], cur[prs, P:P + S], 1.0 / w, ub[prs, P:P + S], ALU.mult, ALU.subtract,
                    (BU[curi], BU[0]), tuple(MIX))

            for c in range(4):
                si = c % 2
                if c < 3:
                    load_w_cols(slots[si], SL[si], ch_w[1 + si], w2d, [(c * 256, 256)])
                    for ecl in range(2):
                        cc = c * 2 + ecl
                        for tt in range(NT):
                            bank = tt % 4
                            mm(ps[:, bank, :], [(slots[si][:, kc, ecl * 128:(ecl + 1) * 128], xb[:, kc, tsl(tt)]) for kc in range(8)],
                               (SL[si], XB[tt]), (PB[bank],))
                            act(ub[:, PADW + tt * TS:PADW + (tt + 1) * TS], ps[:, bank, :], AF.Copy, (PB[bank],), (BU[0],))
                        g0 = (cc * 128) // 192
                        g1 = (cc * 128 + 64) // 192
                        if g0 == g1:
                            pool_half(cc, slice(0, 128), g0)
                        else:
                            pool_half(cc, slice(0, 64), g0)
                            pool_half(cc, slice(64, 128), g1)
                else:
                    load_w_cols(slots[si], SL[si], ch_w[1 + si], w2d, [(768, 256)])

                    def consumer(tt, ec, bank):
                        Sd.op("act", lambda e, o=qm[:, ec, tsl(tt)], i=ps[:, bank, :]: e.mul(o, i, 0.125),
                              (PB[bank],), (QM[tt],))
                    inproj_block(slots[si], SL[si], 256, consumer)
            for tt in range(NT):
                for dc in range(6):
                    mm(ps[:, dc, :], [(pw[:, kc, dc * 128:(dc + 1) * 128], mix[:, kc, tsl(tt)]) for kc in range(6)],
                       (PW, MIX[tt]), (PB[dc],))
                for dc in range(6):
                    act(mix[:, dc, tsl(tt)], ps[:, dc, :], AF.Identity, (PB[dc], CONST), (MIX[tt],), scale=pscale[:, dc:dc + 1])
            mem_attention(qm, QM, memout, MO, pT, PT, rden, RD)
            Sd.barrier()
            car2 = Carver()
            car2.take([128, 8, S], BF16)
            cat = [mix[:, c, :] for c in range(6)] + [memout[:, c, :] for c in range(2)]
            CAT = [(MIX[t], MO[t]) for t in range(NT)]
            outproj_ln(l, c_w_out[jC], 6, cat, CAT, car2)
            Sd.barrier()

        def mixer_b(l, jB):
            car = Carver()
            mix = car.take([128, 2, S], BF16)
            memout = car.take([128, 2, S], BF16)
            qm = car.take([128, 2, S], BF16)
            slots = [car.take([128, 8, 384], BF16) for _ in range(2)]
            SL = [Tk("sl0"), Tk("sl1")]
            qs = car.take([128, S], BF16)
            QS = Tk("qs")
            kEO = car.take([128, 2, S], BF16)
            KEO = Tk("keo")
            vEO = car.take([128, 2, 16, 128], BF16)
            VEO = Tk("veo")
            acc = car.take([128, 2, S], F32)
            NA = Tk("na")
            ebuf = car.take([128, 4, 2, 256], F32)
            EB = [Tk("eb%d" % i) for i in range(4)]
            pTd = car.take([128, 4, 2, 256], BF16)
            PTD = [Tk("ptd%d" % i) for i in range(4)]
            pT = car.take([128, 2, 2, TS], BF16)
            PT = [Tk("pt0"), Tk("pt1")]
            rden = car.take([128, 2, TS], F32)
            RD = [Tk("rd0"), Tk("rd1")]
            MIX = [Tk("mix%d" % t) for t in range(NT)]
            MO = [Tk("mo%d" % t) for t in range(NT)]
            QM = [Tk("qm%d" % t) for t in range(NT)]
            w2d = b_w_in[jB]
            mset("pool", kEO[:], 0.0, (KEO,))
            mset("pool", vEO[:], 0.0, (VEO,))
            it = 0
            for pr in range(2):
                mset("pool", acc[:].rearrange("p a b -> p (a b)"), 0.0, (NA,))
                for g, (win, dil) in enumerate(DIL):
                    ci = g * 2 + pr
                    si = it % 2
                    it += 1
                    load_w_cols(slots[si], SL[si], ch_w[1 + si], w2d,
                                [(ci * 128, 128), (768 + ci * 128, 128), (1536 + ci * 128, 128)])
                    slot = slots[si]
                    for tt in range(NT):
                        b0 = (tt % 2) * 2
                        mm(ps[:, b0, :], [(slot[:, kc, 0:128], xb[:, kc, tsl(tt)]) for kc in range(8)],
                           (SL[si], XB[tt]), (PB[b0],))
                        Sd.op("act", lambda e, o=qs[:, tsl(tt)], i=ps[:, b0, :]: e.mul(o, i, 0.125), (PB[b0],), (QS,))
                        mm(ps[:, b0 + 1, :], [(slot[:, kc, 128:256], xb[:, kc, tsl(tt)]) for kc in range(8)],
                           (SL[si], XB[tt]), (PB[b0 + 1],))
                        act(kEO[0:64, 0, tsl(tt)], ps[0:64, b0 + 1, :], AF.Copy, (PB[b0 + 1],), (KEO,))
                        cp("dve", kEO[64:128, 1, tsl(tt)], ps[64:128, b0 + 1, :], (PB[b0 + 1],), (KEO,))
                    n_sub = S // dil
                    nkb = n_sub // 128
                    ti = 0
                    for r in range(dil):
                        for kb in range(nkb):
                            bank = 4 + (ti // 4) % 2
                            sub = (ti % 4) * 128
                            start = kb * 128 * dil + r
                            toks = slice(start, start + 127 * dil + 1, dil)
                            mm(ps[:, bank, sub:sub + 128], [(xb[:, kc, toks], slot[:, kc, 256:384]) for kc in range(8)],
                               (SL[si],) + tuple(XB), (PB[bank],))
                            act(vEO[:, 0, ti, 0:64], ps[:, bank, sub:sub + 64], AF.Copy, (PB[bank],), (VEO,))
                            cp("dve", vEO[:, 1, ti, 64:128], ps[:, bank, sub + 64:sub + 128], (PB[bank],), (VEO,))
                            ti += 1
                    ti = 0
                    for r in range(dil):
                        for kb in range(nkb):
                            j0 = max(0, kb * 128 - 64)
                            j1 = min(n_sub, kb * 128 + 192)
                            nq = j1 - j0
                            col0 = j0 - (kb * 128 - 64)
                            kst = kb * 128 * dil + r
                            ktoks = slice(kst, kst + 127 * dil + 1, dil)
                            qst = j0 * dil + r
                            qtoks = slice(qst, qst + (nq - 1) * dil + 1, dil)
                            bp = ti % 4
                            psS = ps[:, bp, :].rearrange("p (a b) -> p a b", a=2)
                            psN = ps[:, 4 + bp, :].rearrange("p (a b) -> p a b", a=2)
                            for hh in range(2):
                                mm(psS[:, hh, 0:nq], [(kEO[:, hh, ktoks], qs[:, qtoks])], (KEO, QS), (PB[bp],))
                            act(ebuf[:, bp, :, 0:nq], psS[:, :, 0:nq], AF.Exp, (PB[bp],), (EB[bp],))
                            tt_("pool", pTd[:, bp, :, 0:nq], ebuf[:, bp, :, 0:nq],
                                expb[:, g * 4 + 2 * pr:g * 4 + 2 * pr + 2, col0:col0 + nq], ALU.mult,
                                (EB[bp], CONST), (PTD[bp],))
                            mm(psN[:, 0, 0:nq], [(vEO[:, hh, ti, :], pTd[:, bp, hh, 0:nq]) for hh in range(2)],
                               (VEO, PTD[bp]), (PB[4 + bp],))
                            mm(psN[:, 1, 0:nq], [(onespad[:, hh, :], pTd[:, bp, hh, 0:nq]) for hh in range(2)],
                               (CONST, PTD[bp]), (PB[4 + bp],))
                            tt_("dve", acc[:, :, qtoks], psN[:, :, 0:nq], acc[:, :, qtoks], ALU.add, (PB[4 + bp], NA), (NA,))
                            ti += 1
                Sd.op("dve", lambda e: e.reciprocal(acc[:, 1, :], acc[:, 1, :]), (NA,), (NA,))
                tt_("pool", mix[:, pr, :], acc[:, 0, :], acc[:, 1, :], ALU.mult, (NA,), tuple(MIX))
            si = it % 2
            load_w_cols(slots[si], SL[si], ch_w[1 + si], w2d, [(2304, 256)])

            def consumer(tt, ec, bank):
                Sd.op("act", lambda e, o=qm[:, ec, tsl(tt)], i=ps[:, bank, :]: e.mul(o, i, 0.125),
                      (PB[bank],), (QM[tt],))
            inproj_block(slots[si][:, :, 0:256], SL[si], 256, consumer)
            mem_attention(qm, QM, memout, MO, pT, PT, rden, RD)
            Sd.barrier()
            car2 = Carver()
            car2.take([128, 4, S], BF16)
            cat = [mix[:, c, :] for c in range(2)] + [memout[:, c, :] for c in range(2)]
            CAT = [(MIX[t], MO[t]) for t in range(NT)]
            outproj_ln(l, b_w_out[jB], 2, cat, CAT, car2)
            Sd.barrier()

        class FFNState:
            pass

        def ffn_setup():
            st = FFNState()
            car = Carver()
            st.wg = [car.take([128, 8, 512], BF16) for _ in range(2)]
            st.wu = [car.take([128, 8, 512], BF16) for _ in range(2)]
            st.wd = [car.take([128, 4, D], BF16) for _ in range(2)]
            st.WS = [Tk("ws0"), Tk("ws1")]
            st.h = car.take([128, 2, 4, TS], BF16)
            st.H = [Tk("h0"), Tk("h1")]
            st.sg = car.take([128, 2, TS], F32)
            st.SG = [Tk("sg0"), Tk("sg1")]
            st.sgc = car.take([128, 2, TS], F32)
            st.SGC = [Tk("sgc0"), Tk("sgc1")]
            st.cb = car.take([128, 2, S], F32)
            st.CB = [Tk("cb0"), Tk("cb1")]
            st.blk = 0
            st.car = car
            return st

        def ffn_pass(st, wg2d, wu2d, wd2d, dff, coef_slot):
            f0 = 0
            while f0 < dff:
                fb = min(512, dff - f0)
                nfc = fb // 128
                si = st.blk % 2
                st.blk += 1
                ch = ch_w[si * 3:(si * 3) + 3]
                dma("pool", st.wg[si][:, :, 0:fb], wg2d[:, f0:f0 + fb].rearrange("(c p) e -> p c e", p=128), ch[0], (), (st.WS[si],))
                dma("pool", st.wu[si][:, :, 0:fb], wu2d[:, f0:f0 + fb].rearrange("(c p) e -> p c e", p=128), ch[1], (), (st.WS[si],))
                dma("pool", st.wd[si][:, 0:nfc, :], wd2d[f0:f0 + fb, :].rearrange("(c p) e -> p c e", p=128), ch[2], (), (st.WS[si],))
                WSt = st.WS[si]

                def gu(tt, fc, si=si, WSt=WSt):
                    bg, bu = (fc % 2) * 2, (fc % 2) * 2 + 1
                    hp = tt % 2
                    mm(ps[:, bg, :], [(st.wg[si][:, kc, fc * 128:(fc + 1) * 128], xb[:, kc, tsl(tt)]) for kc in range(8)],
                       (WSt, XB[tt]), (PB[bg],))
                    mm(ps[:, bu, :], [(st.wu[si][:, kc, fc * 128:(fc + 1) * 128], xb[:, kc, tsl(tt)]) for kc in range(8)],
                       (WSt, XB[tt]), (PB[bu],))
                    act(st.sg[:, fc % 2, :], ps[:, bg, :], AF.Silu, (PB[bg],), (st.SG[fc % 2],))
                    if coef_slot is None:
                        tt_("dve", st.h[:, hp, fc, :], ps[:, bu, :], st.sg[:, fc % 2, :], ALU.mult,
                            (PB[bu], st.SG[fc % 2]), (st.H[hp],))
                    else:
                        tt_("pool", st.sgc[:, fc % 2, :], st.sg[:, fc % 2, :], st.cb[:, coef_slot, tsl(tt)], ALU.mult,
                            (st.SG[fc % 2], st.CB[coef_slot]), (st.SGC[fc % 2],))
                        tt_("dve", st.h[:, hp, fc, :], ps[:, bu, :], st.sgc[:, fc % 2, :], ALU.mult,
                            (PB[bu], st.SGC[fc % 2]), (st.H[hp],))

                def down(tt, half, si=si, WSt=WSt, nfc=nfc):
                    hp = tt % 2
                    for dl in range(4):
                        dc = half * 4 + dl
                        mm(ps[:, 4 + dl, :], [(st.wd[si][:, fc, dc * 128:(dc + 1) * 128], st.h[:, hp, fc, :]) for fc in range(nfc)],
                           (WSt, st.H[hp]), (PB[4 + dl],))
                    Sd.op("dve", lambda e, o=xacc[:, half * 4:half * 4 + 4, tsl(tt)], a=ps[:, 4:8, :]:
                          e.tensor_tensor(o, a, o, ALU.add), (PB[4], PB[5], PB[6], PB[7], XA[tt]), (XA[tt],))

                for tt in range(NT + 1):
                    for fc in range(nfc):
                        if tt < NT:
                            gu(tt, fc)
                        if tt > 0 and fc == (nfc // 2) - 1:
                            down(tt - 1, 0)
                    if tt > 0:
                        down(tt - 1, 1)
                f0 += fb

        def dense_ffn(l, f):
            st = ffn_setup()
            ffn_pass(st, ffn_w_gate[f], ffn_w_up[f], ffn_w_down[f], DFF, None)
            Sd.barrier()

        def moe_ffn(l, f):
            sparse = (MOE_MODE == "cap")
            car = Carver()
            posm = car.take([128, 16, NE], F32)
            lg = car.take([128, 16, NE], F32)
            m1 = car.take([128, 16], F32)
            m2 = car.take([128, 16], F32)
            mk1 = car.take([128, 16, NE], F32)
            mk2 = car.take([128, 16, NE], F32)
            l2 = car.take([128, 16, NE], F32)
            w1 = car.take([128, 16], F32)
            w2 = car.take([128, 16], F32)
            coef = car.take([128, 16, NE], F32)
            coefT = car.take([NE, S], F32)
            R = Tk("route")
            fl = lambda a: a.rearrange("p a b -> p (a b)")
            for tc_ in range(16):
                tt = tc_ // 4
                mm(ps[:, 0, tc_ * NE:(tc_ + 1) * NE],
                   [(xacc[:, kc, tc_ * 128:(tc_ + 1) * 128], router[:, f, kc, :]) for kc in range(8)],
                   (XA[tt], CONST), (PB[0],))
            Sd.op("act", lambda e: e.mul(fl(lg), ps[:, 0, 0:16 * NE], 1.0 / ALPHA), (PB[0],), (R,))
            Sd.op("dve", lambda e: e.tensor_reduce(m1[:], lg[:], AX.X, ALU.max), (R,), (R,))
            tt_("dve", mk1[:], lg[:], m1[:].unsqueeze(2).to_broadcast([128, 16, NE]), ALU.is_equal, (R,), (R,))
            stt("dve", l2[:], mk1[:], -1e30, lg[:], ALU.mult, ALU.add, (R,), (R,))
            Sd.op("dve", lambda e: e.tensor_reduce(m2[:], l2[:], AX.X, ALU.max), (R,), (R,))
            tt_("dve", mk2[:], l2[:], m2[:].unsqueeze(2).to_broadcast([128, 16, NE]), ALU.is_equal, (R,), (R,))
            if sparse:
                msk = car.take([128, 16, NE], F32)
                tot = car.take([128, 16, NE], F32)
                off = car.take([128, 16, NE], F32)
                pos = car.take([128, 16, NE], F32)
                posT = car.take([NE, S], F32)
                lst = car.take([128, 128], F32)
                dma("sp", lst, c_lstrict, ch_misc[2], (), (R,))
                tt_("dve", msk[:], mk1[:], mk2[:], ALU.add, (R,), (R,))
                mm(ps[:, 5, 0:128], [(lst, fl(msk))], (R,), (PB[5],))
                mm(ps[:, 6, 0:128], [(ones32[:], fl(msk))], (R, CONST), (PB[6],))
                Sd.op("act", lambda e: e.copy(fl(tot), ps[:, 6, 0:128]), (PB[6],), (R,))
                mset("dve", off[:, 0, :], 0.0, (R,))
                for tc_ in range(1, 16):
                    tt_("dve", off[:, tc_, :], off[:, tc_ - 1, :], tot[:, tc_ - 1, :], ALU.add, (R,), (R,))
                tt_("dve", fl(pos), ps[:, 5, 0:128], fl(off), ALU.add, (PB[5], R), (R,))
                stt("dve", posm[:], pos[:], 1.0, msk[:], ALU.add, ALU.mult, (R,), (R,))
                ts_("dve", posm[:], posm[:], -1.0, None, ALU.add, None, (R,), (R,))
                for tc_ in range(16):
                    bank = 4 + tc_ // 4
                    sub = (tc_ % 4) * 128
                    Sd.op("pe", lambda e, o=ps[0:NE, bank, sub:sub + 128], i=posm[:, tc_, :]: e.transpose(o, i, ident[:]),
                          (R, CONST), (PB[bank],))
                for q in range(4):
                    cp("dve", posT[:, q * 512:(q + 1) * 512], ps[0:NE, 4 + q, :], (PB[4 + q],), (R,))
                dma("sp", pos_scr, posT, ch_pos, (R,), (R,))
            tt_("dve", w2[:], m2[:], m1[:], ALU.subtract, (R,), (R,))
            act(w2[:], w2[:], AF.Exp, (R,), (R,))
            ts_("dve", w1[:], w2[:], 1.0, None, ALU.add, None, (R,), (R,))
            Sd.op("dve", lambda e: e.reciprocal(w1[:], w1[:]), (R,), (R,))
            ts_("dve", w2[:], w1[:], -1.0, 1.0, ALU.mult, ALU.add, (R,), (R,))
            tt_("dve", mk1[:], mk1[:], w1[:].unsqueeze(2).to_broadcast([128, 16, NE]), ALU.mult, (R,), (R,))
            tt_("dve", mk2[:], mk2[:], w2[:].unsqueeze(2).to_broadcast([128, 16, NE]), ALU.mult, (R,), (R,))
            tt_("dve", coef[:], mk1[:], mk2[:], ALU.add, (R,), (R,))
            for tc_ in range(16):
                bank = 1 + tc_ // 4
                sub = (tc_ % 4) * 128
                Sd.op("pe", lambda e, o=ps[0:NE, bank, sub:sub + 128], i=coef[:, tc_, :]: e.transpose(o, i, ident[:]),
                      (R, CONST), (PB[bank],))
            for q in range(4):
                Sd.op("act", lambda e, o=coefT[:, q * 512:(q + 1) * 512], i=ps[0:NE, 1 + q, :]: e.copy(o, i), (PB[1 + q],), (R,))
            dma("sp", coef_scr, coefT, ch_coef, (R,), (R,))
            Sd.barrier()
            if not sparse:
                st = ffn_setup()
                for ex in range(NE):
                    cs = ex % 2
                    dma("sp", st.cb[:, cs, :], coef_scr[ex:ex + 1, :].to_broadcast([128, S]), ch_cb[cs], (R,), (st.CB[cs],))
                    ffn_pass(st, moe_w_gate[f, ex], moe_w_up[f, ex], moe_w_down[f, ex], DFFE, cs)
                Sd.barrier()
                return
            carD = Carver()
            posm = carD.take([128, 16, NE], F32)
            xtm = carD.take([128, 16, D], BF16)
            Pb = [carD.take([128, 16, CAP], BF16) for _ in range(2)]
            xds = [carD.take([128, 8, CAP], BF16) for _ in range(2)]
            iot = carD.take([128, CAP], F32)
            XTM, IOT, XDSCR = Tk("xtm"), Tk("iot"), Tk("xdscr")
            PTK = [Tk("p0"), Tk("p1")]
            XDS = [Tk("xds0"), Tk("xds1")]
            dma("sp", iot, c_iota, ch_misc[3], (), (IOT,))
            for tc_ in range(16):
                bank = tc_ % 2
                psb = ps[:, bank, :].bitcast(BF16)
                for kc in range(8):
                    Sd.op("pe", lambda e, o=psb[:, kc * 128:(kc + 1) * 128], i=xb[:, kc, tc_ * 128:(tc_ + 1) * 128]:
                          e.transpose(o, i, identb[:]), (XB[tc_ // 4], CONST), (PB[bank],))
                if tc_ % 2 == 0:
                    act(xtm[:, tc_, :], psb, AF.Copy, (PB[bank],), (XTM,))
                else:
                    cp("dve", xtm[:, tc_, :], psb, (PB[bank],), (XTM,))
            for ex in range(NE):
                pi = ex % 2
                for tc_ in range(16):
                    ts_("dve", Pb[pi][:, tc_, :], iot, posm[:, tc_, ex:ex + 1], None, ALU.is_equal, None, (IOT, R), (PTK[pi],))
                for dc in range(8):
                    b0 = 2 + (dc % 3) * 2
                    for ti, (s0, sn) in enumerate(STILES):
                        mm(ps[:, b0 + ti, 0:sn],
                           [(xtm[:, tc_, dc * 128:(dc + 1) * 128], Pb[pi][:, tc_, s0:s0 + sn]) for tc_ in range(16)],
                           (XTM, PTK[pi]), (PB[b0 + ti],))
                    act(xds[pi][:, dc, 0:512], ps[:, b0, 0:512], AF.Copy, (PB[b0],), (XDS[pi],))
                    cp("dve", xds[pi][:, dc, 512:CAP], ps[:, b0 + 1, 0:CAP - 512], (PB[b0 + 1],), (XDS[pi],))
                dma("sp", xd_scr[ex].rearrange("(c p) s -> p c s", p=128), xds[pi], ch_xd[pi], (XDS[pi],), (XDSCR,))
            Sd.barrier()
            carE = Carver()
            xd = [carE.take([128, 8, CAP], BF16) for _ in range(2)]
            PT = carE.take([128, NSC, S], BF16)
            yd = carE.take([128, NSC, D], F32)
            ydb = carE.take([128, NSC, D], BF16)
            wg = [carE.take([128, 8, 256], BF16) for _ in range(2)]
            wu = [carE.take([128, 8, 256], BF16) for _ in range(2)]
            wd = [carE.take([128, 2, D], BF16) for _ in range(2)]
            carX = Carver(base=xb[:].rearrange("p a b -> p (a b)"), nbytes=32 * 1024)
            hT = carX.take([128, 2, 2, CAP], BF16)
            sg = carX.take([128, 2, CAP], F32)
            pcb = carX.take([128, S], F32)
            wg.append(carX.take([128, 8, 256], BF16))
            wu.append(carX.take([128, 8, 256], BF16))
            wd.append(carX.take([128, 2, D], BF16))
            XD = [Tk("xd0"), Tk("xd1")]
            WG = [Tk("wg%d" % i) for i in range(3)]
            WU = [Tk("wu%d" % i) for i in range(3)]
            WD = [Tk("wd%d" % i) for i in range(3)]
            HT = [Tk("ht0"), Tk("ht1")]
            SG = [Tk("sg0"), Tk("sg1")]
            PTT, YD, YDB, PCB = Tk("pt"), Tk("yd"), Tk("ydb"), Tk("pcb")
            state = {"blk": 0, "r": 0}

            def down_part(prev, half):
                si, hp = prev
                for sc in range(NSC):
                    bank = 4 + state["r"] % 4
                    state["r"] += 1
                    mm(ps[:, bank, :],
                       [(hT[:, hp, fc, sc * 128:(sc + 1) * 128], wd[si][:, fc, half * 512:(half + 1) * 512]) for fc in range(2)],
                       (WD[si], HT[hp]), (PB[bank],))
                    ydv = yd[:, sc, half * 512:(half + 1) * 512]
                    tt_("dve", ydv, ps[:, bank, :], ydv, ALU.add, (PB[bank], YD), (YD,))

            for ex in range(NE):
                xi = ex % 2
                dma("sp", xd[xi], xd_scr[ex].rearrange("(c p) s -> p c s", p=128), ch_xdl[xi], (XDSCR,), (XD[xi],))
                dma("sp", pcb, pos_scr[ex:ex + 1, :].to_broadcast([128, S]), ch_cb[0], (R,), (PCB,))
                mset("pool", yd[:].rearrange("p a b -> p (a b)"), 0.0, (YD,))
                for sc in range(NSC):
                    ts_("dve", PT[:, sc, :], pcb, slotid[:, sc:sc + 1], None, ALU.is_equal, None, (PCB, CONST), (PTT,))
                dma("sp", pcb, coef_scr[ex:ex + 1, :].to_broadcast([128, S]), ch_cb[1], (R,), (PCB,))
                for sc in range(NSC):
                    tt_("dve", PT[:, sc, :], PT[:, sc, :], pcb, ALU.mult, (PCB, PTT), (PTT,))
                prev = None
                for b in range(DFFE // 256):
                    si = state["blk"] % 3
                    hp = state["blk"] % 2
                    state["blk"] += 1
                    f0 = b * 256
                    ch = ch_w[si * 3:si * 3 + 3]
                    dma("pool", wg[si], moe_w_gate[f, ex][:, f0:f0 + 256].rearrange("(c p) e -> p c e", p=128), ch[0], (), (WG[si],))
                    dma("pool", wu[si], moe_w_up[f, ex][:, f0:f0 + 256].rearrange("(c p) e -> p c e", p=128), ch[1], (), (WU[si],))
                    dma("pool", wd[si], moe_w_down[f, ex][f0:f0 + 256, :].rearrange("(c p) e -> p c e", p=128), ch[2], (), (WD[si],))
                    for fc in range(2):
                        for ti, (s0, sn) in enumerate(STILES):
                            mm(ps[:, ti, 0:sn],
                               [(wg[si][:, kc, fc * 128:(fc + 1) * 128], xd[xi][:, kc, s0:s0 + sn]) for kc in range(8)],
                               (WG[si], XD[xi]), (PB[ti],))
                            mm(ps[:, 2 + ti, 0:sn],
                               [(wu[si][:, kc, fc * 128:(fc + 1) * 128], xd[xi][:, kc, s0:s0 + sn]) for kc in range(8)],
                               (WU[si], XD[xi]), (PB[2 + ti],))
                        for ti, (s0, sn) in enumerate(STILES):
                            act(sg[:, fc, s0:s0 + sn], ps[:, ti, 0:sn], AF.Silu, (PB[ti],), (SG[fc],))
                        for ti, (s0, sn) in enumerate(STILES):
                            tt_("dve", hT[:, hp, fc, s0:s0 + sn], ps[:, 2 + ti, 0:sn], sg[:, fc, s0:s0 + sn], ALU.mult,
                                (PB[2 + ti], SG[fc]), (HT[hp],))
                        if prev is not None:
                            down_part(prev, fc)
                    prev = (si, hp)
                down_part(prev, 0)
                down_part(prev, 1)
                act(ydb[:].rearrange("p a b -> p (a b)"), yd[:].rearrange("p a b -> p (a b)"), AF.Copy, (YD,), (YDB,))
                for tt in range(NT):
                    for dc in range(8):
                        bank = 4 + state["r"] % 4
                        state["r"] += 1
                        mm(ps[:, bank, :], [(ydb[:, sc, dc * 128:(dc + 1) * 128], PT[:, sc, tsl(tt)]) for sc in range(NSC)],
                           (YDB, PTT), (PB[bank],))
                        tt_("dve", xacc[:, dc, tsl(tt)], ps[:, bank, :], xacc[:, dc, tsl(tt)], ALU.add,
                            (PB[bank], XA[tt]), (XA[tt],))
            Sd.barrier()

        def ln2(l, final):
            car = Carver()
            sq = [car.take([128, 8, TS], F32) for _ in range(2)]
            tmp = [car.take([128, 4, TS], F32) for _ in range(2)]
            SQ = [Tk("sq0"), Tk("sq1")]
            TMP = [[Tk("tmp%d" % i) for i in range(4)] for _ in range(2)]
            for tt in range(NT):
                layer_norm(tt, l, 1, sq[tt % 2], tmp[tt % 2], SQ[tt % 2], TMP[tt % 2], final, (tt % 4) * 2, (tt % 4) * 2 + 1)
            Sd.barrier()

        for s in range(nseq):
            for tt in range(NT):
                dma("sp", xacc[:, :, tsl(tt)], xT[s][:, tsl(tt)].rearrange("(c p) t -> p c t", p=128), ch_x[tt], (), (XA[tt],))
            for tt in range(NT):
                cp("dve", xb[:, :, tsl(tt)], xacc[:, :, tsl(tt)], (XA[tt],), (XB[tt],))
                Sd.op("act", lambda e, v=xacc[:, :, tsl(tt)]: e.mul(v, v, ALPHA), (XA[tt], XB[tt]), (XA[tt],))
            car = Carver()
            memb = car.take([128, 8, 256], BF16)
            wkv = car.take([128, 8, 512], BF16)
            MB, WK = Tk("memb"), Tk("wkv")
            dma("pool", memb, memT[s].rearrange("(c p) m -> p c m", p=128), ch_misc[0], (), (MB,))
            dma("pool", wkv, w_mem_kv.rearrange("(c p) e -> p c e", p=128), ch_misc[1], (), (WK,))
            for ec in range(2):
                mm(ps[:, ec, 0:256], [(wkv[:, kc, ec * 128:(ec + 1) * 128], memb[:, kc, :]) for kc in range(8)],
                   (MB, WK), (PB[ec],))
                act(kTpad[0:64, 2 * ec, :], ps[0:64, ec, 0:256], AF.Copy, (PB[ec],), (KV,))
                cp("dve", kTpad[64:128, 2 * ec + 1, :], ps[64:128, ec, 0:256], (PB[ec],), (KV,))
            for mc in range(2):
                mm(ps[:, 2 + mc, 0:256], [(memb[:, kc, mc * 128:(mc + 1) * 128], wkv[:, kc, 256:512]) for kc in range(8)],
                   (MB, WK), (PB[2 + mc],))
                for h in range(4):
                    o = vpad[:, h, mc, (h % 2) * 64:(h % 2) * 64 + 64]
                    i = ps[:, 2 + mc, h * 64:(h + 1) * 64]
                    if h % 2 == 0:
                        act(o, i, AF.Copy, (PB[2 + mc],), (KV,))
                    else:
                        cp("dve", o, i, (PB[2 + mc],), (KV,))
            Sd.barrier()
            for li, l in enumerate(layers):
                kind, j = l % 3, l // 3
                if kind == 0:
                    mixer_a(l, j)
                elif kind == 1:
                    mixer_b(l, j)
                else:
                    mixer_c(l, j)
                if l % 2 == 0:
                    dense_ffn(l, l // 2)
                else:
                    moe_ffn(l, l // 2)
                last = (li == len(layers) - 1)
                ln2(l, last)
                if debug and not last:
                    for tt in range(NT):
                        dma("sp", dbg_out[li, s][:, tsl(tt)].rearrange("(c p) t -> p c t", p=128), xacc[:, :, tsl(tt)],
                            ch_out[tt], (XA[tt],), ())
            for tt in range(NT):
                dma("sp", yT[s][:, tsl(tt)].rearrange("(c p) t -> p c t", p=128), xacc[:, :, tsl(tt)],
                    ch_out[tt], (XA[tt],), ())

        Sd.finalize()
        for c in Sd.chans:
            if c.count > 0:
                c.sem = es.enter_context(nc.semaphore("c_" + c.name))
        with nc.Block() as block:
            Sd.emit(nc, block, engsem)
    return nc


def _alibi_slopes(n):
    return 2.0 ** (-8.0 * np.arange(1, n + 1, dtype=np.float64) / n)


def _host_consts(ln_g, ln_b, a_conv_w, c_pool_w, c_pool_scale):
    c = {}
    lnp = np.zeros((128, 128), np.float32)
    lnp[:, 0:64] = ln_g.reshape(8, 8, 128).transpose(2, 0, 1).reshape(128, 64)
    lnp[:, 64:128] = ln_b.reshape(8, 8, 128).transpose(2, 0, 1).reshape(128, 64)
    c["c_lnp"] = lnp
    c["c_convw"] = np.ascontiguousarray(a_conv_w.reshape(6, 6, 128).transpose(2, 0, 1).reshape(128, 36))
    c["c_pscale"] = np.ascontiguousarray(c_pool_scale.reshape(6, 128).T)
    c["c_ident"] = np.eye(128, dtype=np.float32)
    slopes = _alibi_slopes(12).reshape(3, 4)
    p = np.arange(128)[:, None]
    jl = np.arange(256)[None, :]
    delta = jl - 64 - p
    eb = np.zeros((128, 12, 256), np.float32)
    for g, (win, dil) in enumerate(DIL):
        for h in range(4):
            v = np.exp(-slopes[g, h] * np.abs(delta) * dil)
            eb[:, g * 4 + h, :] = np.where(np.abs(delta) <= 64, v, 0.0)
    c["c_expb"] = eb.reshape(128, 12 * 256)
    ed = np.ones((128, 4, 16), np.float32)
    for wi, w in enumerate(POOLW):
        for t in range(8):
            lo, hi = max(t - w // 2, 0), min(t + w // 2 - 1, S - 1)
            ed[:, wi, t] = w / float(hi - lo + 1)
            t2 = S - 8 + t
            lo, hi = max(t2 - w // 2, 0), min(t2 + w // 2 - 1, S - 1)
            ed[:, wi, 8 + t] = w / float(hi - lo + 1)
    c["c_edge"] = ed.reshape(128, 64)
    c["c_lstrict"] = np.triu(np.ones((128, 128), np.float32), 1)
    c["c_iota"] = np.ascontiguousarray(np.broadcast_to(np.arange(CAP, dtype=np.float32)[None, :], (128, CAP)))
    c["c_slotid"] = (np.arange(128, dtype=np.float32)[:, None] + 128.0 * np.arange(8, dtype=np.float32)[None, :])
    pw = np.zeros((MIXW, MIXW), np.float32)
    for g in range(4):
        pw[g * 192:(g + 1) * 192, g * 192:(g + 1) * 192] = c_pool_w[0, g]
    c["pwfull"] = pw
    return c


LAYERS = [0, 1, 2, 3]
NSEQ = 2
_CACHE = {}


def kernel(x, mem, w_mem_kv, a_w_in, a_conv_w, a_w_out, b_w_in, b_w_out,
           c_w_in, c_pool_w, c_pool_scale, c_w_out, ln_g, ln_b,
           ffn_w_gate, ffn_w_up, ffn_w_down, moe_router, moe_w_gate, moe_w_up, moe_w_down,
           _layers=None, _debug=False, _ncores=8):
    layers = LAYERS if _layers is None else _layers
    f32 = lambda a: np.ascontiguousarray(np.asarray(a, dtype=np.float32))
    x = f32(x)
    mem = f32(mem)
    consts = _host_consts(f32(ln_g), f32(ln_b), f32(a_conv_w), f32(c_pool_w), f32(c_pool_scale))
    shared = {
        "w_mem_kv": f32(w_mem_kv), "a_w_in": f32(a_w_in), "a_w_out": f32(a_w_out),
        "b_w_in": f32(b_w_in), "b_w_out": f32(b_w_out), "c_w_in": f32(c_w_in), "c_w_out": f32(c_w_out),
        "ffn_w_gate": f32(ffn_w_gate), "ffn_w_up": f32(ffn_w_up), "ffn_w_down": f32(ffn_w_down),
        "moe_router": f32(moe_router), "moe_w_gate": f32(moe_w_gate), "moe_w_up": f32(moe_w_up),
        "moe_w_down": f32(moe_w_down),
    }
    shared.update(consts)
    key = (tuple(layers), NSEQ, _debug)
    if key not in _CACHE:
        _CACHE[key] = build_program(layers, NSEQ, _debug)
    nc = _CACHE[key]
    in_maps = []
    for c in range(_ncores):
        m = dict(shared)
        m["xT"] = np.ascontiguousarray(x[c * NSEQ:(c + 1) * NSEQ].transpose(0, 2, 1))
        m["memT"] = np.ascontiguousarray(mem[c * NSEQ:(c + 1) * NSEQ].transpose(0, 2, 1))
        in_maps.append(m)
    res = run_bass_kernel_spmd(nc, in_maps, core_ids=list(range(_ncores)))
    out = np.empty((_ncores * NSEQ, S, D), np.float32)
    for c in range(_ncores):
        out[c * NSEQ:(c + 1) * NSEQ] = res.results[c]["yT"].transpose(0, 2, 1)
    if _debug:
        return out, [r.get("dbg") for r in res.results]
    return out
```

```python
import contextlib
import numpy as np
import concourse.bass as bass
import concourse.mybir as mybir
from concourse.bass_utils import run_bass_kernel_spmd

F32 = mybir.dt.float32
BF16 = mybir.dt.bfloat16
AF = mybir.ActivationFunctionType
ALU = mybir.AluOpType
AX = mybir.AxisListType

D = 1024
S = 2048
NT = 4
TS = 512
DEPTH = 4
ALPHA = float((2 * DEPTH) ** 0.25)
LN_EPS = 1e-5
MIXW = 768
DFF = 2816
DFFE = 3584
NE = 8
DIL = ((128, 1), (512, 4), (2048, 16))
POOLW = (2, 4, 8, 16)
ARENA_BYTES = 96 * 1024
MOE_MODE = "cap"
CAP = 640
NSC = CAP // 128
STILES = ((0, 512), (512, CAP - 512))


class Tk:
    __slots__ = ("name", "w", "r", "rd")

    def __init__(self, name=""):
        self.name = name
        self.w = None
        self.r = {}
        self.rd = []


class Chan:
    __slots__ = ("sem", "count", "name")

    def __init__(self, name):
        self.name = name
        self.sem = None
        self.count = 0


class Op:
    __slots__ = ("eng", "fn", "deps", "signal", "sigval", "kind", "chan", "dmaval")


ENGS = ("pe", "act", "dve", "pool", "sp")


class Sched:
    def __init__(self):
        self.ops = []
        self.extra = {e: [] for e in ENGS}
        self.last = {e: None for e in ENGS}
        self.chans = []
        self.last_dma = {}

    def chan(self, name):
        c = Chan(name)
        self.chans.append(c)
        return c

    def op(self, eng, fn, reads=(), writes=(), chan=None):
        o = Op()
        o.eng = eng
        o.fn = fn
        o.signal = False
        o.sigval = None
        o.kind = "d" if chan is not None else "c"
        o.chan = chan
        o.dmaval = None
        deps = {}
        for t in reads:
            if t.w is not None:
                deps[id(t.w)] = t.w
        for t in writes:
            if t.w is not None:
                deps[id(t.w)] = t.w
            for r in t.r.values():
                deps[id(r)] = r
            for r in t.rd:
                deps[id(r)] = r
        for d in self.extra[eng]:
            deps[id(d)] = d
        self.extra[eng] = []
        dl = []
        for d in deps.values():
            if d is o:
                continue
            if d.kind == "c" and o.kind == "c" and d.eng == "pe" and eng == "pe":
                continue
            d.signal = True
            dl.append(d)
        o.deps = dl
        if chan is not None:
            chan.count += 16
            o.dmaval = chan.count
            self.last_dma[id(chan)] = o
        for t in reads:
            if o.kind == "d":
                t.rd.append(o)
            else:
                t.r[eng] = o
        for t in writes:
            t.w = o
            t.r = {}
            t.rd = []
        if o.kind == "c":
            self.last[eng] = o
        self.ops.append(o)
        return o

    def barrier(self):
        deps = [o for o in self.last.values() if o is not None]
        deps += list(self.last_dma.values())
        for e in ENGS:
            self.extra[e] = list(deps)

    def finalize(self):
        cnt = {e: 0 for e in ENGS}
        for o in self.ops:
            if o.kind == "c" and o.signal:
                cnt[o.eng] += 1
                o.sigval = cnt[o.eng]
        self.sigcounts = cnt

    def emit(self, nc, block, engsem):
        handles = {"pe": "tensor", "act": "scalar", "dve": "vector", "pool": "gpsimd", "sp": "sync"}
        final = [(c.sem, c.count) for c in self.chans if c.count > 0]

        def run(engname):
            def body(e):
                waited = {}
                for o in self.ops:
                    if o.eng != engname:
                        continue
                    for d in o.deps:
                        if d.kind == "d":
                            sem, val = d.chan.sem, d.dmaval
                        else:
                            sem, val = engsem[d.eng], d.sigval
                        k = id(sem)
                        if waited.get(k, 0) >= val:
                            continue
                        e.wait_ge(sem, val)
                        waited[k] = val
                    ins = o.fn(e)
                    if o.kind == "d":
                        ins.then_inc(o.chan.sem, 16)
                    elif o.signal:
                        ins.then_inc(engsem[o.eng], 1)
                if engname == "sp":
                    for sem, val in final:
                        e.wait_ge(sem, val)
            return body

        for en in ENGS:
            getattr(block, handles[en])(run(en))


def build_program(layers, nseq, debug=False):
    nc = bass.Bass("TRN2", target_bir_lowering=False)
    Sd = Sched()

    def dram_in(name, shape):
        return nc.dram_tensor(name, list(shape), F32, kind="ExternalInput").ap()

    xT = dram_in("xT", [nseq, D, S])
    memT = dram_in("memT", [nseq, D, 256])
    w_mem_kv = dram_in("w_mem_kv", [D, 512])
    a_w_in = dram_in("a_w_in", [2, D, 2560])
    a_w_out = dram_in("a_w_out", [2, D, D])
    b_w_in = dram_in("b_w_in", [1, D, 2560])
    b_w_out = dram_in("b_w_out", [1, 512, D])
    c_w_in = dram_in("c_w_in", [1, D, D])
    c_w_out = dram_in("c_w_out", [1, D, D])
    pwfull = dram_in("pwfull", [MIXW, MIXW])
    ffn_w_gate = dram_in("ffn_w_gate", [2, D, DFF])
    ffn_w_up = dram_in("ffn_w_up", [2, D, DFF])
    ffn_w_down = dram_in("ffn_w_down", [2, DFF, D])
    moe_router = dram_in("moe_router", [2, D, NE])
    moe_w_gate = dram_in("moe_w_gate", [2, NE, D, DFFE])
    moe_w_up = dram_in("moe_w_up", [2, NE, D, DFFE])
    moe_w_down = dram_in("moe_w_down", [2, NE, DFFE, D])
    c_lnp = dram_in("c_lnp", [128, 128])
    c_convw = dram_in("c_convw", [128, 36])
    c_pscale = dram_in("c_pscale", [128, 6])
    c_ident = dram_in("c_ident", [128, 128])
    c_expb = dram_in("c_expb", [128, 12 * 256])
    c_edge = dram_in("c_edge", [128, 64])
    c_lstrict = dram_in("c_lstrict", [128, 128])
    c_iota = dram_in("c_iota", [128, CAP])
    c_slotid = dram_in("c_slotid", [128, 8])
    yT = nc.dram_tensor("yT", [nseq, D, S], F32, kind="ExternalOutput").ap()
    coef_scr = nc.dram_tensor("coef_scr", [NE, S], F32, kind="Internal").ap()
    pos_scr = nc.dram_tensor("pos_scr", [NE, S], F32, kind="Internal").ap()
    xd_scr = nc.dram_tensor("xd_scr", [NE, D, CAP], BF16, kind="Internal").ap()
    dbg_out = None
    if debug:
        dbg_out = nc.dram_tensor("dbg", [len(layers), nseq, D, S], F32, kind="ExternalOutput").ap()

    es = contextlib.ExitStack()
    with es:
        def sb(name, shape, dt):
            return es.enter_context(nc.sbuf_tensor(name, list(shape), dt))

        xacc = sb("xacc", [128, 8, S], F32)
        xb = sb("xb", [128, 8, S], BF16)
        lnp = sb("lnp", [128, 256], F32)
        convw = sb("convw", [128, 36], F32)
        pscale = sb("pscale", [128, 6], F32)
        router = sb("router", [128, 2, 8, NE], F32)
        ident = sb("ident", [128, 128], F32)
        ones32 = sb("ones32", [128, 128], F32)
        onespad = sb("onespad", [128, 2, 128], BF16)
        kTpad = sb("kTpad", [128, 4, 256], BF16)
        vpad = sb("vpad", [128, 4, 2, 128], BF16)
        expb = sb("expb", [128, 12, 256], BF16)
        edge = sb("edge", [128, 4, 16], F32)
        slotid = sb("slotid", [128, 8], F32)
        identb = sb("identb", [128, 128], BF16)
        arena = sb("arena", [128, ARENA_BYTES // 2], BF16)
        ps = es.enter_context(nc.psum_tensor("ps", [128, 8, 512], F32))

        engsem = {e: es.enter_context(nc.semaphore("s_" + e)) for e in ENGS}

        XA = [Tk("xa%d" % t) for t in range(NT)]
        XB = [Tk("xb%d" % t) for t in range(NT)]
        PB = [Tk("ps%d" % b) for b in range(8)]
        CONST = Tk("const")
        KV = Tk("kv")
        ch_const = Sd.chan("const")
        ch_x = [Sd.chan("x%d" % t) for t in range(NT)]
        ch_out = [Sd.chan("o%d" % t) for t in range(NT)]
        ch_w = [Sd.chan("w%d" % i) for i in range(9)]
        ch_misc = [Sd.chan("m%d" % i) for i in range(4)]
        ch_coef = Sd.chan("coef")
        ch_cb = [Sd.chan("cb%d" % i) for i in range(2)]
        ch_xd = [Sd.chan("xd%d" % i) for i in range(2)]
        ch_xdl = [Sd.chan("xdl%d" % i) for i in range(2)]
        ch_pos = Sd.chan("pos")

        def tsl(tt):
            return slice(tt * TS, (tt + 1) * TS)

        class Carver:
            def __init__(self, base=None, nbytes=ARENA_BYTES):
                self.off = 0
                self.base = arena if base is None else base
                self.nbytes = nbytes

            def take(self, shape, dt):
                esz = 4 if dt == F32 else 2
                n = 1
                for s_ in shape[1:]:
                    n *= s_
                nbytes = n * esz
                assert self.off % 4 == 0
                assert self.off + nbytes <= self.nbytes, (self.off, nbytes)
                v = self.base[:, self.off // 2:(self.off + nbytes) // 2]
                if dt == F32:
                    v = v.bitcast(F32)
                self.off += nbytes
                if shape[0] != 128:
                    v = v[0:shape[0]]
                if len(shape) == 3:
                    v = v.rearrange("p (a b) -> p a b", a=shape[1])
                elif len(shape) == 4:
                    v = v.rearrange("p (a b c) -> p a b c", a=shape[1], b=shape[2])
                return v

        def dma(eng, out, in_, chan, reads=(), writes=()):
            Sd.op(eng, lambda e, o=out, i=in_: e.dma_start(out=o, in_=i), reads, writes, chan=chan)

        def mm(out, pairs, reads, writes):
            def fn(e, out=out, pairs=pairs):
                n = len(pairs)
                ins = None
                for i, (l, r) in enumerate(pairs):
                    ins = e.matmul(out, l, r, start=(i == 0), stop=(i == n - 1))
                return ins
            Sd.op("pe", fn, reads, writes)

        def act(out, in_, func, reads, writes, bias=None, scale=None):
            kw = {}
            if bias is not None:
                kw["bias"] = bias
            if scale is not None:
                kw["scale"] = scale
            Sd.op("act", lambda e, o=out, i=in_, f=func, kw=kw: e.activation(o, i, f, **kw), reads, writes)

        def tt_(eng, out, in0, in1, op, reads, writes):
            Sd.op(eng, lambda e, o=out, a=in0, b=in1, op=op: e.tensor_tensor(o, a, b, op), reads, writes)

        def ts_(eng, out, in0, s1, s2, op0, op1, reads, writes):
            if op1 is None:
                Sd.op(eng, lambda e, o=out, a=in0, s1=s1, op0=op0: e.tensor_scalar(o, a, s1, None, op0), reads, writes)
            else:
                Sd.op(eng, lambda e, o=out, a=in0, s1=s1, s2=s2, op0=op0, op1=op1:
                      e.tensor_scalar(o, a, s1, s2, op0, op1), reads, writes)

        def stt(eng, out, in0, scalar, in1, op0, op1, reads, writes):
            Sd.op(eng, lambda e, o=out, a=in0, s=scalar, b=in1, op0=op0, op1=op1:
                  e.scalar_tensor_tensor(o, a, s, b, op0, op1), reads, writes)

        def cp(eng, out, in_, reads, writes):
            Sd.op(eng, lambda e, o=out, i=in_: e.tensor_copy(o, i), reads, writes)

        def mset(eng, ap, val, writes):
            Sd.op(eng, lambda e, a=ap, v=val: e.memset(a, v), (), writes)

        dma("sp", lnp[:, 0:128], c_lnp, ch_const, (), (CONST,))
        dma("sp", convw[:], c_convw, ch_const, (), (CONST,))
        dma("sp", pscale[:], c_pscale, ch_const, (), (CONST,))
        dma("sp", ident[:], c_ident, ch_const, (), (CONST,))
        dma("pool", expb[:].rearrange("p a b -> p (a b)"), c_expb, ch_const, (), (CONST,))
        dma("sp", edge[:].rearrange("p a b -> p (a b)"), c_edge, ch_const, (), (CONST,))
        for f in range(2):
            dma("sp", router[:, f], moe_router[f].rearrange("(c p) e -> p c e", p=128), ch_const, (), (CONST,))
        dma("sp", slotid[:], c_slotid, ch_const, (), (CONST,))
        Sd.op("act", lambda e: e.mul(lnp[:, 128:256], lnp[:, 0:128], ALPHA), (CONST,), (CONST,))
        cp("dve", identb[:], ident[:], (CONST,), (CONST,))
        mset("dve", ones32[:], 1.0, (CONST,))
        mset("dve", onespad[:], 0.0, (CONST,))
        mset("dve", onespad[:, 0, 0:64], 1.0, (CONST,))
        mset("dve", onespad[:, 1, 64:128], 1.0, (CONST,))
        mset("dve", kTpad[:], 0.0, (KV,))
        mset("dve", vpad[:], 0.0, (KV,))

        def lncol(l, j, c):
            return (l * 2 + j) * 8 + c

        def ln_part1(tt, sq, tmp, SQ, TMP, bank_a, bank_b):
            v = xacc[:, :, tsl(tt)]
            act(sq, v, AF.Square, (XA[tt],), (SQ,))
            mm(ps[:, bank_a, :], [(ones32[:], xacc[:, c, tsl(tt)]) for c in range(8)],
               (XA[tt], CONST), (PB[bank_a],))
            mm(ps[:, bank_b, :], [(ones32[:], sq[:, c, :]) for c in range(8)], (SQ, CONST), (PB[bank_b],))

        def ln_part1b(tt, sq, tmp, SQ, TMP, bank_a, bank_b):
            mean, msq, var, rstd = tmp[:, 0], tmp[:, 1], tmp[:, 2], tmp[:, 3]
            ts_("dve", mean, ps[:, bank_a, :], 1.0 / D, None, ALU.mult, None, (PB[bank_a],), (TMP[0],))
            tt_("dve", msq, mean, mean, ALU.mult, (TMP[0],), (TMP[1],))
            stt("dve", var, ps[:, bank_b, :], 1.0 / D, msq, ALU.mult, ALU.subtract, (PB[bank_b], TMP[1]), (TMP[2],))
            ts_("dve", var, var, LN_EPS, None, ALU.add, None, (TMP[2],), (TMP[2],))
            act(var, var, AF.Sqrt, (TMP[2],), (TMP[2],))
            Sd.op("dve", lambda e: e.reciprocal(rstd, var), (TMP[2],), (TMP[3],))

        def ln_part2(tt, l, j, sq, tmp, SQ, TMP, final):
            v = xacc[:, :, tsl(tt)]
            mean, rstd = tmp[:, 0], tmp[:, 3]
            mb = mean.unsqueeze(1).to_broadcast([128, 8, TS])
            rb = rstd.unsqueeze(1).to_broadcast([128, 8, TS])
            tt_("dve", sq, v, mb, ALU.subtract, (XA[tt], TMP[0]), (SQ,))
            tt_("dve", sq, sq, rb, ALU.mult, (SQ, TMP[3]), (SQ,))
            for c in range(8):
                col = lncol(l, j, c)
                g, b_ = lnp[:, col:col + 1], lnp[:, 64 + col:64 + col + 1]
                ga, ba = lnp[:, 128 + col:128 + col + 1], lnp[:, 192 + col:192 + col + 1]
                if final:
                    act(xacc[:, c, tsl(tt)], sq[:, c, :], AF.Identity, (SQ, CONST), (XA[tt],), bias=b_, scale=g)
                else:
                    act(xb[:, c, tsl(tt)], sq[:, c, :], AF.Identity, (SQ, CONST), (XB[tt],), bias=b_, scale=g)
                    act(xacc[:, c, tsl(tt)], sq[:, c, :], AF.Identity, (SQ, CONST), (XA[tt],), bias=ba, scale=ga)

        def load_w_cols(slot, slot_tk, chan, src2d, colgroups):
            off = 0
            for (c0, n) in colgroups:
                dma("pool", slot[:, :, off:off + n], src2d[:, c0:c0 + n].rearrange("(c p) e -> p c e", p=128),
                    chan, (), (slot_tk,))
                off += n

        def mem_attention(qm, QM, memout, MO, pT, PT, rden, RD):
            for tt in range(NT):
                for pr in range(2):
                    bn, bd = 4 + (pr % 2) * 2, 5 + (pr % 2) * 2
                    for hh in range(2):
                        h = 2 * pr + hh
                        b0 = (hh % 2) * 2
                        for mc in range(2):
                            mm(ps[:, b0 + mc, :], [(kTpad[:, h, mc * 128:(mc + 1) * 128], qm[:, pr, tsl(tt)])],
                               (KV, QM[tt]), (PB[b0 + mc],))
                        act(pT[:, hh], ps[:, b0:b0 + 2, :], AF.Exp, (PB[b0], PB[b0 + 1]), (PT[hh],))
                    mm(ps[:, bn, :], [(vpad[:, 2 * pr + hh, mc, :], pT[:, hh, mc, :]) for hh in range(2) for mc in range(2)],
                       (KV, PT[0], PT[1]), (PB[bn],))
                    mm(ps[:, bd, :], [(onespad[:, hh, :], pT[:, hh, mc, :]) for hh in range(2) for mc in range(2)],
                       (CONST, PT[0], PT[1]), (PB[bd],))
                    Sd.op("dve", lambda e, o=rden[:, pr % 2, :], i=ps[:, bd, :]: e.reciprocal(o, i), (PB[bd],), (RD[pr % 2],))
                    tt_("dve", memout[:, pr, tsl(tt)], ps[:, bn, :], rden[:, pr % 2, :], ALU.mult,
                        (PB[bn], RD[pr % 2]), (MO[tt],))

        def outproj_ln(l, wout2d, nmix, cat_aps, CAT, car):
            kc_n = len(cat_aps)
            wo = car.take([128, kc_n, D], BF16)
            WO = Tk("wo")
            dma("pool", wo, wout2d.rearrange("(c p) e -> p c e", p=128), ch_w[0], (), (WO,))
            sq = [car.take([128, 8, TS], F32) for _ in range(2)]
            tmp = [car.take([128, 4, TS], F32) for _ in range(2)]
            SQ = [Tk("sq0"), Tk("sq1")]
            TMP = [[Tk("tmp%d" % i) for i in range(4)] for _ in range(2)]
            for tt in range(NT):
                for dc in range(8):
                    bank = dc % 4
                    mm(ps[:, bank, :], [(wo[:, kc, dc * 128:(dc + 1) * 128], cat_aps[kc][:, tsl(tt)]) for kc in range(kc_n)],
                       (WO,) + tuple(CAT[tt]), (PB[bank],))
                    tt_("dve", xacc[:, dc, tsl(tt)], ps[:, bank, :], xacc[:, dc, tsl(tt)], ALU.add,
                        (PB[bank], XA[tt]), (XA[tt],))
                ln_part1(tt, sq[tt % 2], tmp[tt % 2], SQ[tt % 2], TMP[tt % 2], 4 + (tt % 2) * 2, 5 + (tt % 2) * 2)
                if tt > 0:
                    ln_part2(tt - 1, l, 0, sq[(tt - 1) % 2], tmp[(tt - 1) % 2], SQ[(tt - 1) % 2], TMP[(tt - 1) % 2], False)
                ln_part1b(tt, sq[tt % 2], tmp[tt % 2], SQ[tt % 2], TMP[tt % 2], 4 + (tt % 2) * 2, 5 + (tt % 2) * 2)
            ln_part2(NT - 1, l, 0, sq[(NT - 1) % 2], tmp[(NT - 1) % 2], SQ[(NT - 1) % 2], TMP[(NT - 1) % 2], False)

        def inproj_block(slot, SLOT, ncols, consumer):
            nch = ncols // 128
            k = 0
            for tt in range(NT):
                for ec in range(nch):
                    bank = k % 4
                    k += 1
                    mm(ps[:, bank, :], [(slot[:, kc, ec * 128:(ec + 1) * 128], xb[:, kc, tsl(tt)]) for kc in range(8)],
                       (SLOT, XB[tt]), (PB[bank],))
                    consumer(tt, ec, bank)

        def mixer_a(l, jA):
            car = Carver()
            mix = car.take([128, 6, S], BF16)
            memout = car.take([128, 2, S], BF16)
            qm = car.take([128, 2, S], BF16)
            slots = [car.take([128, 8, 384], BF16) for _ in range(2)]
            SL = [Tk("sl0"), Tk("sl1")]
            gcs = car.take([128, 2, TS], F32)
            GCS = [Tk("gcs0"), Tk("gcs1")]
            z = car.take([128, S + 2], F32)
            Z = Tk("z")
            gb = car.take([128, S], BF16)
            GB = Tk("gb")
            tmpc = car.take([128, 2, S], F32)
            TC = [Tk("tc0"), Tk("tc1")]
            pT = car.take([128, 2, 2, TS], BF16)
            PT = [Tk("pt0"), Tk("pt1")]
            rden = car.take([128, 2, TS], F32)
            RD = [Tk("rd0"), Tk("rd1")]
            MIX = [Tk("mix%d" % t) for t in range(NT)]
            MO = [Tk("mo%d" % t) for t in range(NT)]
            QM = [Tk("qm%d" % t) for t in range(NT)]
            w2d = a_w_in[jA]
            mset("pool", z[:, 0:1], 0.0, (Z,))
            mset("pool", z[:, S + 1:S + 2], 0.0, (Z,))
            for c in range(7):
                si = c % 2
                if c < 6:
                    load_w_cols(slots[si], SL[si], ch_w[1 + si], w2d,
                                [(c * 128, 128), (768 + c * 128, 128), (1536 + c * 128, 128)])
                else:
                    load_w_cols(slots[si], SL[si], ch_w[1 + si], w2d, [(2304, 256)])
                if c < 6:
                    def consumer(tt, ec, bank, c=c):
                        if ec == 0:
                            act(gb[:, tsl(tt)], ps[:, bank, :], AF.Copy, (PB[bank],), (GB,))
                        elif ec == 1:
                            act(gcs[:, tt % 2, :], ps[:, bank, :], AF.Copy, (PB[bank],), (GCS[tt % 2],))
                        else:
                            tt_("dve", z[:, 1 + tt * TS:1 + (tt + 1) * TS], ps[:, bank, :], gcs[:, tt % 2, :], ALU.mult,
                                (PB[bank], GCS[tt % 2]), (Z,))
                    inproj_block(slots[si], SL[si], 384, consumer)
                    wc = lambda k, c=c: convw[:, (jA * 3 + k) * 6 + c:(jA * 3 + k) * 6 + c + 1]
                    act(tmpc[:, 0, :], z[:, 0:S], AF.Identity, (Z, CONST), (TC[0],), scale=wc(0))
                    stt("dve", tmpc[:, 1, :], z[:, 1:S + 1], wc(1), tmpc[:, 0, :], ALU.mult, ALU.add,
                        (Z, CONST, TC[0]), (TC[1],))
                    stt("dve", tmpc[:, 0, :], z[:, 2:S + 2], wc(2), tmpc[:, 1, :], ALU.mult, ALU.add,
                        (Z, CONST, TC[1]), (TC[0],))
                    tt_("pool", mix[:, c, :], tmpc[:, 0, :], gb[:], ALU.mult, (TC[0], GB), tuple(MIX))
                else:
                    def consumer(tt, ec, bank):
                        Sd.op("act", lambda e, o=qm[:, ec, tsl(tt)], i=ps[:, bank, :]: e.mul(o, i, 0.125),
                              (PB[bank],), (QM[tt],))
                    inproj_block(slots[si], SL[si], 256, consumer)
            mem_attention(qm, QM, memout, MO, pT, PT, rden, RD)
            Sd.barrier()
            car2 = Carver()
            car2.take([128, 8, S], BF16)
            cat = [mix[:, c, :] for c in range(6)] + [memout[:, c, :] for c in range(2)]
            CAT = [(MIX[t], MO[t]) for t in range(NT)]
            outproj_ln(l, a_w_out[jA], 6, cat, CAT, car2)
            Sd.barrier()

        def mixer_c(l, jC):
            car = Carver()
            mix = car.take([128, 6, S], BF16)
            memout = car.take([128, 2, S], BF16)
            qm = car.take([128, 2, S], BF16)
            slots = [car.take([128, 8, 256], BF16) for _ in range(2)]
            SL = [Tk("sl0"), Tk("sl1")]
            pw = car.take([128, 6, MIXW], BF16)
            PW = Tk("pw")
            PADW = 16
            bufs = [car.take([128, S + 2 * PADW], F32) for _ in range(3)]
            BU = [Tk("bu%d" % i) for i in range(3)]
            pT = car.take([128, 2, 2, TS], BF16)
            PT = [Tk("pt0"), Tk("pt1")]
            rden = car.take([128, 2, TS], F32)
            RD = [Tk("rd0"), Tk("rd1")]
            etmp = car.take([128, 16], F32)
            ET = Tk("et")
            MIX = [Tk("mix%d" % t) for t in range(NT)]
            MO = [Tk("mo%d" % t) for t in range(NT)]
            QM = [Tk("qm%d" % t) for t in range(NT)]
            w2d = c_w_in[jC]
            dma("pool", pw, pwfull.rearrange("(c p) e -> p c e", p=128), ch_w[3], (), (PW,))
            for i in range(3):
                mset("pool", bufs[i][:, 0:PADW], 0.0, (BU[i],))
                mset("pool", bufs[i][:, S + PADW:S + 2 * PADW], 0.0, (BU[i],))
            ub, pa, pb = bufs

            def window(prs, wi):
                w = POOLW[wi]
                P = PADW
                lo, hi = -15, S + 15
                tt_("dve", pa[prs, P + lo:P + hi], ub[prs, P + lo - 1:P + hi - 1], ub[prs, P + lo:P + hi], ALU.add,
                    (BU[0],), (BU[1],))
                cur, other, curi, othi = pa, pb, 1, 2
                step = 1
                lo_c, hi_c = lo, hi
                ww = 2
                while ww < w:
                    lo_n, hi_n = lo_c + step, hi_c - step
                    tt_("dve", other[prs, P + lo_n:P + hi_n], cur[prs, P + lo_n - step:P + hi_n - step],
                        cur[prs, P + lo_n + step:P + hi_n + step], ALU.add, (BU[curi],), (BU[othi],))
                    cur, other, curi, othi = other, cur, othi, curi
                    lo_c, hi_c = lo_n, hi_n
                    step *= 2
                    ww *= 2
                return cur, curi

            def pool_half(c, prs, wi):
                cur, curi = window(prs, wi)
                w = POOLW[wi]
                P = PADW
                tt_("pool", cur[prs, P:P + 8], cur[prs, P:P + 8], edge[prs, wi, 0:8], ALU.mult, (BU[curi], CONST), (BU[curi],))
                tt_("pool", cur[prs, P + S - 8:P + S], cur[prs, P + S - 8:P + S], edge[prs, wi, 8:16], ALU.mult,
                    (BU[curi], CONST), (BU[curi],))
                stt("dve", mix[prs, c, :], cur[prs, P:P + S], 1.0 / w, ub[prs, P:P + S], ALU.mult, ALU.subtract,
                    (BU[curi], BU[0]), tuple(MIX))

            for c in range(4):
                si = c % 2
                if c < 3:
                    load_w_cols(slots[si], SL[si], ch_w[1 + si], w2d, [(c * 256, 256)])
                    for ecl in range(2):
                        cc = c * 2 + ecl
                        for tt in range(NT):
                            bank = tt % 4
                            mm(ps[:, bank, :], [(slots[si][:, kc, ecl * 128:(ecl + 1) * 128], xb[:, kc, tsl(tt)]) for kc in range(8)],
                               (SL[si], XB[tt]), (PB[bank],))
                            act(ub[:, PADW + tt * TS:PADW + (tt + 1) * TS], ps[:, bank, :], AF.Copy, (PB[bank],), (BU[0],))
                        g0 = (cc * 128) // 192
                        g1 = (cc * 128 + 64) // 192
                        if g0 == g1:
                            pool_half(cc, slice(0, 128), g0)
                        else:
                            pool_half(cc, slice(0, 64), g0)
                            pool_half(cc, slice(64, 128), g1)
                else:
                    load_w_cols(slots[si], SL[si], ch_w[1 + si], w2d, [(768, 256)])

                    def consumer(tt, ec, bank):
                        Sd.op("act", lambda e, o=qm[:, ec, tsl(tt)], i=ps[:, bank, :]: e.mul(o, i, 0.125),
                              (PB[bank],), (QM[tt],))
                    inproj_block(slots[si], SL[si], 256, consumer)
            for tt in range(NT):
                for dc in range(6):
                    mm(ps[:, dc, :], [(pw[:, kc, dc * 128:(dc + 1) * 128], mix[:, kc, tsl(tt)]) for kc in range(6)],
                       (PW, MIX[tt]), (PB[dc],))
                for dc in range(6):
                    act(mix[:, dc, tsl(tt)], ps[:, dc, :], AF.Identity, (PB[dc], CONST), (MIX[tt],), scale=pscale[:, dc:dc + 1])
            mem_attention(qm, QM, memout, MO, pT, PT, rden, RD)
            Sd.barrier()
            car2 = Carver()
            car2.take([128, 8, S], BF16)
            cat = [mix[:, c, :] for c in range(6)] + [memout[:, c, :] for c in range(2)]
            CAT = [(MIX[t], MO[t]) for t in range(NT)]
            outproj_ln(l, c_w_out[jC], 6, cat, CAT, car2)
            Sd.barrier()

        def mixer_b(l, jB):
            car = Carver()
            mix = car.take([128, 2, S], BF16)
            memout = car.take([128, 2, S], BF16)
            qm = car.take([128, 2, S], BF16)
            slots = [car.take([128, 8, 384], BF16) for _ in range(2)]
            SL = [Tk("sl0"), Tk("sl1")]
            qs = car.take([128, S], BF16)
            QS = Tk("qs")
            kEO = car.take([128, 2, S], BF16)
            KEO = Tk("keo")
            vEO = car.take([128, 2, 16, 128], BF16)
            VEO = Tk("veo")
            acc = car.take([128, 2, S], F32)
            NA = Tk("na")
            ebuf = car.take([128, 4, 2, 256], F32)
            EB = [Tk("eb%d" % i) for i in range(4)]
            pTd = car.take([128, 4, 2, 256], BF16)
            PTD = [Tk("ptd%d" % i) for i in range(4)]
            pT = car.take([128, 2, 2, TS], BF16)
            PT = [Tk("pt0"), Tk("pt1")]
            rden = car.take([128, 2, TS], F32)
            RD = [Tk("rd0"), Tk("rd1")]
            MIX = [Tk("mix%d" % t) for t in range(NT)]
            MO = [Tk("mo%d" % t) for t in range(NT)]
            QM = [Tk("qm%d" % t) for t in range(NT)]
            w2d = b_w_in[jB]
            mset("pool", kEO[:], 0.0, (KEO,))
            mset("pool", vEO[:], 0.0, (VEO,))
            it = 0
            for pr in range(2):
                mset("pool", acc[:].rearrange("p a b -> p (a b)"), 0.0, (NA,))
                for g, (win, dil) in enumerate(DIL):
                    ci = g * 2 + pr
                    si = it % 2
                    it += 1
                    load_w_cols(slots[si], SL[si], ch_w[1 + si], w2d,
                                [(ci * 128, 128), (768 + ci * 128, 128), (1536 + ci * 128, 128)])
                    slot = slots[si]
                    for tt in range(NT):
                        b0 = (tt % 2) * 2
                        mm(ps[:, b0, :], [(slot[:, kc, 0:128], xb[:, kc, tsl(tt)]) for kc in range(8)],
                           (SL[si], XB[tt]), (PB[b0],))
                        Sd.op("act", lambda e, o=qs[:, tsl(tt)], i=ps[:, b0, :]: e.mul(o, i, 0.125), (PB[b0],), (QS,))
                        mm(ps[:, b0 + 1, :], [(slot[:, kc, 128:256], xb[:, kc, tsl(tt)]) for kc in range(8)],
                           (SL[si], XB[tt]), (PB[b0 + 1],))
                        act(kEO[0:64, 0, tsl(tt)], ps[0:64, b0 + 1, :], AF.Copy, (PB[b0 + 1],), (KEO,))
                        cp("dve", kEO[64:128, 1, tsl(tt)], ps[64:128, b0 + 1, :], (PB[b0 + 1],), (KEO,))
                    n_sub = S // dil
                    nkb = n_sub // 128
                    ti = 0
                    for r in range(dil):
                        for kb in range(nkb):
                            bank = 4 + (ti // 4) % 2
                            sub = (ti % 4) * 128
                            start = kb * 128 * dil + r
                            toks = slice(start, start + 127 * dil + 1, dil)
                            mm(ps[:, bank, sub:sub + 128], [(xb[:, kc, toks], slot[:, kc, 256:384]) for kc in range(8)],
                               (SL[si],) + tuple(XB), (PB[bank],))
                            act(vEO[:, 0, ti, 0:64], ps[:, bank, sub:sub + 64], AF.Copy, (PB[bank],), (VEO,))
                            cp("dve", vEO[:, 1, ti, 64:128], ps[:, bank, sub + 64:sub + 128], (PB[bank],), (VEO,))
                            ti += 1
                    ti = 0
                    for r in range(dil):
                        for kb in range(nkb):
                            j0 = max(0, kb * 128 - 64)
                            j1 = min(n_sub, kb * 128 + 192)
                            nq = j1 - j0
                            col0 = j0 - (kb * 128 - 64)
                            kst = kb * 128 * dil + r
                            ktoks = slice(kst, kst + 127 * dil + 1, dil)
                            qst = j0 * dil + r
                            qtoks = slice(qst, qst + (nq - 1) * dil + 1, dil)
                            bp = ti % 4
                            psS = ps[:, bp, :].rearrange("p (a b) -> p a b", a=2)
                            psN = ps[:, 4 + bp, :].rearrange("p (a b) -> p a b", a=2)
                            for hh in range(2):
                                mm(psS[:, hh, 0:nq], [(kEO[:, hh, ktoks], qs[:, qtoks])], (KEO, QS), (PB[bp],))
                            act(ebuf[:, bp, :, 0:nq], psS[:, :, 0:nq], AF.Exp, (PB[bp],), (EB[bp],))
                            tt_("pool", pTd[:, bp, :, 0:nq], ebuf[:, bp, :, 0:nq],
                                expb[:, g * 4 + 2 * pr:g * 4 + 2 * pr + 2, col0:col0 + nq], ALU.mult,
                                (EB[bp], CONST), (PTD[bp],))
                            mm(psN[:, 0, 0:nq], [(vEO[:, hh, ti, :], pTd[:, bp, hh, 0:nq]) for hh in range(2)],
                               (VEO, PTD[bp]), (PB[4 + bp],))
                            mm(psN[:, 1, 0:nq], [(onespad[:, hh, :], pTd[:, bp, hh, 0:nq]) for hh in range(2)],
                               (CONST, PTD[bp]), (PB[4 + bp],))
                            tt_("dve", acc[:, :, qtoks], psN[:, :, 0:nq], acc[:, :, qtoks], ALU.add, (PB[4 + bp], NA), (NA,))
                            ti += 1
                Sd.op("dve", lambda e: e.reciprocal(acc[:, 1, :], acc[:, 1, :]), (NA,), (NA,))
                tt_("pool", mix[:, pr, :], acc[:, 0, :], acc[:, 1, :], ALU.mult, (NA,), tuple(MIX))
            si = it % 2
            load_w_cols(slots[si], SL[si], ch_w[1 + si], w2d, [(2304, 256)])

            def consumer(tt, ec, bank):
                Sd.op("act", lambda e, o=qm[:, ec, tsl(tt)], i=ps[:, bank, :]: e.mul(o, i, 0.125),
                      (PB[bank],), (QM[tt],))
            inproj_block(slots[si][:, :, 0:256], SL[si], 256, consumer)
            mem_attention(qm, QM, memout, MO, pT, PT, rden, RD)
            Sd.barrier()
            car2 = Carver()
            car2.take([128, 4, S], BF16)
            cat = [mix[:, c, :] for c in range(2)] + [memout[:, c, :] for c in range(2)]
            CAT = [(MIX[t], MO[t]) for t in range(NT)]
            outproj_ln(l, b_w_out[jB], 2, cat, CAT, car2)
            Sd.barrier()

        class FFNState:
            pass

        def ffn_setup():
            st = FFNState()
            car = Carver()
            st.wg = [car.take([128, 8, 512], BF16) for _ in range(2)]
            st.wu = [car.take([128, 8, 512], BF16) for _ in range(2)]
            st.wd = [car.take([128, 4, D], BF16) for _ in range(2)]
            st.WS = [Tk("ws0"), Tk("ws1")]
            st.WG = [Tk("fwg0"), Tk("fwg1")]
            st.WU = [Tk("fwu0"), Tk("fwu1")]
            st.WD = [Tk("fwd0"), Tk("fwd1")]
            st.h = car.take([128, 2, 4, TS], BF16)
            st.H = [Tk("h0"), Tk("h1")]
            st.sg = car.take([128, 2, TS], F32)
            st.SG = [Tk("sg0"), Tk("sg1")]
            st.sgc = car.take([128, 2, TS], F32)
            st.SGC = [Tk("sgc0"), Tk("sgc1")]
            st.cb = car.take([128, 2, S], F32)
            st.CB = [Tk("cb0"), Tk("cb1")]
            st.blk = 0
            st.car = car
            return st

        def ffn_pass(st, wg2d, wu2d, wd2d, dff, coef_slot):
            f0 = 0
            while f0 < dff:
                fb = min(512, dff - f0)
                nfc = fb // 128
                si = st.blk % 2
                st.blk += 1
                ch = ch_w[si * 3:(si * 3) + 3]
                dma("pool", st.wg[si][:, :, 0:fb], wg2d[:, f0:f0 + fb].rearrange("(c p) e -> p c e", p=128), ch[0], (), (st.WG[si],))
                dma("pool", st.wu[si][:, :, 0:fb], wu2d[:, f0:f0 + fb].rearrange("(c p) e -> p c e", p=128), ch[1], (), (st.WU[si],))
                dma("pool", st.wd[si][:, 0:nfc, :], wd2d[f0:f0 + fb, :].rearrange("(c p) e -> p c e", p=128), ch[2], (), (st.WD[si],))
                WSt = st.WS[si]

                def gu(tt, fc, si=si, WSt=WSt):
                    bg, bu = (fc % 2) * 2, (fc % 2) * 2 + 1
                    hp = tt % 2
                    mm(ps[:, bg, :], [(st.wg[si][:, kc, fc * 128:(fc + 1) * 128], xb[:, kc, tsl(tt)]) for kc in range(8)],
                       (st.WG[si], XB[tt]), (PB[bg],))
                    mm(ps[:, bu, :], [(st.wu[si][:, kc, fc * 128:(fc + 1) * 128], xb[:, kc, tsl(tt)]) for kc in range(8)],
                       (st.WU[si], XB[tt]), (PB[bu],))
                    act(st.sg[:, fc % 2, :], ps[:, bg, :], AF.Silu, (PB[bg],), (st.SG[fc % 2],))
                    if coef_slot is None:
                        tt_("dve", st.h[:, hp, fc, :], ps[:, bu, :], st.sg[:, fc % 2, :], ALU.mult,
                            (PB[bu], st.SG[fc % 2]), (st.H[hp],))
                    else:
                        tt_("pool", st.sgc[:, fc % 2, :], st.sg[:, fc % 2, :], st.cb[:, coef_slot, tsl(tt)], ALU.mult,
                            (st.SG[fc % 2], st.CB[coef_slot]), (st.SGC[fc % 2],))
                        tt_("dve", st.h[:, hp, fc, :], ps[:, bu, :], st.sgc[:, fc % 2, :], ALU.mult,
                            (PB[bu], st.SGC[fc % 2]), (st.H[hp],))

                def down(tt, half, si=si, WSt=WSt, nfc=nfc):
                    hp = tt % 2
                    for dl in range(4):
                        dc = half * 4 + dl
                        mm(ps[:, 4 + dl, :], [(st.wd[si][:, fc, dc * 128:(dc + 1) * 128], st.h[:, hp, fc, :]) for fc in range(nfc)],
                           (st.WD[si], st.H[hp]), (PB[4 + dl],))
                    Sd.op("dve", lambda e, o=xacc[:, half * 4:half * 4 + 4, tsl(tt)], a=ps[:, 4:8, :]:
                          e.tensor_tensor(o, a, o, ALU.add), (PB[4], PB[5], PB[6], PB[7], XA[tt]), (XA[tt],))

                for tt in range(NT + 1):
                    for fc in range(nfc):
                        if tt < NT:
                            gu(tt, fc)
                        if tt > 0 and fc == (nfc // 2) - 1:
                            down(tt - 1, 0)
                    if tt > 0:
                        down(tt - 1, 1)
                f0 += fb

        def dense_ffn(l, f):
            st = ffn_setup()
            ffn_pass(st, ffn_w_gate[f], ffn_w_up[f], ffn_w_down[f], DFF, None)
            Sd.barrier()

        def moe_ffn(l, f):
            sparse = (MOE_MODE == "cap")
            car = Carver()
            posm = car.take([128, 16, NE], F32)
            lg = car.take([128, 16, NE], F32)
            m1 = car.take([128, 16], F32)
            m2 = car.take([128, 16], F32)
            mk1 = car.take([128, 16, NE], F32)
            mk2 = car.take([128, 16, NE], F32)
            l2 = car.take([128, 16, NE], F32)
            w1 = car.take([128, 16], F32)
            w2 = car.take([128, 16], F32)
            coef = car.take([128, 16, NE], F32)
            coefT = car.take([NE, S], F32)
            R = Tk("route")
            fl = lambda a: a.rearrange("p a b -> p (a b)")
            for tc_ in range(16):
                tt = tc_ // 4
                mm(ps[:, 0, tc_ * NE:(tc_ + 1) * NE],
                   [(xacc[:, kc, tc_ * 128:(tc_ + 1) * 128], router[:, f, kc, :]) for kc in range(8)],
                   (XA[tt], CONST), (PB[0],))
            Sd.op("act", lambda e: e.mul(fl(lg), ps[:, 0, 0:16 * NE], 1.0 / ALPHA), (PB[0],), (R,))
            Sd.op("dve", lambda e: e.tensor_reduce(m1[:], lg[:], AX.X, ALU.max), (R,), (R,))
            tt_("dve", mk1[:], lg[:], m1[:].unsqueeze(2).to_broadcast([128, 16, NE]), ALU.is_equal, (R,), (R,))
            stt("dve", l2[:], mk1[:], -1e30, lg[:], ALU.mult, ALU.add, (R,), (R,))
            Sd.op("dve", lambda e: e.tensor_reduce(m2[:], l2[:], AX.X, ALU.max), (R,), (R,))
            tt_("dve", mk2[:], l2[:], m2[:].unsqueeze(2).to_broadcast([128, 16, NE]), ALU.is_equal, (R,), (R,))
            if sparse:
                msk = car.take([128, 16, NE], F32)
                tot = car.take([128, 16, NE], F32)
                off = car.take([128, 16, NE], F32)
                pos = car.take([128, 16, NE], F32)
                posT = car.take([NE, S], F32)
                lst = car.take([128, 128], F32)
                dma("sp", lst, c_lstrict, ch_misc[2], (), (R,))
                tt_("dve", msk[:], mk1[:], mk2[:], ALU.add, (R,), (R,))
                mm(ps[:, 5, 0:128], [(lst, fl(msk))], (R,), (PB[5],))
                mm(ps[:, 6, 0:128], [(ones32[:], fl(msk))], (R, CONST), (PB[6],))
                Sd.op("act", lambda e: e.copy(fl(tot), ps[:, 6, 0:128]), (PB[6],), (R,))
                mset("dve", off[:, 0, :], 0.0, (R,))
                for tc_ in range(1, 16):
                    tt_("dve", off[:, tc_, :], off[:, tc_ - 1, :], tot[:, tc_ - 1, :], ALU.add, (R,), (R,))
                tt_("dve", fl(pos), ps[:, 5, 0:128], fl(off), ALU.add, (PB[5], R), (R,))
                stt("dve", posm[:], pos[:], 1.0, msk[:], ALU.add, ALU.mult, (R,), (R,))
                ts_("dve", posm[:], posm[:], -1.0, None, ALU.add, None, (R,), (R,))
                for tc_ in range(16):
                    bank = 4 + tc_ // 4
                    sub = (tc_ % 4) * 128
                    Sd.op("pe", lambda e, o=ps[0:NE, bank, sub:sub + 128], i=posm[:, tc_, :]: e.transpose(o, i, ident[:]),
                          (R, CONST), (PB[bank],))
                for q in range(4):
                    cp("dve", posT[:, q * 512:(q + 1) * 512], ps[0:NE, 4 + q, :], (PB[4 + q],), (R,))
                dma("sp", pos_scr, posT, ch_pos, (R,), (R,))
            tt_("dve", w2[:], m2[:], m1[:], ALU.subtract, (R,), (R,))
            act(w2[:], w2[:], AF.Exp, (R,), (R,))
            ts_("dve", w1[:], w2[:], 1.0, None, ALU.add, None, (R,), (R,))
            Sd.op("dve", lambda e: e.reciprocal(w1[:], w1[:]), (R,), (R,))
            ts_("dve", w2[:], w1[:], -1.0, 1.0, ALU.mult, ALU.add, (R,), (R,))
            tt_("dve", mk1[:], mk1[:], w1[:].unsqueeze(2).to_broadcast([128, 16, NE]), ALU.mult, (R,), (R,))
            tt_("dve", mk2[:], mk2[:], w2[:].unsqueeze(2).to_broadcast([128, 16, NE]), ALU.mult, (R,), (R,))
            tt_("dve", coef[:], mk1[:], mk2[:], ALU.add, (R,), (R,))
            for tc_ in range(16):
                bank = 1 + tc_ // 4
                sub = (tc_ % 4) * 128
                Sd.op("pe", lambda e, o=ps[0:NE, bank, sub:sub + 128], i=coef[:, tc_, :]: e.transpose(o, i, ident[:]),
                      (R, CONST), (PB[bank],))
            for q in range(4):
                Sd.op("act", lambda e, o=coefT[:, q * 512:(q + 1) * 512], i=ps[0:NE, 1 + q, :]: e.copy(o, i), (PB[1 + q],), (R,))
            dma("sp", coef_scr, coefT, ch_coef, (R,), (R,))
            Sd.barrier()
            if not sparse:
                st = ffn_setup()
                for ex in range(NE):
                    cs = ex % 2
                    dma("sp", st.cb[:, cs, :], coef_scr[ex:ex + 1, :].to_broadcast([128, S]), ch_cb[cs], (R,), (st.CB[cs],))
                    ffn_pass(st, moe_w_gate[f, ex], moe_w_up[f, ex], moe_w_down[f, ex], DFFE, cs)
                Sd.barrier()
                return
            carD = Carver()
            posm = carD.take([128, 16, NE], F32)
            xtm = carD.take([128, 16, D], BF16)
            Pb = [carD.take([128, 16, CAP], BF16) for _ in range(2)]
            xds = [carD.take([128, 8, CAP], BF16) for _ in range(2)]
            iot = carD.take([128, CAP], F32)
            XTM, IOT, XDSCR = Tk("xtm"), Tk("iot"), Tk("xdscr")
            PTK = [Tk("p0"), Tk("p1")]
            XDS = [Tk("xds0"), Tk("xds1")]
            dma("sp", iot, c_iota, ch_misc[3], (), (IOT,))
            for tc_ in range(16):
                bank = tc_ % 2
                psb = ps[:, bank, :].bitcast(BF16)
                for kc in range(8):
                    Sd.op("pe", lambda e, o=psb[:, kc * 128:(kc + 1) * 128], i=xb[:, kc, tc_ * 128:(tc_ + 1) * 128]:
                          e.transpose(o, i, identb[:]), (XB[tc_ // 4], CONST), (PB[bank],))
                if tc_ % 2 == 0:
                    act(xtm[:, tc_, :], psb, AF.Copy, (PB[bank],), (XTM,))
                else:
                    cp("dve", xtm[:, tc_, :], psb, (PB[bank],), (XTM,))
            for ex in range(NE):
                pi = ex % 2
                for tc_ in range(16):
                    ts_("dve", Pb[pi][:, tc_, :], iot, posm[:, tc_, ex:ex + 1], None, ALU.is_equal, None, (IOT, R), (PTK[pi],))
                for dc in range(8):
                    b0 = 2 + (dc % 3) * 2
                    for ti, (s0, sn) in enumerate(STILES):
                        mm(ps[:, b0 + ti, 0:sn],
                           [(xtm[:, tc_, dc * 128:(dc + 1) * 128], Pb[pi][:, tc_, s0:s0 + sn]) for tc_ in range(16)],
                           (XTM, PTK[pi]), (PB[b0 + ti],))
                    act(xds[pi][:, dc, 0:512], ps[:, b0, 0:512], AF.Copy, (PB[b0],), (XDS[pi],))
                    cp("dve", xds[pi][:, dc, 512:CAP], ps[:, b0 + 1, 0:CAP - 512], (PB[b0 + 1],), (XDS[pi],))
                dma("sp", xd_scr[ex].rearrange("(c p) s -> p c s", p=128), xds[pi], ch_xd[pi], (XDS[pi],), (XDSCR,))
            Sd.barrier()
            carE = Carver()
            xd = [carE.take([128, 8, CAP], BF16) for _ in range(2)]
            PT = carE.take([128, NSC, S], BF16)
            yd = carE.take([128, NSC, D], F32)
            ydb = carE.take([128, NSC, D], BF16)
            wg = [carE.take([128, 8, 256], BF16) for _ in range(2)]
            wu = [carE.take([128, 8, 256], BF16) for _ in range(2)]
            wd = [carE.take([128, 2, D], BF16) for _ in range(2)]
            carX = Carver(base=xb[:].rearrange("p a b -> p (a b)"), nbytes=32 * 1024)
            hT = carX.take([128, 2, 2, CAP], BF16)
            sg = carX.take([128, 2, CAP], F32)
            pcb = carX.take([128, S], F32)
            wg.append(carX.take([128, 8, 256], BF16))
            wu.append(carX.take([128, 8, 256], BF16))
            wd.append(carX.take([128, 2, D], BF16))
            XD = [Tk("xd0"), Tk("xd1")]
            WG = [Tk("wg%d" % i) for i in range(3)]
            WU = [Tk("wu%d" % i) for i in range(3)]
            WD = [Tk("wd%d" % i) for i in range(3)]
            HT = [Tk("ht0"), Tk("ht1")]
            SG = [Tk("sg0"), Tk("sg1")]
            PTT, YD, YDB, PCB = Tk("pt"), Tk("yd"), Tk("ydb"), Tk("pcb")
            state = {"blk": 0, "r": 0}

            def down_part(prev, half):
                si, hp = prev
                for sc in range(NSC):
                    bank = 4 + state["r"] % 4
                    state["r"] += 1
                    mm(ps[:, bank, :],
                       [(hT[:, hp, fc, sc * 128:(sc + 1) * 128], wd[si][:, fc, half * 512:(half + 1) * 512]) for fc in range(2)],
                       (WD[si], HT[hp]), (PB[bank],))
                    ydv = yd[:, sc, half * 512:(half + 1) * 512]
                    tt_("dve", ydv, ps[:, bank, :], ydv, ALU.add, (PB[bank], YD), (YD,))

            for ex in range(NE):
                xi = ex % 2
                dma("sp", xd[xi], xd_scr[ex].rearrange("(c p) s -> p c s", p=128), ch_xdl[xi], (XDSCR,), (XD[xi],))
                dma("sp", pcb, pos_scr[ex:ex + 1, :].to_broadcast([128, S]), ch_cb[0], (R,), (PCB,))
                mset("pool", yd[:].rearrange("p a b -> p (a b)"), 0.0, (YD,))
                for sc in range(NSC):
                    ts_("dve", PT[:, sc, :], pcb, slotid[:, sc:sc + 1], None, ALU.is_equal, None, (PCB, CONST), (PTT,))
                dma("sp", pcb, coef_scr[ex:ex + 1, :].to_broadcast([128, S]), ch_cb[1], (R,), (PCB,))
                for sc in range(NSC):
                    tt_("dve", PT[:, sc, :], PT[:, sc, :], pcb, ALU.mult, (PCB, PTT), (PTT,))
                prev = None
                for b in range(DFFE // 256):
                    si = state["blk"] % 3
                    hp = state["blk"] % 2
                    state["blk"] += 1
                    f0 = b * 256
                    ch = ch_w[si * 3:si * 3 + 3]
                    dma("pool", wg[si], moe_w_gate[f, ex][:, f0:f0 + 256].rearrange("(c p) e -> p c e", p=128), ch[0], (), (WG[si],))
                    dma("pool", wu[si], moe_w_up[f, ex][:, f0:f0 + 256].rearrange("(c p) e -> p c e", p=128), ch[1], (), (WU[si],))
                    dma("pool", wd[si], moe_w_down[f, ex][f0:f0 + 256, :].rearrange("(c p) e -> p c e", p=128), ch[2], (), (WD[si],))
                    for fc in range(2):
                        for ti, (s0, sn) in enumerate(STILES):
                            mm(ps[:, ti, 0:sn],
                               [(wg[si][:, kc, fc * 128:(fc + 1) * 128], xd[xi][:, kc, s0:s0 + sn]) for kc in range(8)],
                               (WG[si], XD[xi]), (PB[ti],))
                            mm(ps[:, 2 + ti, 0:sn],
                               [(wu[si][:, kc, fc * 128:(fc + 1) * 128], xd[xi][:, kc, s0:s0 + sn]) for kc in range(8)],
                               (WU[si], XD[xi]), (PB[2 + ti],))
                        for ti, (s0, sn) in enumerate(STILES):
                            act(sg[:, fc, s0:s0 + sn], ps[:, ti, 0:sn], AF.Silu, (PB[ti],), (SG[fc],))
                        for ti, (s0, sn) in enumerate(STILES):
                            tt_("dve", hT[:, hp, fc, s0:s0 + sn], ps[:, 2 + ti, 0:sn], sg[:, fc, s0:s0 + sn], ALU.mult,
                                (PB[2 + ti], SG[fc]), (HT[hp],))
                        if prev is not None:
                            down_part(prev, fc)
                    prev = (si, hp)
                down_part(prev, 0)
                down_part(prev, 1)
                act(ydb[:].rearrange("p a b -> p (a b)"), yd[:].rearrange("p a b -> p (a b)"), AF.Copy, (YD,), (YDB,))
                for tt in range(NT):
                    for dc in range(8):
                        bank = 4 + state["r"] % 4
                        state["r"] += 1
                        mm(ps[:, bank, :], [(ydb[:, sc, dc * 128:(dc + 1) * 128], PT[:, sc, tsl(tt)]) for sc in range(NSC)],
                           (YDB, PTT), (PB[bank],))
                        tt_("dve", xacc[:, dc, tsl(tt)], ps[:, bank, :], xacc[:, dc, tsl(tt)], ALU.add,
                            (PB[bank], XA[tt]), (XA[tt],))
            Sd.barrier()

        def ln2(l, final):
            car = Carver()
            sq = [car.take([128, 8, TS], F32) for _ in range(2)]
            tmp = [car.take([128, 4, TS], F32) for _ in range(2)]
            SQ = [Tk("sq0"), Tk("sq1")]
            TMP = [[Tk("tmp%d" % i) for i in range(4)] for _ in range(2)]
            for tt in range(NT):
                ln_part1(tt, sq[tt % 2], tmp[tt % 2], SQ[tt % 2], TMP[tt % 2], (tt % 4) * 2, (tt % 4) * 2 + 1)
                if tt > 0:
                    ln_part2(tt - 1, l, 1, sq[(tt - 1) % 2], tmp[(tt - 1) % 2], SQ[(tt - 1) % 2], TMP[(tt - 1) % 2], final)
                ln_part1b(tt, sq[tt % 2], tmp[tt % 2], SQ[tt % 2], TMP[tt % 2], (tt % 4) * 2, (tt % 4) * 2 + 1)
            ln_part2(NT - 1, l, 1, sq[(NT - 1) % 2], tmp[(NT - 1) % 2], SQ[(NT - 1) % 2], TMP[(NT - 1) % 2], final)
            Sd.barrier()

        for s in range(nseq):
            for tt in range(NT):
                dma("sp", xacc[:, :, tsl(tt)], xT[s][:, tsl(tt)].rearrange("(c p) t -> p c t", p=128), ch_x[tt], (), (XA[tt],))
            for tt in range(NT):
                cp("dve", xb[:, :, tsl(tt)], xacc[:, :, tsl(tt)], (XA[tt],), (XB[tt],))
                Sd.op("act", lambda e, v=xacc[:, :, tsl(tt)]: e.mul(v, v, ALPHA), (XA[tt], XB[tt]), (XA[tt],))
            car = Carver()
            memb = car.take([128, 8, 256], BF16)
            wkv = car.take([128, 8, 512], BF16)
            MB, WK = Tk("memb"), Tk("wkv")
            dma("pool", memb, memT[s].rearrange("(c p) m -> p c m", p=128), ch_misc[0], (), (MB,))
            dma("pool", wkv, w_mem_kv.rearrange("(c p) e -> p c e", p=128), ch_misc[1], (), (WK,))
            for ec in range(2):
                mm(ps[:, ec, 0:256], [(wkv[:, kc, ec * 128:(ec + 1) * 128], memb[:, kc, :]) for kc in range(8)],
                   (MB, WK), (PB[ec],))
                act(kTpad[0:64, 2 * ec, :], ps[0:64, ec, 0:256], AF.Copy, (PB[ec],), (KV,))
                cp("dve", kTpad[64:128, 2 * ec + 1, :], ps[64:128, ec, 0:256], (PB[ec],), (KV,))
            for mc in range(2):
                mm(ps[:, 2 + mc, 0:256], [(memb[:, kc, mc * 128:(mc + 1) * 128], wkv[:, kc, 256:512]) for kc in range(8)],
                   (MB, WK), (PB[2 + mc],))
                for h in range(4):
                    o = vpad[:, h, mc, (h % 2) * 64:(h % 2) * 64 + 64]
                    i = ps[:, 2 + mc, h * 64:(h + 1) * 64]
                    if h % 2 == 0:
                        act(o, i, AF.Copy, (PB[2 + mc],), (KV,))
                    else:
                        cp("dve", o, i, (PB[2 + mc],), (KV,))
            Sd.barrier()
            for li, l in enumerate(layers):
                kind, j = l % 3, l // 3
                if kind == 0:
                    mixer_a(l, j)
                elif kind == 1:
                    mixer_b(l, j)
                else:
                    mixer_c(l, j)
                if l % 2 == 0:
                    dense_ffn(l, l // 2)
                else:
                    moe_ffn(l, l // 2)
                last = (li == len(layers) - 1)
                ln2(l, last)
                if debug and not last:
                    for tt in range(NT):
                        dma("sp", dbg_out[li, s][:, tsl(tt)].rearrange("(c p) t -> p c t", p=128), xacc[:, :, tsl(tt)],
                            ch_out[tt], (XA[tt],), ())
            for tt in range(NT):
                dma("sp", yT[s][:, tsl(tt)].rearrange("(c p) t -> p c t", p=128), xacc[:, :, tsl(tt)],
                    ch_out[tt], (XA[tt],), ())

        Sd.finalize()
        for c in Sd.chans:
            if c.count > 0:
                c.sem = es.enter_context(nc.semaphore("c_" + c.name))
        with nc.Block() as block:
            Sd.emit(nc, block, engsem)
    return nc


def _alibi_slopes(n):
    return 2.0 ** (-8.0 * np.arange(1, n + 1, dtype=np.float64) / n)


def _host_consts(ln_g, ln_b, a_conv_w, c_pool_w, c_pool_scale):
    c = {}
    lnp = np.zeros((128, 128), np.float32)
    lnp[:, 0:64] = ln_g.reshape(8, 8, 128).transpose(2, 0, 1).reshape(128, 64)
    lnp[:, 64:128] = ln_b.reshape(8, 8, 128).transpose(2, 0, 1).reshape(128, 64)
    c["c_lnp"] = lnp
    c["c_convw"] = np.ascontiguousarray(a_conv_w.reshape(6, 6, 128).transpose(2, 0, 1).reshape(128, 36))
    c["c_pscale"] = np.ascontiguousarray(c_pool_scale.reshape(6, 128).T)
    c["c_ident"] = np.eye(128, dtype=np.float32)
    slopes = _alibi_slopes(12).reshape(3, 4)
    p = np.arange(128)[:, None]
    jl = np.arange(256)[None, :]
    delta = jl - 64 - p
    eb = np.zeros((128, 12, 256), np.float32)
    for g, (win, dil) in enumerate(DIL):
        for h in range(4):
            v = np.exp(-slopes[g, h] * np.abs(delta) * dil)
            eb[:, g * 4 + h, :] = np.where(np.abs(delta) <= 64, v, 0.0)
    c["c_expb"] = eb.reshape(128, 12 * 256)
    ed = np.ones((128, 4, 16), np.float32)
    for wi, w in enumerate(POOLW):
        for t in range(8):
            lo, hi = max(t - w // 2, 0), min(t + w // 2 - 1, S - 1)
            ed[:, wi, t] = w / float(hi - lo + 1)
            t2 = S - 8 + t
            lo, hi = max(t2 - w // 2, 0), min(t2 + w // 2 - 1, S - 1)
            ed[:, wi, 8 + t] = w / float(hi - lo + 1)
    c["c_edge"] = ed.reshape(128, 64)
    c["c_lstrict"] = np.triu(np.ones((128, 128), np.float32), 1)
    c["c_iota"] = np.ascontiguousarray(np.broadcast_to(np.arange(CAP, dtype=np.float32)[None, :], (128, CAP)))
    c["c_slotid"] = (np.arange(128, dtype=np.float32)[:, None] + 128.0 * np.arange(8, dtype=np.float32)[None, :])
    pw = np.zeros((MIXW, MIXW), np.float32)
    for g in range(4):
        pw[g * 192:(g + 1) * 192, g * 192:(g + 1) * 192] = c_pool_w[0, g]
    c["pwfull"] = pw
    return c


LAYERS = [0, 1, 2, 3]
NSEQ = 2
_CACHE = {}


def kernel(x, mem, w_mem_kv, a_w_in, a_conv_w, a_w_out, b_w_in, b_w_out,
           c_w_in, c_pool_w, c_pool_scale, c_w_out, ln_g, ln_b,
           ffn_w_gate, ffn_w_up, ffn_w_down, moe_router, moe_w_gate, moe_w_up, moe_w_down,
           _layers=None, _debug=False, _ncores=8):
    layers = LAYERS if _layers is None else _layers
    f32 = lambda a: np.ascontiguousarray(np.asarray(a, dtype=np.float32))
    x = f32(x)
    mem = f32(mem)
    consts = _host_consts(f32(ln_g), f32(ln_b), f32(a_conv_w), f32(c_pool_w), f32(c_pool_scale))
    shared = {
        "w_mem_kv": f32(w_mem_kv), "a_w_in": f32(a_w_in), "a_w_out": f32(a_w_out),
        "b_w_in": f32(b_w_in), "b_w_out": f32(b_w_out), "c_w_in": f32(c_w_in), "c_w_out": f32(c_w_out),
        "ffn_w_gate": f32(ffn_w_gate), "ffn_w_up": f32(ffn_w_up), "ffn_w_down": f32(ffn_w_down),
        "moe_router": f32(moe_router), "moe_w_gate": f32(moe_w_gate), "moe_w_up": f32(moe_w_up),
        "moe_w_down": f32(moe_w_down),
    }
    shared.update(consts)
    key = (tuple(layers), NSEQ, _debug)
    if key not in _CACHE:
        _CACHE[key] = build_program(layers, NSEQ, _debug)
    nc = _CACHE[key]
    in_maps = []
    for c in range(_ncores):
        m = dict(shared)
        m["xT"] = np.ascontiguousarray(x[c * NSEQ:(c + 1) * NSEQ].transpose(0, 2, 1))
        m["memT"] = np.ascontiguousarray(mem[c * NSEQ:(c + 1) * NSEQ].transpose(0, 2, 1))
        in_maps.append(m)
    res = run_bass_kernel_spmd(nc, in_maps, core_ids=list(range(_ncores)))
    out = np.empty((_ncores * NSEQ, S, D), np.float32)
    for c in range(_ncores):
        out[c * NSEQ:(c + 1) * NSEQ] = res.results[c]["yT"].transpose(0, 2, 1)
    if _debug:
        return out, [r.get("dbg") for r in res.results]
    return out
```
